# Optimizing a Trainium2 kernel written in Bass

```python
import math
import jax, jax.numpy as jnp
from jax import lax
import numpy as np

D_MODEL = 1024
BATCH = 32
SEQ = 2048
DEPTH = 4

SSM_WIDTH = D_MODEL // 4
SSM_GROUP = 16
SSM_GROUPS = SSM_WIDTH // SSM_GROUP
SSM_STATE = 64
HEAD_DIM = 64
DSA_HEADS = (3 * D_MODEL // 8) // HEAD_DIM
DSA_WIDTH = DSA_HEADS * HEAD_DIM
DSA_LATENT = 2 * HEAD_DIM
IDX_HEADS = 4
IDX_DIM = 64
TOPK_MAX = 256
FOX_HEADS = (3 * D_MODEL // 8) // HEAD_DIM
FOX_WIDTH = FOX_HEADS * HEAD_DIM
REL_BUCKETS = 32
REL_MAX_EXACT = 16
REL_MAX_DIST = 128
FFN_HIDDEN = 2816
BLOCK_Q = 128
EPS = 1e-6
NEG = -1e30
IN_SPLITS = (SSM_WIDTH, DSA_WIDTH, DSA_LATENT, IDX_HEADS * IDX_DIM, IDX_DIM, IDX_HEADS,
             FOX_WIDTH, FOX_WIDTH, FOX_WIDTH, FOX_HEADS, D_MODEL, D_MODEL, D_MODEL)
IN_WIDTH = sum(IN_SPLITS)

kernel_name = 'hybrid_s5_dsa_fox_gated_block'


def _split_points():
    pts, acc = [], 0
    for w in IN_SPLITS[:-1]:
        acc += w
        pts.append(acc)
    return pts


def rms_norm(x, g):
    xf = x.astype(jnp.float32)
    y = xf * lax.rsqrt(jnp.mean(xf * xf, axis=-1, keepdims=True) + EPS)
    return (y * g.astype(jnp.float32)).astype(x.dtype)


def swiglu(h, w_gate, w_up, w_down):
    return (jax.nn.silu(h @ w_gate) * (h @ w_up)) @ w_down


def t5_bucket(dist):
    d = jnp.maximum(dist, 0)
    df = jnp.maximum(d, 1).astype(jnp.float32)
    log_b = REL_MAX_EXACT + (jnp.log(df / REL_MAX_EXACT) / math.log(REL_MAX_DIST / REL_MAX_EXACT)
                             * (REL_BUCKETS - REL_MAX_EXACT)).astype(jnp.int32)
    log_b = jnp.minimum(log_b, REL_BUCKETS - 1)
    return jnp.where(d < REL_MAX_EXACT, d, log_b)


def _cplx_scan_combine(e1, e2):
    a1r, a1i, b1r, b1i = e1
    a2r, a2i, b2r, b2i = e2
    return (a2r * a1r - a2i * a1i,
            a2r * a1i + a2i * a1r,
            a2r * b1r - a2i * b1i + b2r,
            a2r * b1i + a2i * b1r + b2i)


def s5_branch(u, lam_re, lam_im, log_dt, b_re, b_im, c_re, c_im, d_skip, w_glu):
    bsz, seq, _ = u.shape
    f32 = jnp.float32
    uf = u.astype(f32).reshape(bsz, seq, SSM_GROUPS, SSM_GROUP)
    dt = jnp.exp(log_dt.astype(f32))[:, None]
    lr, li = lam_re.astype(f32), lam_im.astype(f32)
    mag = jnp.exp(lr * dt)
    ab_re, ab_im = mag * jnp.cos(li * dt), mag * jnp.sin(li * dt)
    den = lr * lr + li * li
    nr, ni = ab_re - 1.0, ab_im
    s_re = (nr * lr + ni * li) / den
    s_im = (ni * lr - nr * li) / den
    br, bi = b_re.astype(f32), b_im.astype(f32)
    bb_re = s_re[..., None] * br - s_im[..., None] * bi
    bb_im = s_re[..., None] * bi + s_im[..., None] * br
    bu_re = jnp.einsum('gnp,bsgp->bsgn', bb_re, uf)
    bu_im = jnp.einsum('gnp,bsgp->bsgn', bb_im, uf)
    a_re = jnp.broadcast_to(ab_re, bu_re.shape)
    a_im = jnp.broadcast_to(ab_im, bu_im.shape)
    _, _, h_re, h_im = lax.associative_scan(_cplx_scan_combine, (a_re, a_im, bu_re, bu_im), axis=1)
    y = (jnp.einsum('gpn,bsgn->bsgp', c_re.astype(f32), h_re)
         - jnp.einsum('gpn,bsgn->bsgp', c_im.astype(f32), h_im))
    y = y.reshape(bsz, seq, SSM_WIDTH) + d_skip.astype(f32) * u.astype(f32)
    z = jax.nn.gelu(y).astype(u.dtype) @ w_glu
    val, gate = jnp.split(z, 2, axis=-1)
    return val * jax.nn.sigmoid(gate)


def dsa_branch(q, c_kv, q_idx, k_idx, w_idx, kv_norm, w_uk, w_uv, rel_bias, n_keys):
    bsz, seq = q.shape[0], q.shape[1]
    f32 = jnp.float32
    c = rms_norm(c_kv, kv_norm)
    k = c @ w_uk
    v = c @ w_uv
    qi = q_idx.astype(f32) * (IDX_DIM ** -0.5)
    ki = k_idx.astype(f32)
    wi = w_idx.astype(f32) * (IDX_HEADS ** -0.5)
    scale = HEAD_DIM ** -0.5
    gather = jax.vmap(lambda kb, ib: kb[ib])
    outs = []
    for start in range(0, seq, BLOCK_Q):
        end = start + BLOCK_Q
        qpos = jnp.arange(start, end)
        kpos = jnp.arange(end)
        causal = kpos[None, :] <= qpos[:, None]
        dots = jnp.einsum('bqhd,bkd->bqhk', qi[:, start:end], ki[:, :end])
        score = jnp.einsum('bqh,bqhk->bqk', wi[:, start:end], jax.nn.relu(dots))
        score = jnp.where(causal[None], score, NEG)
        n_sel = min(n_keys, end)
        _, sel = lax.top_k(score, n_sel)
        k_sel = gather(k, sel)
        v_sel = gather(v, sel)
        valid = sel <= qpos[None, :, None]
        bias = jnp.moveaxis(rel_bias[t5_bucket(qpos[None, :, None] - sel)], -1, 2)
        s = (jnp.einsum('bqhd,bqkd->bqhk', q[:, start:end], k_sel).astype(f32) * scale
             + bias.astype(f32))
        s = jnp.where(valid[:, :, None, :], s, NEG)
        p = jax.nn.softmax(s, axis=-1).astype(v.dtype)
        outs.append(jnp.einsum('bqhk,bqkd->bqhd', p, v_sel))
    return jnp.concatenate(outs, axis=1).reshape(bsz, seq, DSA_WIDTH)


def fox_branch(q, k, v, f_logit):
    bsz, seq = q.shape[0], q.shape[1]
    f32 = jnp.float32
    cum = jnp.swapaxes(jnp.cumsum(jax.nn.log_sigmoid(f_logit.astype(f32)), axis=1), 1, 2)
    scale = HEAD_DIM ** -0.5
    outs = []
    for start in range(0, seq, BLOCK_Q):
        end = start + BLOCK_Q
        qpos = jnp.arange(start, end)
        kpos = jnp.arange(end)
        causal = kpos[None, :] <= qpos[:, None]
        s = (jnp.einsum('bqhd,bkhd->bhqk', q[:, start:end], k[:, :end]).astype(f32) * scale
             + cum[:, :, start:end, None] - cum[:, :, None, :end])
        s = jnp.where(causal[None, None], s, NEG)
        p = jax.nn.softmax(s, axis=-1).astype(v.dtype)
        outs.append(jnp.einsum('bhqk,bkhd->bqhd', p, v[:, :end]))
    return jnp.concatenate(outs, axis=1).reshape(bsz, seq, FOX_WIDTH)


def setup_inputs(seed: int = 0) -> dict:
    key = jax.random.key(seed)
    ks = jax.random.split(key, 30)
    L, D, F = DEPTH, D_MODEL, FFN_HIDDEN
    G, N, P = SSM_GROUPS, SSM_STATE, SSM_GROUP

    def nrm(k, shape, scale):
        return jax.random.normal(k, shape, jnp.float32) * scale

    def gain(k, shape):
        return 1.0 + 0.01 * jax.random.normal(k, shape, jnp.float32)

    lam_im0 = jnp.pi * jnp.arange(N, dtype=jnp.float32)
    return {
        'x': jax.random.normal(ks[0], (BATCH, SEQ, D), jnp.float32),
        'ffn1_norm': gain(ks[1], (L, D)),
        'ffn1_w_gate': nrm(ks[2], (L, D, F), D ** -0.5),
        'ffn1_w_up': nrm(ks[3], (L, D, F), D ** -0.5),
        'ffn1_w_down': nrm(ks[4], (L, F, D), F ** -0.5),
        'mix_norm': gain(ks[5], (L, D)),
        'w_in': nrm(ks[6], (L, D, IN_WIDTH), D ** -0.5),
        'ssm_lambda_re': -0.5 + 0.01 * jax.random.normal(ks[7], (L, G, N), jnp.float32),
        'ssm_lambda_im': lam_im0 + 0.01 * jax.random.normal(ks[8], (L, G, N), jnp.float32),
        'ssm_log_dt': jax.random.uniform(ks[9], (L, G), jnp.float32, math.log(1e-3), math.log(1e-1)),
        'ssm_b_re': nrm(ks[10], (L, G, N, P), (2 * P) ** -0.5),
        'ssm_b_im': nrm(ks[11], (L, G, N, P), (2 * P) ** -0.5),
        'ssm_c_re': nrm(ks[12], (L, G, P, N), (2 * N) ** -0.5),
        'ssm_c_im': nrm(ks[13], (L, G, P, N), (2 * N) ** -0.5),
        'ssm_d': nrm(ks[14], (L, SSM_WIDTH), 1.0),
        'ssm_w_glu': nrm(ks[15], (L, SSM_WIDTH, 2 * SSM_WIDTH), SSM_WIDTH ** -0.5),
        'dsa_kv_norm': gain(ks[16], (L, DSA_LATENT)),
        'dsa_w_uk': nrm(ks[17], (L, DSA_LATENT, HEAD_DIM), DSA_LATENT ** -0.5),
        'dsa_w_uv': nrm(ks[18], (L, DSA_LATENT, HEAD_DIM), DSA_LATENT ** -0.5),
        'rel_bias': nrm(ks[19], (REL_BUCKETS, DSA_HEADS), 0.1),
        'fox_b_f': jax.random.uniform(ks[20], (L, FOX_HEADS), jnp.float32, 1.0, 4.0),
        'w_branch_ssm': nrm(ks[21], (L, SSM_WIDTH, D), SSM_WIDTH ** -0.5),
        'w_branch_dsa': nrm(ks[22], (L, DSA_WIDTH, D), DSA_WIDTH ** -0.5),
        'w_branch_fox': nrm(ks[23], (L, FOX_WIDTH, D), FOX_WIDTH ** -0.5),
        'w_out': nrm(ks[24], (L, D, D), D ** -0.5),
        'ffn2_norm': gain(ks[25], (L, D)),
        'ffn2_w_gate': nrm(ks[26], (L, D, F), D ** -0.5),
        'ffn2_w_up': nrm(ks[27], (L, D, F), D ** -0.5),
        'ffn2_w_down': nrm(ks[28], (L, F, D), F ** -0.5),
        'final_norm': gain(ks[29], (D,)),
    }


def reference(x, ffn1_norm, ffn1_w_gate, ffn1_w_up, ffn1_w_down, mix_norm, w_in,
              ssm_lambda_re, ssm_lambda_im, ssm_log_dt, ssm_b_re, ssm_b_im, ssm_c_re, ssm_c_im,
              ssm_d, ssm_w_glu, dsa_kv_norm, dsa_w_uk, dsa_w_uv, rel_bias, fox_b_f,
              w_branch_ssm, w_branch_dsa, w_branch_fox, w_out,
              ffn2_norm, ffn2_w_gate, ffn2_w_up, ffn2_w_down, final_norm):
    bsz, seq, _ = x.shape
    n_keys = min(TOPK_MAX, seq // 4)
    pts = _split_points()
    for l in range(DEPTH):
        h = rms_norm(x, ffn1_norm[l])
        x = x + 0.5 * swiglu(h, ffn1_w_gate[l], ffn1_w_up[l], ffn1_w_down[l])
        h = rms_norm(x, mix_norm[l])
        (u, q_b, c_kv, q_i, k_i, w_i, q_c, k_c, v_c, f_c, g_a, g_b, g_c) = jnp.split(h @ w_in[l], pts, axis=-1)
        a = s5_branch(u, ssm_lambda_re[l], ssm_lambda_im[l], ssm_log_dt[l], ssm_b_re[l], ssm_b_im[l],
                      ssm_c_re[l], ssm_c_im[l], ssm_d[l], ssm_w_glu[l])
        b = dsa_branch(q_b.reshape(bsz, seq, DSA_HEADS, HEAD_DIM), c_kv,
                       q_i.reshape(bsz, seq, IDX_HEADS, IDX_DIM), k_i, w_i,
                       dsa_kv_norm[l], dsa_w_uk[l], dsa_w_uv[l], rel_bias, n_keys)
        c = fox_branch(q_c.reshape(bsz, seq, FOX_HEADS, HEAD_DIM),
                       k_c.reshape(bsz, seq, FOX_HEADS, HEAD_DIM),
                       v_c.reshape(bsz, seq, FOX_HEADS, HEAD_DIM),
                       f_c + fox_b_f[l])
        merged = (jax.nn.sigmoid(g_a) * (a @ w_branch_ssm[l])
                  + jax.nn.sigmoid(g_b) * (b @ w_branch_dsa[l])
                  + jax.nn.sigmoid(g_c) * (c @ w_branch_fox[l]))
        x = x + merged @ w_out[l]
        h = rms_norm(x, ffn2_norm[l])
        x = x + 0.5 * swiglu(h, ffn2_w_gate[l], ffn2_w_up[l], ffn2_w_down[l])
    return rms_norm(x, final_norm)
```

```python
import contextlib
import math
import numpy as np
import concourse.bass as bass
import concourse.mybir as mybir
from concourse.bass_utils import run_bass_kernel_spmd

F32 = mybir.dt.float32
BF16 = mybir.dt.bfloat16
ALU = mybir.AluOpType
AF = mybir.ActivationFunctionType
AX = mybir.AxisListType

D = 1024
FH = 2816
NFC = FH // 128
S = 2048
NSEQ = 4
TOK = NSEQ * S
L = 4
TB = 1024
NTB = TOK // TB
EPS = 1e-6
NCORES = 8

IN_SPLITS = (256, 384, 128, 256, 64, 4, 384, 384, 384, 6, 1024, 1024, 1024)
OFF = np.concatenate([[0], np.cumsum(IN_SPLITS)]).tolist()
(O_U, O_QB, O_CKV, O_QI, O_KI, O_WI, O_QC, O_KC, O_VC, O_FC, O_GA, O_GB, O_GC, O_END) = OFF
NFM = 39
NTM = 394


def fm_chunk_cols():
    chunks = []
    for c in range(8):
        chunks.append(list(range(c * 128, (c + 1) * 128)))
    chunks.append(list(range(O_KI, O_KI + 64)) + [-1] * 64)
    for c in range(3):
        chunks.append(list(range(O_QC + c * 128, O_QC + (c + 1) * 128)))
    for c in range(3):
        chunks.append(list(range(O_KC + c * 128, O_KC + (c + 1) * 128)))
    for c in range(24):
        chunks.append(list(range(O_GA + c * 128, O_GA + (c + 1) * 128)))
    assert len(chunks) == NFM
    return chunks


def FL(t):
    return t[:].rearrange("p a b -> p (a b)")


class Buf:
    __slots__ = ("name", "w", "r", "sems", "psum")

    def __init__(self, name, psum=False):
        self.name = name
        self.psum = psum
        self.w = None
        self.r = {}
        self.sems = {}


class Builder:
    def __init__(self, n_layers=L, debug=None):
        self.nl = n_layers
        self.debug = debug or {}
        self.nc = bass.Bass("TRN2", target_bir_lowering=False)
        nc = self.nc
        self.es = contextlib.ExitStack()
        self.engs = {"pe": nc.tensor, "act": nc.scalar, "dve": nc.vector, "pool": nc.gpsimd, "sp": nc.sync}
        self.esem = {}
        self.ecnt = {}
        self.seen = {}
        for e in self.engs:
            self.esem[e] = self.es.enter_context(nc.semaphore("E" + e))
            self.ecnt[e] = 0
            self.seen[e] = {}
        self.dma_sems = []
        self.free_sems = {"hw": [], "sw": []}
        self.phase_sems = []
        self.in_phase = False
        self.nbuf = 0
        self.out_toks = []

    def buf(self, name, psum=False):
        self.nbuf += 1
        return Buf(f"{name}_{self.nbuf}", psum)

    def _wait(self, eng, reads, writes):
        need = {}

        def add(tok):
            key, sem, val = tok
            if key == "pe" and eng == "pe":
                return
            k = id(sem)
            if k not in need or need[k][1] < val:
                need[k] = (sem, val)

        for b in reads:
            if b.w is not None:
                add(b.w)
            if b.psum:
                for kk, t in b.r.items():
                    if kk != eng:
                        add(t)
        for b in writes:
            if b.w is not None:
                add(b.w)
            for t in b.r.values():
                add(t)
        seen = self.seen[eng]
        h = self.engs[eng]
        for k, (sem, val) in need.items():
            if seen.get(k, 0) >= val:
                continue
            h.wait_ge(sem, val)
            seen[k] = val

    def I(self, eng, reads, writes, fn):
        self._wait(eng, reads, writes)
        ins = fn()
        self.ecnt[eng] += 1
        ins.then_inc(self.esem[eng], 1)
        tok = (eng, self.esem[eng], self.ecnt[eng])
        for b in reads:
            b.r[eng] = tok
        for b in writes:
            b.w = tok
            b.r = {}
        return tok

    def dma(self, q, out, in_, owner, reads=(), writes=(), **kw):
        self._wait(q, reads, writes)
        qk = "sw" if q == "pool" else "hw"
        if qk not in owner.sems:
            if self.free_sems[qk]:
                owner.sems[qk] = self.free_sems[qk].pop()
            else:
                owner.sems[qk] = [self.es.enter_context(self.nc.semaphore("D" + qk + owner.name)), 0]
                self.dma_sems.append(owner.sems[qk])
            if self.in_phase:
                self.phase_sems.append((qk, owner.sems[qk]))
        sc = owner.sems[qk]
        ins = self.engs[q].dma_start(out=out, in_=in_, **kw)
        sc[1] += 16
        ins.then_inc(sc[0], 16)
        tok = ("dma:" + qk + owner.name, sc[0], sc[1])
        for b in reads:
            b.r[tok[0]] = tok
        for b in writes:
            b.w = tok
            b.r = {}
        return tok

    def phase_begin(self):
        self.in_phase = True
        self.phase_sems = []

    def phase_end(self):
        self.barrier()
        for qk, sc in self.phase_sems:
            self.free_sems[qk].append(sc)
        self.phase_sems = []
        self.in_phase = False

    def barrier(self):
        for e, h in self.engs.items():
            seen = self.seen[e]
            for e2 in self.engs:
                if e2 == e or self.ecnt[e2] == 0:
                    continue
                k = id(self.esem[e2])
                if seen.get(k, 0) < self.ecnt[e2]:
                    h.wait_ge(self.esem[e2], self.ecnt[e2])
                    seen[k] = self.ecnt[e2]
            for sc in self.dma_sems:
                k = id(sc[0])
                if seen.get(k, 0) < sc[1]:
                    h.wait_ge(sc[0], sc[1])
                    seen[k] = sc[1]

    def sb(self, es, name, shape, dt):
        self.nbuf += 1
        t = es.enter_context(self.nc.sbuf_tensor(f"s_{name}_{self.nbuf}", shape, dt))
        return t

    def dram(self, name, shape, dt, kind="Internal"):
        return self.nc.dram_tensor(name, shape, dt, kind=kind).ap()

    def build(self):
        nc = self.nc
        nl = self.nl
        es = self.es
        self.xT_in = self.dram("xT", [D, TOK], F32, "ExternalInput")
        self.normw_in = self.dram("normw", [128, 3 * L * 8 + 8], F32, "ExternalInput")
        self.wgu_in = self.dram("wgu", [nl * 2 * NFC * 128, 2 * 8 * 128], F32, "ExternalInput")
        self.wd_in = self.dram("wd", [nl * 2 * 8 * 128, NFC * 128], F32, "ExternalInput")
        self.winfm_in = self.dram("winfm", [nl * NFM * 128, 8 * 128], F32, "ExternalInput")
        self.wintm_in = self.dram("wintm", [nl * 128, 8 * NTM], F32, "ExternalInput")
        self.wbr_in = self.dram("wbr", [nl * 8 * 128, 8 * 128], F32, "ExternalInput")
        self.wout_in = self.dram("wout", [nl * 8 * 128, 8 * 128], F32, "ExternalInput")
        self.consts_in = self.dram("consts", [128, 6 * 128], F32, "ExternalInput")
        self.foxbf_in = self.dram("foxbf", [128, L * 6], F32, "ExternalInput")
        self.kvnorm_in = self.dram("kvnorm", [128, L], F32, "ExternalInput")
        self.wuk_in = self.dram("wuk", [L * 128, 64], F32, "ExternalInput")
        self.wuv_in = self.dram("wuv", [L * 128, 64], F32, "ExternalInput")
        self.rb31_in = self.dram("rb31", [1, 6 * S], F32, "ExternalInput")
        self.rbT_in = self.dram("rbT", [128, 2 * 6 * 128], F32, "ExternalInput")
        self.s5p_in = self.dram("s5p", [128, L * 8 * 3], F32, "ExternalInput")
        self.s5b_in = self.dram("s5b", [128, L * 8 * 2 * 16], F32, "ExternalInput")
        self.s5c_in = self.dram("s5c", [128, L * 8 * 2 * 16], F32, "ExternalInput")
        self.s5d_in = self.dram("s5d", [128, L * 2], F32, "ExternalInput")
        self.wglu_in = self.dram("wglu", [L * 128, 2 * 512], F32, "ExternalInput")
        self.out = self.dram("out", [D, TOK], F32, "ExternalOutput")
        self.wbr_b = self.dram("wbr_b", [nl * 8 * 128, 8 * 128], BF16)
        self.wout_b = self.dram("wout_b", [nl * 8 * 128, 8 * 128], BF16)
        self.d_br = self.dram("d_br", [8, 128, TOK], BF16, "ExternalOutput" if self.debug.get("dump") else "Internal")
        self.b_dbr = self.buf("dbr")
        self.xw = self.dram("xw", [D, TOK], F32)
        self.wgu_b = self.dram("wgu_b", [nl * 2 * NFC * 128, 2 * 8 * 128], BF16)
        self.wd_b = self.dram("wd_b", [nl * 2 * 8 * 128, NFC * 128], BF16)
        self.winfm_b = self.dram("winfm_b", [nl * NFM * 128, 8 * 128], BF16)
        self.wintm_b = self.dram("wintm_b", [nl * 128, 8 * NTM], BF16)
        dk = "ExternalOutput" if self.debug.get("dump") else "Internal"
        self.d_fm = self.dram("d_fm", [NFM, 128, TOK], BF16, dk)
        self.d_vc = self.dram("d_vc", [TOK, 384], BF16, dk)
        self.d_wf = self.dram("d_wf", [TOK, 16], F32, dk)
        self.b_xw = self.buf("xw")
        self.b_wcast = self.buf("wcast")
        self.b_dfm = self.buf("dfm")
        self.b_dtm = self.buf("dtm")

        self.normw = self.sb(es, "normw", [128, 3 * L * 8 + 8], F32)
        self.b_normw = self.buf("normw")
        self.ones_bf = self.sb(es, "ones_bf", [128, 128], BF16)
        self.b_ones = self.buf("ones")
        self.eps_t = self.sb(es, "eps_t", [128, 1], F32)
        self.I("dve", [], [self.b_ones], lambda: nc.vector.memset(self.ones_bf[:], 1.0))
        self.I("dve", [], [self.b_ones], lambda: nc.vector.memset(self.eps_t[:], EPS))
        self.dma("sp", self.normw[:], self.normw_in[:, :], self.b_normw, writes=[self.b_normw])
        self.b_cst = self.buf("cst")
        cf = self.sb(es, "constf", [128, 6 * 128], F32)
        self.dma("sp", cf[:], self.consts_in[:, :], self.b_cst, writes=[self.b_cst])
        self.ident_f = cf[:, 0:128]
        self.cneg_f = cf[:, 256:384]
        self.tri_f = cf[:, 384:512]
        self.ones_f = cf[:, 512:640]
        self.ident_bf = self.sb(es, "ident_bf", [128, 128], BF16)
        self.causneg_bf = self.sb(es, "causneg_bf", [128, 128], BF16)
        self.I3 = self.sb(es, "I3", [128, 384], BF16)
        self.one_t = self.sb(es, "one_t", [128, 1], F32)
        self.hpi_t = self.sb(es, "hpi_t", [128, 1], F32)
        self.I("dve", [self.b_cst], [self.b_cst], lambda: nc.vector.tensor_copy(out=self.ident_bf[:], in_=cf[:, 0:128]))
        self.I("dve", [self.b_cst], [self.b_cst], lambda: nc.vector.tensor_copy(out=self.causneg_bf[:], in_=cf[:, 128:256]))
        for k3 in range(3):
            self.I("dve", [self.b_cst], [self.b_cst],
                   lambda: nc.vector.tensor_copy(out=self.I3[:, k3 * 128:(k3 + 1) * 128], in_=cf[:, 0:128]))
        self.I("dve", [], [self.b_cst], lambda: nc.vector.memset(self.one_t[:], 1.0))
        self.I("dve", [], [self.b_cst], lambda: nc.vector.memset(self.hpi_t[:], math.pi / 2))
        self.foxbf = self.sb(es, "foxbf", [128, L * 6], F32)
        self.kvnorm = self.sb(es, "kvnorm", [128, L], F32)
        self.s5d = self.sb(es, "s5d", [128, L * 2], F32)
        self.biasT = self.sb(es, "biasT", [128, 2, 6, 128], BF16)
        self.dma("sp", self.foxbf[:], self.foxbf_in[:, :], self.b_cst, writes=[self.b_cst])
        self.dma("sp", self.kvnorm[:], self.kvnorm_in[:, :], self.b_cst, writes=[self.b_cst])
        self.dma("sp", self.s5d[:], self.s5d_in[:, :], self.b_cst, writes=[self.b_cst])
        self.dma("pool", self.biasT[:].rearrange("p a b c -> p (a b c)"), self.rbT_in[:, :], self.b_cst, writes=[self.b_cst])
        self.ps = []
        self.b_ps = []
        for i in range(8):
            self.ps.append(es.enter_context(nc.psum_tensor(f"ps{i}", [128, 512], F32)))
            self.b_ps.append(self.buf(f"ps{i}", psum=True))

        self.cast_weights()

        self.zero_dbr()
        for l in range(nl + 1):
            if self.debug.get('only_cast'):
                break
            self.phase_a(l)
            if self.debug.get("stop_after_a") == l:
                break
            if l < nl:
                self.phase_b(l)
        self.finish()
        return nc

    def cast_weights(self):
        def cast(dst, src, rows_per):
            n = dst.shape[0]
            for r0 in range(0, n, rows_per):
                r1 = min(n, r0 + rows_per)
                self.dma("pool", dst[r0:r1, :], src[r0:r1, :], self.b_wcast, writes=[self.b_wcast])
        nl = self.nl
        cast(self.wgu_b[0:nl * 2 * NFC * 128, :], self.wgu_in[0:nl * 2 * NFC * 128, :], 128)
        cast(self.wd_b[0:nl * 2 * 8 * 128, :], self.wd_in[0:nl * 2 * 8 * 128, :], 128)
        cast(self.winfm_b[0:nl * NFM * 128, :], self.winfm_in[0:nl * NFM * 128, :], 256)
        cast(self.wintm_b[0:nl * 128, :], self.wintm_in[0:nl * 128, :], 128)
        cast(self.wbr_b, self.wbr_in, 256)
        cast(self.wout_b, self.wout_in, 256)

    def zero_dbr(self):
        nc = self.nc
        self.phase_begin()
        with contextlib.ExitStack() as zes:
            z = self.sb(zes, "zt", [128, 4096], BF16)
            bz = self.buf("zt")
            self.I("dve", [], [bz], lambda: nc.vector.memset(z[:], 0.0))
            for c in range(8):
                for hh in range(2):
                    self.dma("sp", self.d_br[c, :, hh * 4096:(hh + 1) * 4096], z[:], bz, reads=[bz], writes=[self.b_dbr])
            self.phase_end()

    def phase_b(self, l):
        if self.debug.get("no_b"):
            return
        which = self.debug.get("branches", "sdf")
        if "s" in which:
            self.s5_phase(l)
        if "d" in which:
            self.dsa_phase(l)
        if "f" in which:
            self.fox_phase(l)

    def nseq(self):
        return self.debug.get("nseq", NSEQ)

    def s5_phase(self, l):
        nc = self.nc
        self.phase_begin()
        ps, b_ps = self.ps, self.b_ps
        B = self.buf
        LC = 512
        NCH = self.debug.get("nch", S // LC)
        with contextlib.ExitStack() as pes:
            prm = self.sb(pes, "prm", [128, 8, 3], F32)
            Bt = self.sb(pes, "Bt", [128, 8, 2, 16], F32)
            Ct = self.sb(pes, "Ct", [128, 8, 2, 16], F32)
            wglu = self.sb(pes, "wglu", [128, 2, 512], BF16)
            sm = self.sb(pes, "sm", [128, 20, 8], F32)
            tmp16 = self.sb(pes, "tmp16", [128, 16], F32)
            Mre = self.sb(pes, "Mre", [128, 8, 128], F32)
            Mim = self.sb(pes, "Mim", [128, 8, 128], F32)
            BbdT = self.sb(pes, "BbdT", [128, 8, 2, 128], BF16)
            Cbd = self.sb(pes, "Cbd", [128, 8, 2, 128], BF16)
            tabc = self.sb(pes, "tabc", [128, 8, LC], F32)
            tabs = self.sb(pes, "tabs", [128, 8, LC], F32)
            rB = self.sb(pes, "rB", [128, 8, LC], F32)
            tmpa = self.sb(pes, "tmpa", [128, LC], F32)
            tmpb = self.sb(pes, "tmpb", [128, LC], F32)
            b_set = B("s5set")
            b_wg = B("wglu")
            V = lambda k: sm[:, k, :]
            (DT, R, TH, C, Sn, CC, SS, SC, NR, NI, DEN, SRE, SIM, T1, T2, NSK, NSL) = range(17)
            lr, li, ldt = prm[:, :, 0], prm[:, :, 1], prm[:, :, 2]
            self.dma("sp", prm[:].rearrange("p a b -> p (a b)"), self.s5p_in[:, l * 24:(l + 1) * 24], b_set, writes=[b_set])
            self.dma("sp", Bt[:].rearrange("p a b c -> p (a b c)"), self.s5b_in[:, l * 256:(l + 1) * 256], b_set, writes=[b_set])
            self.dma("sp", Ct[:].rearrange("p a b c -> p (a b c)"), self.s5c_in[:, l * 256:(l + 1) * 256], b_set, writes=[b_set])
            self.dma("pool", wglu[:].rearrange("p k m -> p (k m)"), self.wglu_in[l * 128:(l + 1) * 128, :], b_wg, writes=[b_wg])

            def dv(fn):
                self.I("dve", [b_set, self.b_cst], [b_set], fn)

            def ac(fn):
                self.I("act", [b_set, self.b_cst], [b_set], fn)

            def tt(o, a, b, op):
                dv(lambda: nc.vector.tensor_tensor(out=o, in0=a, in1=b, op=op))

            ac(lambda: nc.scalar.activation(out=V(DT), in_=ldt, func=AF.Exp))
            tt(V(T1), lr, V(DT), ALU.mult)
            ac(lambda: nc.scalar.activation(out=V(R), in_=V(T1), func=AF.Exp))
            tt(V(TH), li, V(DT), ALU.mult)
            ac(lambda: nc.scalar.activation(out=V(Sn), in_=V(TH), func=AF.Sin, scale=1.0 / 16))
            ac(lambda: nc.scalar.activation(out=V(C), in_=V(TH), func=AF.Sin, scale=1.0 / 16, bias=self.hpi_t[:]))
            for _ in range(4):
                tt(V(CC), V(C), V(C), ALU.mult)
                tt(V(SS), V(Sn), V(Sn), ALU.mult)
                tt(V(SC), V(Sn), V(C), ALU.mult)
                tt(V(C), V(CC), V(SS), ALU.subtract)
                dv(lambda: nc.vector.tensor_scalar(out=V(Sn), in0=V(SC), scalar1=2.0, scalar2=None, op0=ALU.mult))
            tt(V(T1), V(R), V(C), ALU.mult)
            dv(lambda: nc.vector.tensor_scalar(out=V(NR), in0=V(T1), scalar1=-1.0, scalar2=None, op0=ALU.add))
            tt(V(NI), V(R), V(Sn), ALU.mult)
            tt(V(DEN), lr, lr, ALU.mult)
            tt(V(T1), li, li, ALU.mult)
            tt(V(DEN), V(DEN), V(T1), ALU.add)
            dv(lambda: nc.vector.reciprocal(out=V(DEN), in_=V(DEN)))
            tt(V(T1), V(NR), lr, ALU.mult)
            tt(V(T2), V(NI), li, ALU.mult)
            tt(V(T1), V(T1), V(T2), ALU.add)
            tt(V(SRE), V(T1), V(DEN), ALU.mult)
            tt(V(T1), V(NI), lr, ALU.mult)
            tt(V(T2), V(NR), li, ALU.mult)
            tt(V(T1), V(T1), V(T2), ALU.subtract)
            tt(V(SIM), V(T1), V(DEN), ALU.mult)
            dv(lambda: nc.vector.memset(Mre[:].rearrange("p a b -> p (a b)"), 0.0))
            dv(lambda: nc.vector.memset(Mim[:].rearrange("p a b -> p (a b)"), 0.0))
            dv(lambda: nc.vector.memset(Cbd[:].rearrange("p a b c -> p (a b c)"), 0.0))
            for sc in range(8):
                for hp in range(2):
                    P = slice(hp * 64, hp * 64 + 64)
                    col = ((2 * sc + hp) % 8) * 16
                    cs = slice(col, col + 16)
                    sre_c, sim_c = sm[P, SRE, sc:sc + 1], sm[P, SIM, sc:sc + 1]
                    dv(lambda: nc.vector.tensor_scalar(out=tmp16[P, :], in0=Bt[P, sc, 1, :], scalar1=sim_c, scalar2=None,
                                                       op0=ALU.mult))
                    dv(lambda: nc.vector.scalar_tensor_tensor(out=Mre[P, sc, cs], in0=Bt[P, sc, 0, :], scalar=sre_c,
                                                              in1=tmp16[P, :], op0=ALU.mult, op1=ALU.subtract))
                    dv(lambda: nc.vector.tensor_scalar(out=tmp16[P, :], in0=Bt[P, sc, 1, :], scalar1=sre_c, scalar2=None,
                                                       op0=ALU.mult))
                    dv(lambda: nc.vector.scalar_tensor_tensor(out=Mim[P, sc, cs], in0=Bt[P, sc, 0, :], scalar=sim_c,
                                                              in1=tmp16[P, :], op0=ALU.mult, op1=ALU.add))
                    dv(lambda: nc.vector.tensor_copy(out=Cbd[P, sc, 0, cs], in_=Ct[P, sc, 0, :]))
                    dv(lambda: nc.vector.tensor_scalar(out=Cbd[P, sc, 1, cs], in0=Ct[P, sc, 1, :], scalar1=-1.0,
                                                       scalar2=None, op0=ALU.mult))
            for sc in range(8):
                for ri, M in enumerate((Mre, Mim)):
                    pp, bp = ps[(sc * 2 + ri) % 4], b_ps[(sc * 2 + ri) % 4]
                    self.I("pe", [b_set, self.b_cst], [bp],
                           lambda: nc.tensor.transpose(out=pp[:, 0:128], in_=M[:, sc, :], identity=self.ident_f))
                    self.I("act", [bp, b_set], [b_set],
                           lambda: nc.scalar.copy(out=BbdT[:, sc, ri, :], in_=pp[:, 0:128]))
            dv(lambda: nc.vector.tensor_copy(out=tabc[:, :, 0], in_=V(C)))
            dv(lambda: nc.vector.tensor_copy(out=tabs[:, :, 0], in_=V(Sn)))
            k = 1
            while k < LC:
                dv(lambda: nc.vector.tensor_scalar(out=V(NSK), in0=tabs[:, :, k - 1], scalar1=-1.0, scalar2=None,
                                                   op0=ALU.mult))
                for sc in range(8):
                    ck, sk, nsk = tabc[:, sc, k - 1:k], tabs[:, sc, k - 1:k], sm[:, NSK, sc:sc + 1]
                    dv(lambda: nc.vector.tensor_scalar(out=tmpa[:, 0:k], in0=tabc[:, sc, 0:k], scalar1=ck, scalar2=None,
                                                       op0=ALU.mult))
                    dv(lambda: nc.vector.tensor_scalar(out=tmpb[:, 0:k], in0=tabs[:, sc, 0:k], scalar1=ck, scalar2=None,
                                                       op0=ALU.mult))
                    dv(lambda: nc.vector.scalar_tensor_tensor(out=tabc[:, sc, k:2 * k], in0=tabs[:, sc, 0:k], scalar=nsk,
                                                              in1=tmpa[:, 0:k], op0=ALU.mult, op1=ALU.add))
                    dv(lambda: nc.vector.scalar_tensor_tensor(out=tabs[:, sc, k:2 * k], in0=tabc[:, sc, 0:k], scalar=sk,
                                                              in1=tmpb[:, 0:k], op0=ALU.mult, op1=ALU.add))
                k *= 2
            dv(lambda: nc.vector.tensor_scalar(out=V(NSL), in0=tabs[:, :, LC - 1], scalar1=-1.0, scalar2=None, op0=ALU.mult))
            for sc in range(8):
                dv(lambda: nc.vector.tensor_scalar(out=rB[:, sc, :], in0=tabc[:, sc, :], scalar1=0.0,
                                                   scalar2=sm[:, R, sc:sc + 1], op0=ALU.mult, op1=ALU.add))

            uT = self.sb(pes, "uT", [128, 2, S], BF16)
            ast = self.sb(pes, "ast", [128, 2, S], BF16)
            carry = self.sb(pes, "carry", [128, 8, 2], F32)
            ctmp = self.sb(pes, "ctmp", [128, 2], F32)
            glT = self.sb(pes, "glT", [128, 2, LC], BF16)
            y2 = self.sb(pes, "y2", [128, LC], F32)
            x2 = self.sb(pes, "x2", [128, LC], F32)
            sgg = self.sb(pes, "sgg", [128, LC], F32)
            names = ("t1", "t2", "mre", "mim", "gre", "gim", "u1", "u2")
            W = {n: [self.sb(pes, f"{n}{i}", [128, LC], F32) for i in range(2)] for n in names}
            Hh = {n: [self.sb(pes, f"{n}{i}", [128, LC], BF16) for i in range(2)] for n in ("hre", "him")}
            bW = {n: [B(n) for _ in range(2)] for n in names + ("hre", "him")}
            b_uT, b_ast, b_carry, b_ctmp, b_glT, b_y2, b_x2, b_sgg = [B(n) for n in
                                                                     ("uT", "ast", "carry", "ctmp", "glT", "y2", "x2", "sgg")]
            for s in range(self.nseq()):
                t0 = s * S
                self.dma("sp", uT[:], self.d_fm[0:2, :, t0:t0 + S].rearrange("k p t -> p k t"), b_uT,
                         reads=[self.b_dfm], writes=[b_uT])
                self.I("dve", [], [b_carry], lambda: nc.vector.memset(carry[:].rearrange("p a b -> p (a b)"), 0.0))
                if NCH < S // LC:
                    self.I("dve", [], [b_ast], lambda: nc.vector.memset(ast[:].rearrange("p a b -> p (a b)"), 0.0))
                for c in range(NCH):
                    cs = slice(c * LC, (c + 1) * LC)
                    for cc in range(2):
                        py, bpy = ps[4 + cc], b_ps[4 + cc]
                        for kk, sc in enumerate(range(4 * cc, 4 * cc + 4)):
                            a = sc % 2
                            pbr, bpbr = ps[a * 2], b_ps[a * 2]
                            pbi, bpbi = ps[a * 2 + 1], b_ps[a * 2 + 1]
                            self.I("pe", [b_set, b_uT], [bpbr],
                                   lambda: nc.tensor.matmul(pbr[:], lhsT=BbdT[:, sc, 0, :], rhs=uT[:, cc, cs],
                                                            start=True, stop=True))
                            self.I("pe", [b_set, b_uT], [bpbi],
                                   lambda: nc.tensor.matmul(pbi[:], lhsT=BbdT[:, sc, 1, :], rhs=uT[:, cc, cs],
                                                            start=True, stop=True))
                            tc_, ts_ = tabc[:, sc, :], tabs[:, sc, :]
                            t1, t2, mre, mim = W["t1"][a], W["t2"][a], W["mre"][a], W["mim"][a]
                            gre, gim, u1, u2 = W["gre"][a], W["gim"][a], W["u1"][a], W["u2"][a]
                            hre, him = Hh["hre"][a], Hh["him"][a]

                            def TT(eng, reads, wr, o, x, y, op):
                                e = nc.vector if eng == "dve" else nc.gpsimd
                                self.I(eng, reads, wr, lambda: e.tensor_tensor(out=o, in0=x, in1=y, op=op))

                            TT("dve", [b_set, bpbr], [bW["t1"][a]], t1[:], tc_, pbr[:], ALU.mult)
                            TT("dve", [b_set, bpbi], [bW["t2"][a]], t2[:], ts_, pbi[:], ALU.mult)
                            TT("dve", [bW["t1"][a], bW["t2"][a]], [bW["mre"][a]], mre[:], t1[:], t2[:], ALU.add)
                            TT("dve", [b_set, bpbi], [bW["t1"][a]], t1[:], tc_, pbi[:], ALU.mult)
                            TT("dve", [b_set, bpbr], [bW["t2"][a]], t2[:], ts_, pbr[:], ALU.mult)
                            TT("dve", [bW["t1"][a], bW["t2"][a]], [bW["mim"][a]], mim[:], t1[:], t2[:], ALU.subtract)
                            self.I("dve", [b_set, bW["mre"][a], b_carry], [bW["gre"][a]],
                                   lambda: nc.vector.tensor_tensor_scan(out=gre[:], data0=rB[:, sc, :], data1=mre[:],
                                                                        initial=carry[:, sc, 0:1], op0=ALU.mult,
                                                                        op1=ALU.add))
                            self.I("dve", [b_set, bW["mim"][a], b_carry], [bW["gim"][a]],
                                   lambda: nc.vector.tensor_tensor_scan(out=gim[:], data0=rB[:, sc, :], data1=mim[:],
                                                                        initial=carry[:, sc, 1:2], op0=ALU.mult,
                                                                        op1=ALU.add))
                            cl, sl_, nsl = tabc[:, sc, LC - 1:LC], tabs[:, sc, LC - 1:LC], sm[:, NSL, sc:sc + 1]
                            self.I("dve", [bW["gre"][a], b_set], [b_ctmp],
                                   lambda: nc.vector.tensor_scalar(out=ctmp[:, 0:1], in0=gre[:, LC - 1:LC], scalar1=cl,
                                                                   scalar2=None, op0=ALU.mult))
                            self.I("dve", [bW["gre"][a], b_set], [b_ctmp],
                                   lambda: nc.vector.tensor_scalar(out=ctmp[:, 1:2], in0=gre[:, LC - 1:LC], scalar1=sl_,
                                                                   scalar2=None, op0=ALU.mult))
                            self.I("dve", [bW["gim"][a], b_set, b_ctmp], [b_carry],
                                   lambda: nc.vector.scalar_tensor_tensor(out=carry[:, sc, 0:1], in0=gim[:, LC - 1:LC],
                                                                          scalar=nsl, in1=ctmp[:, 0:1], op0=ALU.mult,
                                                                          op1=ALU.add))
                            self.I("dve", [bW["gim"][a], b_set, b_ctmp], [b_carry],
                                   lambda: nc.vector.scalar_tensor_tensor(out=carry[:, sc, 1:2], in0=gim[:, LC - 1:LC],
                                                                          scalar=cl, in1=ctmp[:, 1:2], op0=ALU.mult,
                                                                          op1=ALU.add))
                            TT("pool", [b_set, bW["gre"][a]], [bW["u1"][a]], u1[:], tc_, gre[:], ALU.mult)
                            TT("pool", [b_set, bW["gim"][a]], [bW["u2"][a]], u2[:], ts_, gim[:], ALU.mult)
                            TT("pool", [bW["u1"][a], bW["u2"][a]], [bW["hre"][a]], hre[:], u1[:], u2[:], ALU.subtract)
                            TT("pool", [b_set, bW["gre"][a]], [bW["u1"][a]], u1[:], ts_, gre[:], ALU.mult)
                            TT("pool", [b_set, bW["gim"][a]], [bW["u2"][a]], u2[:], tc_, gim[:], ALU.mult)
                            TT("pool", [bW["u1"][a], bW["u2"][a]], [bW["him"][a]], him[:], u1[:], u2[:], ALU.add)
                            self.I("pe", [b_set, bW["hre"][a]], [bpy],
                                   lambda: nc.tensor.matmul(py[:], lhsT=Cbd[:, sc, 0, :], rhs=hre[:], start=(kk == 0),
                                                            stop=False))
                            self.I("pe", [b_set, bW["him"][a]], [bpy],
                                   lambda: nc.tensor.matmul(py[:], lhsT=Cbd[:, sc, 1, :], rhs=him[:], start=False,
                                                            stop=(kk == 3)))
                        self.I("dve", [bpy, b_uT, self.b_cst], [b_y2],
                               lambda: nc.vector.scalar_tensor_tensor(out=y2[:], in0=uT[:, cc, cs],
                                                                      scalar=self.s5d[:, l * 2 + cc:l * 2 + cc + 1],
                                                                      in1=py[:], op0=ALU.mult, op1=ALU.add))
                        self.I("dve", [b_y2], [b_x2], lambda: nc.vector.tensor_tensor(out=x2[:], in0=y2[:], in1=y2[:], op=ALU.mult))
                        self.I("dve", [b_x2], [b_x2],
                               lambda: nc.vector.tensor_scalar(out=x2[:], in0=x2[:], scalar1=0.044715, scalar2=1.0,
                                                               op0=ALU.mult, op1=ALU.add))
                        self.I("dve", [b_x2, b_y2], [b_x2], lambda: nc.vector.tensor_tensor(out=x2[:], in0=x2[:], in1=y2[:], op=ALU.mult))
                        self.I("act", [b_x2], [b_sgg],
                               lambda: nc.scalar.activation(out=sgg[:], in_=x2[:], func=AF.Sigmoid, scale=2.0 * 0.7978845608028654))
                        self.I("dve", [b_sgg, b_y2], [b_glT],
                               lambda: nc.vector.tensor_tensor(out=glT[:, cc, :], in0=y2[:], in1=sgg[:], op=ALU.mult))
                    for o in range(2):
                        pg_, bpg = ps[6], b_ps[6]
                        pv_, bpv = ps[7], b_ps[7]
                        for kc in range(2):
                            self.I("pe", [b_wg, b_glT], [bpg],
                                   lambda: nc.tensor.matmul(pg_[:], lhsT=wglu[:, kc, (2 + o) * 128:(3 + o) * 128],
                                                            rhs=glT[:, kc, :], start=(kc == 0), stop=(kc == 1)))
                        for kc in range(2):
                            self.I("pe", [b_wg, b_glT], [bpv],
                                   lambda: nc.tensor.matmul(pv_[:], lhsT=wglu[:, kc, o * 128:(o + 1) * 128],
                                                            rhs=glT[:, kc, :], start=(kc == 0), stop=(kc == 1)))
                        self.I("act", [bpg], [b_sgg], lambda: nc.scalar.activation(out=sgg[:], in_=pg_[:], func=AF.Sigmoid))
                        self.I("dve", [bpv, b_sgg], [b_ast],
                               lambda: nc.vector.tensor_tensor(out=ast[:, o, cs], in0=sgg[:], in1=pv_[:], op=ALU.mult))
                self.dma("pool", self.d_br[0:2, :, t0:t0 + S].rearrange("k p t -> p k t"), ast[:], b_ast,
                         reads=[b_ast], writes=[self.b_dbr])
            self.phase_end()

    def fox_phase(self, l):
        nc = self.nc
        self.phase_begin()
        ps, b_ps = self.ps, self.b_ps
        B = self.buf
        NB = S // 128
        NJ = self.debug.get("nj", NB)
        with contextlib.ExitStack() as pes:
            wf = self.sb(pes, "wf", [128, NB, 16], F32)
            V = self.sb(pes, "fV", [128, NB, 384], BF16)
            qT = [self.sb(pes, f"fq{i}", [64, S], BF16) for i in range(2)]
            kT = [self.sb(pes, f"fk{i}", [64, S], BF16) for i in range(2)]
            cst = [self.sb(pes, f"fc{i}", [64, S], BF16) for i in range(2)]
            fb = self.sb(pes, "fb", [128, NB, 6], F32)
            tb = self.sb(pes, "ftb", [128, NB, 6], F32)
            offs = self.sb(pes, "foffs", [128, NB, 6], F32)
            cumT = self.sb(pes, "fcum", [128, NB, 6], F32)
            refB = self.sb(pes, "fref", [128, NB, 6], F32)
            biasJ = [self.sb(pes, f"fbj{i}", [128, NB], F32) for i in range(2)]
            PT = [self.sb(pes, f"fpt{i}", [128, 128], BF16) for i in range(3)]
            rec = [self.sb(pes, f"frec{i}", [64, 128], F32) for i in range(2)]
            b_wf, b_V, b_fb, b_tb, b_offs, b_cum, b_ref = B("wf"), B("V"), B("fb"), B("tb"), B("offs"), B("cum"), B("ref")
            b_qT = [B("q") for _ in range(2)]
            b_kT = [B("k") for _ in range(2)]
            b_cst = [B("c") for _ in range(2)]
            b_bj = [B("bj") for _ in range(2)]
            b_PT = [B("pt") for _ in range(3)]
            b_rec = [B("rec") for _ in range(2)]
            cnt = 0
            if NJ < NB:
                for i2 in range(2):
                    self.I("dve", [], [b_cst[i2]], lambda: nc.vector.memset(cst[i2][:], 0.0))
            for s in range(self.nseq()):
                t0 = s * S
                self.dma("sp", wf[:], self.d_wf[t0:t0 + S, :].rearrange("(n p) c -> p n c", p=128), b_wf,
                         reads=[self.b_dtm], writes=[b_wf])
                self.dma("sp", V[:], self.d_vc[t0:t0 + S, :].rearrange("(n p) c -> p n c", p=128), b_V,
                         reads=[self.b_dtm], writes=[b_V])
                for h in range(6):
                    self.I("dve", [b_wf, self.b_cst], [b_fb],
                           lambda: nc.vector.tensor_scalar(out=fb[:, :, h], in0=wf[:, :, 4 + h],
                                                           scalar1=self.foxbf[:, l * 6 + h:l * 6 + h + 1],
                                                           scalar2=None, op0=ALU.add))
                self.I("act", [b_fb], [b_fb], lambda: nc.scalar.activation(out=FL(fb), in_=FL(fb), func=AF.Exp, scale=-1.0))
                self.I("act", [b_fb, self.b_cst], [b_fb],
                       lambda: nc.scalar.activation(out=FL(fb), in_=FL(fb), func=AF.Ln, bias=self.one_t[:], scale=1.0))
                self.I("pe", [b_fb, self.b_cst], [b_ps[0]],
                       lambda: nc.tensor.matmul(ps[0][:, 0:NB * 6], lhsT=self.tri_f[:], rhs=FL(fb), start=True, stop=True))
                self.I("pe", [b_fb, self.b_cst], [b_ps[1]],
                       lambda: nc.tensor.matmul(ps[1][:, 0:NB * 6], lhsT=self.ones_f[:], rhs=FL(fb), start=True, stop=True))
                self.I("dve", [b_ps[1]], [b_tb], lambda: nc.vector.tensor_copy(out=FL(tb), in_=ps[1][:, 0:NB * 6]))
                self.I("dve", [], [b_offs], lambda: nc.vector.memset(offs[:, 0, :], 0.0))
                for b in range(1, NB):
                    self.I("dve", [b_offs, b_tb], [b_offs],
                           lambda: nc.vector.tensor_tensor(out=offs[:, b, :], in0=offs[:, b - 1, :], in1=tb[:, b - 1, :],
                                                           op=ALU.add))
                self.I("dve", [b_ps[0], b_offs], [b_cum],
                       lambda: nc.vector.tensor_tensor(out=FL(cumT), in0=FL(offs), in1=ps[0][:, 0:NB * 6], op=ALU.add))
                self.I("dve", [b_offs, b_tb], [b_ref],
                       lambda: nc.vector.tensor_tensor(out=FL(refB), in0=FL(offs), in1=FL(tb), op=ALU.add))
                for h in range(6):
                    sl = h % 2
                    r0 = (h % 2) * 64
                    self.dma("sp", qT[sl][:], self.d_fm[9 + h // 2, r0:r0 + 64, t0:t0 + S], b_qT[sl],
                             reads=[self.b_dfm], writes=[b_qT[sl]])
                    self.dma("sp", kT[sl][:], self.d_fm[12 + h // 2, r0:r0 + 64, t0:t0 + S], b_kT[sl],
                             reads=[self.b_dfm], writes=[b_kT[sl]])
                    for j in range(NJ):
                        bj, b_bjj = biasJ[j % 2], b_bj[j % 2]
                        self.I("dve", [b_cum, b_ref], [b_bjj],
                               lambda: nc.vector.tensor_scalar(out=bj[:, 0:j + 1], in0=cumT[:, 0:j + 1, h],
                                                               scalar1=refB[:, j, h:h + 1], scalar2=None,
                                                               op0=ALU.subtract))
                        po, bpo = ps[2 + (j % 2) * 2], b_ps[2 + (j % 2) * 2]
                        pso, bpso = ps[3 + (j % 2) * 2], b_ps[3 + (j % 2) * 2]
                        js = slice(j * 128, (j + 1) * 128)
                        for i in range(j + 1):
                            cnt += 1
                            pq, bpq = ps[6 + cnt % 2], b_ps[6 + cnt % 2]
                            pt, bpt = PT[cnt % 3], b_PT[cnt % 3]
                            isl = slice(i * 128, (i + 1) * 128)
                            self.I("pe", [b_kT[sl], b_qT[sl]], [bpq],
                                   lambda: nc.tensor.matmul(pq[:, 0:128], lhsT=kT[sl][:, isl], rhs=qT[sl][:, js],
                                                            start=True, stop=(i != j)))
                            if i == j:
                                self.I("pe", [self.b_cst], [bpq],
                                       lambda: nc.tensor.matmul(pq[:, 0:128], lhsT=self.ident_bf[:], rhs=self.causneg_bf[:],
                                                                start=False, stop=True))
                            self.I("act", [bpq, b_bjj], [bpt],
                                   lambda: nc.scalar.activation(out=pt[:], in_=pq[:, 0:128], func=AF.Exp,
                                                                bias=bj[:, i:i + 1], scale=0.125))
                            self.I("pe", [b_V, bpt], [bpo],
                                   lambda: nc.tensor.matmul(po[0:64, 0:128], lhsT=V[:, i, h * 64:(h + 1) * 64], rhs=pt[:],
                                                            start=(i == 0), stop=(i == j)))
                            self.I("pe", [self.b_ones, bpt], [bpso],
                                   lambda: nc.tensor.matmul(pso[0:64, 0:128], lhsT=self.ones_bf[:, 0:64], rhs=pt[:],
                                                            start=(i == 0), stop=(i == j)))
                        rc, brc = rec[j % 2], b_rec[j % 2]
                        self.I("dve", [bpso], [brc], lambda: nc.vector.reciprocal(out=rc[:], in_=pso[0:64, 0:128]))
                        self.I("dve", [bpo, brc], [b_cst[sl]],
                               lambda: nc.vector.tensor_tensor(out=cst[sl][:, js], in0=rc[:], in1=po[0:64, 0:128],
                                                               op=ALU.mult))
                    self.dma("pool", self.d_br[5 + h // 2, r0:r0 + 64, t0:t0 + S], cst[sl][:], b_cst[sl],
                             reads=[b_cst[sl]], writes=[self.b_dbr])
            self.phase_end()

    def dsa_phase(self, l):
        nc = self.nc
        self.phase_begin()
        ps, b_ps = self.ps, self.b_ps
        B = self.buf
        NB = S // 128
        NJ = self.debug.get("nj", NB)
        NR = self.debug.get("nrounds", 32)
        with contextlib.ExitStack() as pes:
            wf = self.sb(pes, "dwf", [128, NB, 16], F32)
            ckv = self.sb(pes, "ckv", [128, S], BF16)
            cn = self.sb(pes, "cn", [128, S], BF16)
            sq = [self.sb(pes, f"dsq{i}", [128, 512], BF16) for i in range(2)]
            rs = [self.sb(pes, f"drs{i}", [128, 512], F32) for i in range(2)]
            Kp = self.sb(pes, "Kp", [65, S], BF16)
            Vd = self.sb(pes, "Vd", [128, NB, 64], BF16)
            Qp = self.sb(pes, "Qp", [65, 6, S], BF16)
            qi = self.sb(pes, "qi", [64, 4, S], BF16)
            ki = self.sb(pes, "ki", [64, S], BF16)
            wuk = self.sb(pes, "wuk", [128, 64], BF16)
            wuv = self.sb(pes, "wuv", [128, 64], BF16)
            score = self.sb(pes, "score", [128, S], F32)
            work = self.sb(pes, "work", [128, S], F32)
            rr = [self.sb(pes, f"rr{i}", [128, 512], F32) for i in range(2)]
            m8 = [self.sb(pes, f"m8{i}", [128, 8], F32) for i in range(2)]
            thr0 = self.sb(pes, "thr0", [128, 1], F32)
            negm = [self.sb(pes, f"negm{i}", [128, S], BF16) for i in range(2)]
            PT = [self.sb(pes, f"dpt{i}", [128, 384], BF16) for i in range(3)]
            rec = [self.sb(pes, f"drec{i}", [64, 384], F32) for i in range(2)]
            bst = self.sb(pes, "bst", [64, NB, 6, 128], BF16)
            (b_wf, b_ckv, b_cn, b_Kp, b_Vd, b_Qp, b_qi, b_ki, b_w, b_score, b_work, b_thr0, b_bst) = [
                B(n) for n in ("wf", "ckv", "cn", "Kp", "Vd", "Qp", "qi", "ki", "w", "score", "work", "thr0", "bst")]
            b_sq = [B("sq") for _ in range(2)]
            b_rs = [B("rs") for _ in range(2)]
            b_rr = [B("rr") for _ in range(2)]
            b_m8 = [B("m8") for _ in range(2)]
            b_negm = [B("negm") for _ in range(2)]
            b_PT = [B("pt") for _ in range(3)]
            b_rec = [B("rec") for _ in range(2)]
            self.dma("pool", wuk[:], self.wuk_in[l * 128:(l + 1) * 128, :], b_w, writes=[b_w])
            self.dma("pool", wuv[:], self.wuv_in[l * 128:(l + 1) * 128, :], b_w, writes=[b_w])
            self.I("dve", [], [b_Kp], lambda: nc.vector.memset(Kp[64:65, :], 1.0))
            self.I("dve", [], [b_thr0], lambda: nc.vector.memset(thr0[:], -1e29))
            if NJ < NB:
                self.I("dve", [], [b_bst], lambda: nc.vector.memset(bst[:].rearrange("p a b c -> p (a b c)"), 0.0))
            self.dma("pool", Qp[64:65, :, :], self.rb31_in[0:1, :].rearrange("o (h t) -> o h t", h=6), b_Qp, writes=[b_Qp])
            cnt = 0
            for s in range(self.nseq()):
                t0 = s * S
                self.dma("sp", wf[:], self.d_wf[t0:t0 + S, :].rearrange("(n p) c -> p n c", p=128), b_wf,
                         reads=[self.b_dtm], writes=[b_wf])
                self.dma("sp", ckv[:], self.d_fm[5, :, t0:t0 + S], b_ckv, reads=[self.b_dfm], writes=[b_ckv])
                for h in range(6):
                    r0 = (h % 2) * 64
                    self.dma("sp", Qp[0:64, h, :], self.d_fm[2 + h // 2, r0:r0 + 64, t0:t0 + S], b_Qp,
                             reads=[self.b_dfm], writes=[b_Qp])
                for h in range(4):
                    r0 = (h % 2) * 64
                    self.dma("sp", qi[:, h, :], self.d_fm[6 + h // 2, r0:r0 + 64, t0:t0 + S], b_qi,
                             reads=[self.b_dfm], writes=[b_qi])
                self.dma("sp", ki[:], self.d_fm[8, 0:64, t0:t0 + S], b_ki, reads=[self.b_dfm], writes=[b_ki])
                for c4 in range(4):
                    cs = slice(c4 * 512, (c4 + 1) * 512)
                    i2 = c4 % 2
                    pp, bp = ps[i2], b_ps[i2]
                    self.I("act", [b_ckv], [b_sq[i2]],
                           lambda: nc.scalar.activation(out=sq[i2][:], in_=ckv[:, cs], func=AF.Square))
                    self.I("pe", [b_sq[i2], self.b_ones], [bp],
                           lambda: nc.tensor.matmul(pp[:], lhsT=self.ones_bf[:], rhs=sq[i2][:], start=True, stop=True))
                    self.I("act", [bp, self.b_ones], [b_rs[i2]],
                           lambda: nc.scalar.activation(out=rs[i2][:], in_=pp[:], func=AF.Sqrt, bias=self.eps_t[:],
                                                        scale=1.0 / 128))
                    self.I("dve", [b_rs[i2]], [b_rs[i2]], lambda: nc.vector.reciprocal(out=rs[i2][:], in_=rs[i2][:]))
                    self.I("dve", [b_ckv, b_rs[i2], self.b_cst], [b_cn],
                           lambda: nc.vector.scalar_tensor_tensor(out=cn[:, cs], in0=ckv[:, cs],
                                                                  scalar=self.kvnorm[:, l:l + 1], in1=rs[i2][:],
                                                                  op0=ALU.mult, op1=ALU.mult))
                for c4 in range(4):
                    cs = slice(c4 * 512, (c4 + 1) * 512)
                    pp, bp = ps[2 + c4 % 2], b_ps[2 + c4 % 2]
                    self.I("pe", [b_w, b_cn], [bp],
                           lambda: nc.tensor.matmul(pp[0:64, :], lhsT=wuk[:], rhs=cn[:, cs], start=True, stop=True))
                    self.I("act", [bp], [b_Kp], lambda: nc.scalar.copy(out=Kp[0:64, cs], in_=pp[0:64, :]))
                for blk in range(NB):
                    pp, bp = ps[4 + blk % 2], b_ps[4 + blk % 2]
                    self.I("pe", [b_w, b_cn], [bp],
                           lambda: nc.tensor.matmul(pp[:, 0:64], lhsT=cn[:, blk * 128:(blk + 1) * 128], rhs=wuv[:],
                                                    start=True, stop=True))
                    self.I("dve", [bp], [b_Vd], lambda: nc.vector.tensor_copy(out=Vd[:, blk, :], in_=pp[:, 0:64]))
                for j in range(NJ):
                    nk = (j + 1) * 128
                    js = slice(j * 128, (j + 1) * 128)
                    for h in range(4):
                        for c0 in range(0, nk, 512):
                            w = min(512, nk - c0)
                            cnt += 1
                            pi, bpi = ps[cnt % 2], b_ps[cnt % 2]
                            rt, brt = rr[cnt % 2], b_rr[cnt % 2]
                            self.I("pe", [b_qi, b_ki], [bpi],
                                   lambda: nc.tensor.matmul(pi[:, 0:w], lhsT=qi[:, h, js], rhs=ki[:, c0:c0 + w],
                                                            start=True, stop=True))
                            self.I("act", [bpi], [brt],
                                   lambda: nc.scalar.activation(out=rt[:, 0:w], in_=pi[:, 0:w], func=AF.Relu))
                            if h == 0:
                                self.I("dve", [brt, b_wf], [b_score],
                                       lambda: nc.vector.tensor_scalar(out=score[:, c0:c0 + w], in0=rt[:, 0:w],
                                                                       scalar1=wf[:, j, 0:1], scalar2=None, op0=ALU.mult))
                            else:
                                self.I("dve", [brt, b_wf, b_score], [b_score],
                                       lambda: nc.vector.scalar_tensor_tensor(
                                           out=score[:, c0:c0 + w], in0=rt[:, 0:w], scalar=wf[:, j, h:h + 1],
                                           in1=score[:, c0:c0 + w], op0=ALU.mult, op1=ALU.add))
                    self.I("dve", [b_score, self.b_cst], [b_score],
                           lambda: nc.vector.tensor_tensor(out=score[:, js], in0=score[:, js], in1=self.cneg_f[:],
                                                           op=ALU.add))
                    if j >= 2:
                        src = score
                        for r in range(NR):
                            m, bm = m8[r % 2], b_m8[r % 2]
                            self.I("dve", [b_score, b_work], [bm], lambda: nc.vector.max(out=m[:], in_=src[:, 0:nk]))
                            if r < NR - 1:
                                self.I("dve", [b_score, b_work, bm], [b_work],
                                       lambda: nc.vector.match_replace(out=work[:, 0:nk], in_to_replace=m[:],
                                                                       in_values=src[:, 0:nk], imm_value=-3.0e38))
                                src = work
                        thr, bthr = m[:, 7:8], bm
                    else:
                        thr, bthr = thr0[:], b_thr0
                    ng, bng = negm[j % 2], b_negm[j % 2]
                    self.I("dve", [b_score, bthr], [bng],
                           lambda: nc.vector.tensor_scalar(out=ng[:, 0:nk], in0=score[:, 0:nk], scalar1=thr,
                                                           scalar2=-30000.0, op0=ALU.is_lt, op1=ALU.mult))
                    for hf in range(2):
                        po, bpo = ps[4 + hf], b_ps[4 + hf]
                        pso, bpso = ps[6 + hf], b_ps[6 + hf]
                        hs = slice(3 * hf, 3 * hf + 3)
                        for i in range(j + 1):
                            cnt += 1
                            pq, bpq = ps[2 + cnt % 2], b_ps[2 + cnt % 2]
                            pt, bpt = PT[cnt % 3], b_PT[cnt % 3]
                            isl = slice(i * 128, (i + 1) * 128)
                            far = (j - i) >= 2
                            kr = 65 if far else 64
                            self.I("pe", [b_Kp, b_Qp], [bpq],
                                   lambda: nc.tensor.matmul(pq[:, 0:384], lhsT=Kp[0:kr, isl], rhs=Qp[0:kr, hs, js],
                                                            start=True, stop=False))
                            if not far:
                                self.I("pe", [self.b_cst], [bpq],
                                       lambda: nc.tensor.matmul(pq[:, 0:384], lhsT=self.ident_bf[:],
                                                                rhs=self.biasT[:, j - i, hs, :], start=False, stop=False))
                            self.I("pe", [bng, self.b_cst], [bpq],
                                   lambda: nc.tensor.matmul(pq[:, 0:384], lhsT=ng[:, isl], rhs=self.I3[:],
                                                            start=False, stop=True))
                            self.I("act", [bpq], [bpt],
                                   lambda: nc.scalar.activation(out=pt[:], in_=pq[:, 0:384], func=AF.Exp))
                            self.I("pe", [b_Vd, bpt], [bpo],
                                   lambda: nc.tensor.matmul(po[0:64, 0:384], lhsT=Vd[:, i, :], rhs=pt[:],
                                                            start=(i == 0), stop=(i == j)))
                            self.I("pe", [self.b_ones, bpt], [bpso],
                                   lambda: nc.tensor.matmul(pso[0:64, 0:384], lhsT=self.ones_bf[:, 0:64], rhs=pt[:],
                                                            start=(i == 0), stop=(i == j)))
                        rc, brc = rec[hf], b_rec[hf]
                        self.I("dve", [bpso], [brc], lambda: nc.vector.reciprocal(out=rc[:], in_=pso[0:64, 0:384]))
                        self.I("dve", [bpo, brc], [b_bst],
                               lambda: nc.vector.tensor_tensor(out=bst[:, j, hs, :].rearrange("p a b -> p (a b)"), in0=rc[:], in1=po[0:64, 0:384],
                                                               op=ALU.mult))
                for h in range(6):
                    r0 = (h % 2) * 64
                    self.dma("pool", self.d_br[2 + h // 2, r0:r0 + 64, t0:t0 + S].rearrange("p (n t) -> p n t", t=128), bst[:, :, h, :], b_bst,
                             reads=[b_bst], writes=[self.b_dbr])
            self.phase_end()

    def phase_a(self, l):
        nc = self.nc
        pes = contextlib.ExitStack()
        self.phase_begin()
        with pes:
            xb = self.sb(pes, "xb", [128, 8, TB], F32)
            hT = self.sb(pes, "hT", [128, 8, TB], BF16)
            actT = self.sb(pes, "actT", [128, NFC, TB], BF16)
            wgu = [self.sb(pes, f"wgu{i}", [128, 2, 8, 128], BF16) for i in range(2)]
            wd = [self.sb(pes, f"wd{i}", [128, NFC, 128], BF16) for i in range(2)]
            wfm = [self.sb(pes, f"wfm{i}", [128, 8, 128], BF16) for i in range(3)]
            wtm = self.sb(pes, "wtm", [128, 8, NTM], BF16)
            sq = [self.sb(pes, f"sq{i}", [128, 512], BF16) for i in range(2)]
            rs = [self.sb(pes, f"rs{i}", [128, 512], F32) for i in range(2)]
            sg = [self.sb(pes, f"sg{i}", [128, 512], F32) for i in range(2)]
            stg = [self.sb(pes, f"stg{i}", [128, TB], BF16) for i in range(4)]
            stv = self.sb(pes, "stv", [128, 8, 384], BF16)
            stw = self.sb(pes, "stw", [128, 8, 16], F32)
            B = self.buf
            b_xb = [B("xb") for _ in range(8)]
            b_hT = [B("hT") for _ in range(8)]
            b_actT = [B("actT") for _ in range(NFC)]
            b_wgu = [B("wgu") for _ in range(2)]
            b_wd = [B("wd") for _ in range(2)]
            b_wfm = [B("wfm") for _ in range(3)]
            b_wtm = B("wtm")
            b_sq = [B("sq") for _ in range(2)]
            b_rs = [B("rs") for _ in range(2)]
            b_sg = [B("sg") for _ in range(2)]
            b_stg = [B("stg") for _ in range(4)]
            b_stv = B("stv")
            b_stw = B("stw")
            ps, b_ps = self.ps, self.b_ps
            self.I("dve", [], [b_stw], lambda: nc.vector.memset(stw[:], 0.0))
            NFCL = self.debug.get('nfc_lim', NFC)

            def nw(g, ll, kc):
                c = (g * L + ll) * 8 + kc
                return self.normw[:, c:c + 1]

            def norm(g, ll, final=False):
                for tg in range(2):
                    cs = slice(tg * 512, (tg + 1) * 512)
                    ssp, b_ss = ps[6 + tg], b_ps[6 + tg]
                    for kc in range(8):
                        i = kc % 2
                        self.I("act", [b_xb[kc]], [b_sq[i]],
                               lambda: nc.scalar.activation(out=sq[i][:], in_=xb[:, kc, cs], func=AF.Square))
                        self.I("pe", [b_sq[i], self.b_ones], [b_ss],
                               lambda: nc.tensor.matmul(ssp[:], lhsT=self.ones_bf[:], rhs=sq[i][:],
                                                        start=(kc == 0), stop=(kc == 7)))
                    self.I("act", [b_ss, self.b_ones], [b_rs[tg]],
                           lambda: nc.scalar.activation(out=rs[tg][:], in_=ssp[:], func=AF.Sqrt,
                                                        bias=self.eps_t[:], scale=1.0 / D))
                    self.I("dve", [b_rs[tg]], [b_rs[tg]],
                           lambda: nc.vector.reciprocal(out=rs[tg][:], in_=rs[tg][:]))
                    for kc in range(8):
                        if final:
                            self.I("dve", [b_xb[kc], b_rs[tg], self.b_normw], [b_xb[kc]],
                                   lambda: nc.vector.scalar_tensor_tensor(
                                       out=xb[:, kc, cs], in0=xb[:, kc, cs],
                                       scalar=self.normw[:, 3 * L * 8 + kc:3 * L * 8 + kc + 1],
                                       in1=rs[tg][:], op0=ALU.mult, op1=ALU.mult))
                        else:
                            self.I("dve", [b_xb[kc], b_rs[tg], self.b_normw], [b_hT[kc]],
                                   lambda: nc.vector.scalar_tensor_tensor(
                                       out=hT[:, kc, cs], in0=xb[:, kc, cs], scalar=nw(g, ll, kc),
                                       in1=rs[tg][:], op0=ALU.mult, op1=ALU.mult))

            def load_wgu(ll, j, fc):
                r0 = ((ll * 2 + j) * NFC + fc) * 128
                i = fc % 2
                self.dma("sp", wgu[i][:], self.wgu_b[r0:r0 + 128, :].rearrange("p (a k f) -> p a k f", a=2, k=8),
                         b_wgu[i], reads=[self.b_wcast], writes=[b_wgu[i]])

            def load_wd(ll, j, dc):
                r0 = ((ll * 2 + j) * 8 + dc) * 128
                i = dc % 2
                self.dma("sp", wd[i][:], self.wd_b[r0:r0 + 128, :].rearrange("p (c d) -> p c d", c=NFC),
                         b_wd[i], reads=[self.b_wcast], writes=[b_wd[i]])

            def ffn(ll, j, g):
                load_wgu(ll, j, 0)
                norm(g, ll)
                for fc in range(NFCL):
                    if fc + 1 < NFCL:
                        load_wgu(ll, j, fc + 1)
                    else:
                        load_wd(ll, j, 0)
                    i = fc % 2
                    for tg in range(2):
                        cs = slice(tg * 512, (tg + 1) * 512)
                        pg, bg = ps[tg * 2], b_ps[tg * 2]
                        pu, bu = ps[tg * 2 + 1], b_ps[tg * 2 + 1]
                        for kc in range(8):
                            self.I("pe", [b_wgu[i], b_hT[kc]], [bg],
                                   lambda: nc.tensor.matmul(pg[:], lhsT=wgu[i][:, 0, kc, :], rhs=hT[:, kc, cs],
                                                            start=(kc == 0), stop=(kc == 7)))
                        for kc in range(8):
                            self.I("pe", [b_wgu[i], b_hT[kc]], [bu],
                                   lambda: nc.tensor.matmul(pu[:], lhsT=wgu[i][:, 1, kc, :], rhs=hT[:, kc, cs],
                                                            start=(kc == 0), stop=(kc == 7)))
                        self.I("act", [bg], [b_sg[tg]],
                               lambda: nc.scalar.activation(out=sg[tg][:], in_=pg[:], func=AF.Silu))
                        self.I("dve", [b_sg[tg], bu], [b_actT[fc]],
                               lambda: nc.vector.tensor_tensor(out=actT[:, fc, cs], in0=sg[tg][:], in1=pu[:],
                                                               op=ALU.mult))
                for dc in range(8):
                    if dc + 1 < 8:
                        load_wd(ll, j, dc + 1)
                    i = dc % 2
                    for tg in range(2):
                        cs = slice(tg * 512, (tg + 1) * 512)
                        pd, bd = ps[4 + tg], b_ps[4 + tg]
                        for fc in range(NFCL):
                            self.I("pe", [b_wd[i], b_actT[fc]], [bd],
                                   lambda: nc.tensor.matmul(pd[:], lhsT=wd[i][:, fc, :], rhs=actT[:, fc, cs],
                                                            start=(fc == 0), stop=(fc == NFCL - 1)))
                        self.I("dve", [bd, b_xb[dc]], [b_xb[dc]],
                               lambda: nc.vector.scalar_tensor_tensor(
                                   out=xb[:, dc, cs], in0=pd[:], scalar=0.5, in1=xb[:, dc, cs],
                                   op0=ALU.mult, op1=ALU.add))

            def load_wfm(ll, c, slot):
                r0 = (ll * NFM + c) * 128
                self.dma("sp", wfm[slot][:], self.winfm_b[r0:r0 + 128, :].rearrange("p (k m) -> p k m", k=8),
                         b_wfm[slot], reads=[self.b_wcast], writes=[b_wfm[slot]])

            def proj(ll, tb):
                t0 = tb * TB
                self.dma("sp", wtm[:], self.wintm_b[ll * 128:(ll + 1) * 128, :].rearrange("p (k m) -> p k m", k=8),
                         b_wtm, reads=[self.b_wcast], writes=[b_wtm])
                load_wfm(ll, 0, 0)
                load_wfm(ll, 1, 1)
                norm(1, ll)
                for c in range(NFM):
                    if c + 2 < NFM:
                        load_wfm(ll, c + 2, (c + 2) % 3)
                    slot = c % 3
                    si = c % 4
                    is_gate = c >= 15
                    for tg in range(2):
                        cs = slice(tg * 512, (tg + 1) * 512)
                        pp, bp = ps[(c * 2 + tg) % 4], b_ps[(c * 2 + tg) % 4]
                        for kc in range(8):
                            self.I("pe", [b_wfm[slot], b_hT[kc]], [bp],
                                   lambda: nc.tensor.matmul(pp[:], lhsT=wfm[slot][:, kc, :], rhs=hT[:, kc, cs],
                                                            start=(kc == 0), stop=(kc == 7)))
                        if is_gate:
                            self.I("act", [bp], [b_stg[si]],
                                   lambda: nc.scalar.activation(out=stg[si][:, cs], in_=pp[:], func=AF.Sigmoid))
                        elif c in (2, 3, 4):
                            self.I("act", [bp], [b_stg[si]],
                                   lambda: nc.scalar.mul(out=stg[si][:, cs], in_=pp[:], mul=0.125))
                        elif (c + tg) % 2 == 0:
                            self.I("act", [bp], [b_stg[si]],
                                   lambda: nc.scalar.copy(out=stg[si][:, cs], in_=pp[:]))
                        else:
                            self.I("dve", [bp], [b_stg[si]],
                                   lambda: nc.vector.tensor_copy(out=stg[si][:, cs], in_=pp[:]))
                    self.dma("pool", self.d_fm[c, :, t0:t0 + TB], stg[si][:], b_stg[si],
                             reads=[b_stg[si]], writes=[self.b_dfm])
                for tt in range(0 if self.debug.get('no_tm') else 8):
                    pp, bp = ps[4 + tt % 2], b_ps[4 + tt % 2]
                    for kc in range(8):
                        self.I("pe", [b_wtm, b_hT[kc]], [bp],
                               lambda: nc.tensor.matmul(pp[:, 0:NTM], lhsT=hT[:, kc, tt * 128:(tt + 1) * 128],
                                                        rhs=wtm[:, kc, :], start=(kc == 0), stop=(kc == 7)))
                    self.I("act", [bp], [b_stv],
                           lambda: nc.scalar.copy(out=stv[:, tt, :], in_=pp[:, 0:384]))
                    self.I("dve", [bp], [b_stw],
                           lambda: nc.vector.tensor_copy(out=stw[:, tt, 0:10], in_=pp[:, 384:394]))
                if self.debug.get('no_tm'):
                    return
                self.dma("pool", self.d_vc[t0:t0 + TB, :].rearrange("(n p) c -> p n c", p=128), stv[:], b_stv,
                         reads=[b_stv], writes=[self.b_dtm])
                if self.debug.get('no_wf'):
                    return
                self.dma("pool", self.d_wf[t0:t0 + TB, :].rearrange("(n p) c -> p n c", p=128), stw[:], b_stw,
                         reads=[b_stw], writes=[self.b_dtm])

            def load_w8(src, ll, oc, slot):
                r0 = (ll * 8 + oc) * 128
                self.dma("sp", wfm[slot][:], src[r0:r0 + 128, :].rearrange("p (k m) -> p k m", k=8),
                         b_wfm[slot], reads=[self.b_wcast], writes=[b_wfm[slot]])

            KCS = [(0, 2), (2, 5), (5, 8)]

            def merge(ll, tb):
                t0 = tb * TB
                self.dma("sp", hT[:], self.d_br[:, :, t0:t0 + TB].rearrange("k p t -> p k t"), b_hT[0],
                         reads=[self.b_dbr], writes=b_hT)
                load_w8(self.wbr_b, ll, 0, 0)
                for oc in range(8):
                    if oc + 1 < 8:
                        load_w8(self.wbr_b, ll, oc + 1, (oc + 1) % 3)
                    else:
                        load_w8(self.wout_b, ll, 0, (oc + 1) % 3)
                    slot = oc % 3
                    for br in range(3):
                        self.dma("sp", stg[br][:], self.d_fm[15 + br * 8 + oc, :, t0:t0 + TB], b_stg[br],
                                 reads=[self.b_dfm], writes=[b_stg[br]])
                    for tg in range(2):
                        cs = slice(tg * 512, (tg + 1) * 512)
                        for br in range(3):
                            pp, bp = ps[tg * 3 + br], b_ps[tg * 3 + br]
                            k0, k1 = KCS[br]
                            for kc in range(k0, k1):
                                self.I("pe", [b_wfm[slot], b_hT[kc]], [bp],
                                       lambda: nc.tensor.matmul(pp[:], lhsT=wfm[slot][:, kc, :], rhs=hT[:, kc, cs],
                                                                start=(kc == k0), stop=(kc == k1 - 1)))
                        p0, p1, p2 = ps[tg * 3], ps[tg * 3 + 1], ps[tg * 3 + 2]
                        q0, q1, q2 = b_ps[tg * 3], b_ps[tg * 3 + 1], b_ps[tg * 3 + 2]
                        self.I("dve", [q0, b_stg[0]], [b_sg[0]],
                               lambda: nc.vector.tensor_tensor(out=sg[0][:], in0=stg[0][:, cs], in1=p0[:], op=ALU.mult))
                        self.I("dve", [q1, b_stg[1]], [b_sg[1]],
                               lambda: nc.vector.tensor_tensor(out=sg[1][:], in0=stg[1][:, cs], in1=p1[:], op=ALU.mult))
                        self.I("dve", [b_sg[0], b_sg[1]], [b_sg[0]],
                               lambda: nc.vector.tensor_tensor(out=sg[0][:], in0=sg[0][:], in1=sg[1][:], op=ALU.add))
                        self.I("dve", [q2, b_stg[2]], [b_sg[1]],
                               lambda: nc.vector.tensor_tensor(out=sg[1][:], in0=stg[2][:, cs], in1=p2[:], op=ALU.mult))
                        self.I("dve", [b_sg[0], b_sg[1]], [b_actT[oc]],
                               lambda: nc.vector.tensor_tensor(out=actT[:, oc, cs], in0=sg[0][:], in1=sg[1][:],
                                                               op=ALU.add))
                for oc in range(8):
                    if oc + 1 < 8:
                        load_w8(self.wout_b, ll, oc + 1, (oc + 9) % 3)
                    slot = (oc + 8) % 3
                    for tg in range(2):
                        cs = slice(tg * 512, (tg + 1) * 512)
                        pp, bp = ps[6 + tg], b_ps[6 + tg]
                        for kc in range(8):
                            self.I("pe", [b_wfm[slot], b_actT[kc]], [bp],
                                   lambda: nc.tensor.matmul(pp[:], lhsT=wfm[slot][:, kc, :], rhs=actT[:, kc, cs],
                                                            start=(kc == 0), stop=(kc == 7)))
                        self.I("dve", [bp, b_xb[oc]], [b_xb[oc]],
                               lambda: nc.vector.tensor_tensor(out=xb[:, oc, cs], in0=xb[:, oc, cs], in1=pp[:],
                                                               op=ALU.add))

            for tb in range(self.debug.get('ntb', NTB)):
                t0 = tb * TB
                src = self.xT_in if l == 0 else self.xw
                self.dma("sp", xb[:], src.rearrange("(k p) t -> p k t", p=128)[:, :, t0:t0 + TB], b_xb[0],
                         reads=([] if l == 0 else [self.b_xw]), writes=b_xb)
                if l > 0:
                    merge(l - 1, tb)
                    ffn(l - 1, 1, 2)
                if l < self.nl:
                    ffn(l, 0, 0)
                    if not self.debug.get("no_proj"):
                        proj(l, tb)
                else:
                    norm(0, 0, final=True)
                dst = self.xw if l < self.nl else self.out
                if self.debug.get("x_to_out"):
                    dst = self.out
                self.dma("pool", dst.rearrange("(k p) t -> p k t", p=128)[:, :, t0:t0 + TB], xb[:], b_xb[0],
                         reads=b_xb, writes=[self.b_xw])
            self.phase_end()

    def finish(self):
        self.barrier()


def prep_weights(inp):
    f = np.float32
    out = {}
    nw = np.zeros((128, 3 * L * 8 + 8), f)
    for g, k in enumerate(["ffn1_norm", "mix_norm", "ffn2_norm"]):
        a = np.asarray(inp[k], f)
        nw[:, g * L * 8:(g + 1) * L * 8] = a.reshape(L, 8, 128).transpose(2, 0, 1).reshape(128, L * 8)
    nw[:, 3 * L * 8:] = np.asarray(inp["final_norm"], f).reshape(8, 128).T
    out["normw"] = nw
    wgu = np.empty((L, 2, NFC, 128, 2, 8, 128), f)
    wd = np.empty((L, 2, 8, 128, NFC, 128), f)
    for j, pre in enumerate(["ffn1", "ffn2"]):
        g = np.asarray(inp[pre + "_w_gate"], f).reshape(L, 8, 128, NFC, 128)
        u = np.asarray(inp[pre + "_w_up"], f).reshape(L, 8, 128, NFC, 128)
        wgu[:, j, :, :, 0] = g.transpose(0, 3, 2, 1, 4)
        wgu[:, j, :, :, 1] = u.transpose(0, 3, 2, 1, 4)
        dn = np.asarray(inp[pre + "_w_down"], f).reshape(L, NFC, 128, 8, 128)
        wd[:, j] = dn.transpose(0, 3, 2, 1, 4)
    out["wgu"] = wgu.reshape(L * 2 * NFC * 128, 2 * 8 * 128)
    out["wd"] = wd.reshape(L * 2 * 8 * 128, NFC * 128)
    w_in = np.asarray(inp["w_in"], f)
    w_pad = np.concatenate([w_in, np.zeros((L, D, 1), f)], axis=2)
    chunks = np.array(fm_chunk_cols())
    fm = w_pad[:, :, chunks]
    fm = fm.reshape(L, 8, 128, NFM, 128).transpose(0, 3, 2, 1, 4)
    out["winfm"] = np.ascontiguousarray(fm).reshape(L * NFM * 128, 8 * 128)
    tmc = list(range(O_VC, O_VC + 384)) + list(range(O_WI, O_WI + 4)) + list(range(O_FC, O_FC + 6))
    tm = w_in[:, :, tmc].reshape(L, 8, 128, NTM).transpose(0, 2, 1, 3)
    out["wintm"] = np.ascontiguousarray(tm).reshape(L * 128, 8 * NTM)
    ii = np.arange(128)
    cst = np.zeros((128, 6 * 128), f)
    cst[:, 0:128] = np.eye(128, dtype=f)
    cst[:, 128:256] = np.where(ii[:, None] > ii[None, :], -30000.0, 0.0)
    cst[:, 256:384] = np.where(ii[None, :] > ii[:, None], -1e30, 0.0)
    cst[:, 384:512] = (ii[:, None] <= ii[None, :]).astype(f)
    cst[:, 512:640] = 1.0
    out["consts"] = cst
    out["foxbf"] = np.ascontiguousarray(np.broadcast_to(np.asarray(inp["fox_b_f"], f).reshape(1, L * 6), (128, L * 6)))
    out["kvnorm"] = np.ascontiguousarray(np.asarray(inp["dsa_kv_norm"], f).T)
    out["wuk"] = np.ascontiguousarray(np.asarray(inp["dsa_w_uk"], f).reshape(L * 128, 64))
    out["wuv"] = np.ascontiguousarray(np.asarray(inp["dsa_w_uv"], f).reshape(L * 128, 64))
    rb = np.asarray(inp["rel_bias"], f)
    out["rb31"] = np.ascontiguousarray(np.broadcast_to(rb[31][:, None], (6, S))).reshape(1, 6 * S)
    dist = ii[None, None, :] - ii[:, None, None] + 128 * np.arange(2)[None, :, None]
    dd = np.maximum(dist, 1).astype(np.float32)
    logb = 16 + (np.log(dd / np.float32(16)) / np.float32(math.log(128 / 16)) * np.float32(16)).astype(np.int32)
    bucket = np.where(np.maximum(dist, 0) < 16, np.maximum(dist, 0), np.minimum(logb, 31))
    rbT = rb[bucket]
    rbT = np.where((dist >= 0)[..., None], rbT, 0.0).astype(f)
    out["rbT"] = np.ascontiguousarray(rbT.transpose(0, 1, 3, 2)).reshape(128, 2 * 6 * 128)
    def st(a):
        a = np.asarray(a, f)
        a = a.reshape((L, 8, 128) + a.shape[3:])
        return np.ascontiguousarray(np.moveaxis(a, 2, 0))
    lr = st(inp["ssm_lambda_re"]); li = st(inp["ssm_lambda_im"])
    ldt = st(np.broadcast_to(np.asarray(inp["ssm_log_dt"], f)[:, :, None], (L, 16, 64)))
    out["s5p"] = np.ascontiguousarray(np.stack([lr, li, ldt], axis=-1)).reshape(128, L * 8 * 3)
    bre = st(inp["ssm_b_re"]); bim = st(inp["ssm_b_im"])
    out["s5b"] = np.ascontiguousarray(np.stack([bre, bim], axis=3)).reshape(128, L * 8 * 2 * 16)
    cre = st(np.asarray(inp["ssm_c_re"], f).transpose(0, 1, 3, 2)); cim = st(np.asarray(inp["ssm_c_im"], f).transpose(0, 1, 3, 2))
    out["s5c"] = np.ascontiguousarray(np.stack([cre, cim], axis=3)).reshape(128, L * 8 * 2 * 16)
    out["s5d"] = np.ascontiguousarray(np.asarray(inp["ssm_d"], f).reshape(L, 2, 128).transpose(2, 0, 1)).reshape(128, L * 2)
    wg = np.asarray(inp["ssm_w_glu"], f).reshape(L, 2, 128, 512).transpose(0, 2, 1, 3)
    out["wglu"] = np.ascontiguousarray(wg).reshape(L * 128, 2 * 512)
    wbr = np.concatenate([np.asarray(inp["w_branch_ssm"], f), np.asarray(inp["w_branch_dsa"], f),
                          np.asarray(inp["w_branch_fox"], f)], axis=1)
    out["wbr"] = np.ascontiguousarray(wbr.reshape(L, 8, 128, 8, 128).transpose(0, 3, 2, 1, 4)).reshape(L * 8 * 128, 8 * 128)
    wo = np.asarray(inp["w_out"], f)
    out["wout"] = np.ascontiguousarray(wo.reshape(L, 8, 128, 8, 128).transpose(0, 3, 2, 1, 4)).reshape(L * 8 * 128, 8 * 128)
    return out


def run(inputs, n_layers=L, debug=None, cores=NCORES):
    b = Builder(n_layers=n_layers, debug=debug)
    nc = b.build()
    w = prep_weights(inputs)
    x = np.asarray(inputs["x"], np.float32)
    in_maps = []
    for c in range(cores):
        xs = x[c * NSEQ:(c + 1) * NSEQ].reshape(TOK, D)
        m = {"xT": np.ascontiguousarray(xs.T)}
        m.update(w)
        if n_layers < L:
            m["wgu"] = m["wgu"][:n_layers * 2 * NFC * 128]
            m["wd"] = m["wd"][:n_layers * 2 * 8 * 128]
            m["winfm"] = m["winfm"][:n_layers * NFM * 128]
            m["wintm"] = m["wintm"][:n_layers * 128]
            m["wbr"] = m["wbr"][:n_layers * 8 * 128]
            m["wout"] = m["wout"][:n_layers * 8 * 128]
        in_maps.append(m)
    res = run_bass_kernel_spmd(nc, in_maps, core_ids=list(range(cores)))
    return res


def kernel(**inputs):
    res = run(inputs)
    outs = []
    for c in range(NCORES):
        o = res.results[c]["out"]
        outs.append(np.ascontiguousarray(o.T).reshape(NSEQ, S, D))
    return np.concatenate(outs, axis=0).astype(np.float32)
```

```python
import contextlib
import math
import numpy as np
import concourse.bass as bass
import concourse.mybir as mybir
from concourse.bass_utils import run_bass_kernel_spmd

F32 = mybir.dt.float32
BF16 = mybir.dt.bfloat16
ALU = mybir.AluOpType
AF = mybir.ActivationFunctionType
AX = mybir.AxisListType

D = 1024
FH = 2816
NFC = FH // 128
S = 2048
NSEQ = 4
TOK = NSEQ * S
L = 4
TB = 1024
NTB = TOK // TB
EPS = 1e-6
NCORES = 8

IN_SPLITS = (256, 384, 128, 256, 64, 4, 384, 384, 384, 6, 1024, 1024, 1024)
OFF = np.concatenate([[0], np.cumsum(IN_SPLITS)]).tolist()
(O_U, O_QB, O_CKV, O_QI, O_KI, O_WI, O_QC, O_KC, O_VC, O_FC, O_GA, O_GB, O_GC, O_END) = OFF
NFM = 39
NTM = 394


def fm_chunk_cols():
    chunks = []
    for c in range(8):
        chunks.append(list(range(c * 128, (c + 1) * 128)))
    chunks.append(list(range(O_KI, O_KI + 64)) + [-1] * 64)
    for c in range(3):
        chunks.append(list(range(O_QC + c * 128, O_QC + (c + 1) * 128)))
    for c in range(3):
        chunks.append(list(range(O_KC + c * 128, O_KC + (c + 1) * 128)))
    for c in range(24):
        chunks.append(list(range(O_GA + c * 128, O_GA + (c + 1) * 128)))
    assert len(chunks) == NFM
    return chunks


def FL(t):
    return t[:].rearrange("p a b -> p (a b)")


class Buf:
    __slots__ = ("name", "w", "r", "sems", "psum")

    def __init__(self, name, psum=False):
        self.name = name
        self.psum = psum
        self.w = None
        self.r = {}
        self.sems = {}


class Builder:
    def __init__(self, n_layers=L, debug=None):
        self.nl = n_layers
        self.debug = debug or {}
        self.nc = bass.Bass("TRN2", target_bir_lowering=False)
        nc = self.nc
        self.es = contextlib.ExitStack()
        self.engs = {"pe": nc.tensor, "act": nc.scalar, "dve": nc.vector, "pool": nc.gpsimd, "sp": nc.sync}
        self.esem = {}
        self.ecnt = {}
        self.seen = {}
        for e in self.engs:
            self.esem[e] = self.es.enter_context(nc.semaphore("E" + e))
            self.ecnt[e] = 0
            self.seen[e] = {}
        self.dma_sems = []
        self.free_sems = {"hw": [], "sw": []}
        self.phase_sems = []
        self.in_phase = False
        self.nbuf = 0
        self.out_toks = []

    def buf(self, name, psum=False):
        self.nbuf += 1
        return Buf(f"{name}_{self.nbuf}", psum)

    def _wait(self, eng, reads, writes):
        need = {}

        def add(tok):
            key, sem, val = tok
            if key == "pe" and eng == "pe":
                return
            k = id(sem)
            if k not in need or need[k][1] < val:
                need[k] = (sem, val)

        for b in reads:
            if b.w is not None:
                add(b.w)
            if b.psum:
                for kk, t in b.r.items():
                    if kk != eng:
                        add(t)
        for b in writes:
            if b.w is not None:
                add(b.w)
            for t in b.r.values():
                add(t)
        seen = self.seen[eng]
        h = self.engs[eng]
        for k, (sem, val) in need.items():
            if seen.get(k, 0) >= val:
                continue
            h.wait_ge(sem, val)
            seen[k] = val

    def I(self, eng, reads, writes, fn):
        self._wait(eng, reads, writes)
        ins = fn()
        self.ecnt[eng] += 1
        ins.then_inc(self.esem[eng], 1)
        tok = (eng, self.esem[eng], self.ecnt[eng])
        for b in reads:
            b.r[eng] = tok
        for b in writes:
            b.w = tok
            b.r = {}
        return tok

    def dma(self, q, out, in_, owner, reads=(), writes=(), **kw):
        self._wait(q, reads, writes)
        qk = "sw" if q == "pool" else "hw"
        if qk not in owner.sems:
            if self.free_sems[qk]:
                owner.sems[qk] = self.free_sems[qk].pop()
            else:
                owner.sems[qk] = [self.es.enter_context(self.nc.semaphore("D" + qk + owner.name)), 0]
                self.dma_sems.append(owner.sems[qk])
            if self.in_phase:
                self.phase_sems.append((qk, owner.sems[qk]))
        sc = owner.sems[qk]
        ins = self.engs[q].dma_start(out=out, in_=in_, **kw)
        sc[1] += 16
        ins.then_inc(sc[0], 16)
        tok = ("dma:" + qk + owner.name, sc[0], sc[1])
        for b in reads:
            b.r[tok[0]] = tok
        for b in writes:
            b.w = tok
            b.r = {}
        return tok

    def phase_begin(self):
        self.in_phase = True
        self.phase_sems = []

    def phase_end(self):
        self.barrier()
        for qk, sc in self.phase_sems:
            self.free_sems[qk].append(sc)
        self.phase_sems = []
        self.in_phase = False

    def barrier(self):
        for e, h in self.engs.items():
            seen = self.seen[e]
            for e2 in self.engs:
                if e2 == e or self.ecnt[e2] == 0:
                    continue
                k = id(self.esem[e2])
                if seen.get(k, 0) < self.ecnt[e2]:
                    h.wait_ge(self.esem[e2], self.ecnt[e2])
                    seen[k] = self.ecnt[e2]
            for sc in self.dma_sems:
                k = id(sc[0])
                if seen.get(k, 0) < sc[1]:
                    h.wait_ge(sc[0], sc[1])
                    seen[k] = sc[1]

    def sb(self, es, name, shape, dt):
        self.nbuf += 1
        t = es.enter_context(self.nc.sbuf_tensor(f"s_{name}_{self.nbuf}", shape, dt))
        return t

    def dram(self, name, shape, dt, kind="Internal"):
        return self.nc.dram_tensor(name, shape, dt, kind=kind).ap()

    def build(self):
        nc = self.nc
        nl = self.nl
        es = self.es
        self.xT_in = self.dram("xT", [D, TOK], F32, "ExternalInput")
        self.normw_in = self.dram("normw", [128, 3 * L * 8 + 8], F32, "ExternalInput")
        self.wgu_in = self.dram("wgu", [nl * 2 * NFC * 128, 2 * 8 * 128], F32, "ExternalInput")
        self.wd_in = self.dram("wd", [nl * 2 * 8 * 128, NFC * 128], F32, "ExternalInput")
        self.winfm_in = self.dram("winfm", [nl * NFM * 128, 8 * 128], F32, "ExternalInput")
        self.wintm_in = self.dram("wintm", [nl * 128, 8 * NTM], F32, "ExternalInput")
        self.wbr_in = self.dram("wbr", [nl * 8 * 128, 8 * 128], F32, "ExternalInput")
        self.wout_in = self.dram("wout", [nl * 8 * 128, 8 * 128], F32, "ExternalInput")
        self.consts_in = self.dram("consts", [128, 6 * 128], F32, "ExternalInput")
        self.foxbf_in = self.dram("foxbf", [128, L * 6], F32, "ExternalInput")
        self.kvnorm_in = self.dram("kvnorm", [128, L], F32, "ExternalInput")
        self.wuk_in = self.dram("wuk", [L * 128, 64], F32, "ExternalInput")
        self.wuv_in = self.dram("wuv", [L * 128, 64], F32, "ExternalInput")
        self.rb31_in = self.dram("rb31", [1, 6 * S], F32, "ExternalInput")
        self.rbT_in = self.dram("rbT", [128, 2 * 6 * 128], F32, "ExternalInput")
        self.s5p_in = self.dram("s5p", [128, L * 8 * 3], F32, "ExternalInput")
        self.s5b_in = self.dram("s5b", [128, L * 8 * 2 * 16], F32, "ExternalInput")
        self.s5c_in = self.dram("s5c", [128, L * 8 * 2 * 16], F32, "ExternalInput")
        self.s5d_in = self.dram("s5d", [128, L * 2], F32, "ExternalInput")
        self.wglu_in = self.dram("wglu", [L * 128, 2 * 512], F32, "ExternalInput")
        self.out = self.dram("out", [D, TOK], F32, "ExternalOutput")
        self.wbr_b = self.dram("wbr_b", [nl * 8 * 128, 8 * 128], BF16)
        self.wout_b = self.dram("wout_b", [nl * 8 * 128, 8 * 128], BF16)
        self.d_br = self.dram("d_br", [8, 128, TOK], BF16, "ExternalOutput" if self.debug.get("dump") else "Internal")
        self.b_dbr = self.buf("dbr")
        self.xw = self.dram("xw", [D, TOK], F32)
        self.wgu_b = self.dram("wgu_b", [nl * 2 * NFC * 128, 2 * 8 * 128], BF16)
        self.wd_b = self.dram("wd_b", [nl * 2 * 8 * 128, NFC * 128], BF16)
        self.winfm_b = self.dram("winfm_b", [nl * NFM * 128, 8 * 128], BF16)
        self.wintm_b = self.dram("wintm_b", [nl * 128, 8 * NTM], BF16)
        dk = "ExternalOutput" if self.debug.get("dump") else "Internal"
        self.d_fm = self.dram("d_fm", [NFM, 128, TOK], BF16, dk)
        self.d_vc = self.dram("d_vc", [TOK, 384], BF16, dk)
        self.d_wf = self.dram("d_wf", [TOK, 16], F32, dk)
        self.b_xw = self.buf("xw")
        self.b_wcast = self.buf("wcast")
        self.b_dfm = self.buf("dfm")
        self.b_dtm = self.buf("dtm")

        self.normw = self.sb(es, "normw", [128, 3 * L * 8 + 8], F32)
        self.b_normw = self.buf("normw")
        self.ones_bf = self.sb(es, "ones_bf", [128, 128], BF16)
        self.b_ones = self.buf("ones")
        self.eps_t = self.sb(es, "eps_t", [128, 1], F32)
        self.I("dve", [], [self.b_ones], lambda: nc.vector.memset(self.ones_bf[:], 1.0))
        self.I("dve", [], [self.b_ones], lambda: nc.vector.memset(self.eps_t[:], EPS))
        self.dma("sp", self.normw[:], self.normw_in[:, :], self.b_normw, writes=[self.b_normw])
        self.b_cst = self.buf("cst")
        cf = self.sb(es, "constf", [128, 6 * 128], F32)
        self.dma("sp", cf[:], self.consts_in[:, :], self.b_cst, writes=[self.b_cst])
        self.ident_f = cf[:, 0:128]
        self.cneg_f = cf[:, 256:384]
        self.tri_f = cf[:, 384:512]
        self.ones_f = cf[:, 512:640]
        self.ident_bf = self.sb(es, "ident_bf", [128, 128], BF16)
        self.causneg_bf = self.sb(es, "causneg_bf", [128, 128], BF16)
        self.I3 = self.sb(es, "I3", [128, 384], BF16)
        self.one_t = self.sb(es, "one_t", [128, 1], F32)
        self.hpi_t = self.sb(es, "hpi_t", [128, 1], F32)
        self.I("dve", [self.b_cst], [self.b_cst], lambda: nc.vector.tensor_copy(out=self.ident_bf[:], in_=cf[:, 0:128]))
        self.I("dve", [self.b_cst], [self.b_cst], lambda: nc.vector.tensor_copy(out=self.causneg_bf[:], in_=cf[:, 128:256]))
        for k3 in range(3):
            self.I("dve", [self.b_cst], [self.b_cst],
                   lambda: nc.vector.tensor_copy(out=self.I3[:, k3 * 128:(k3 + 1) * 128], in_=cf[:, 0:128]))
        self.I("dve", [], [self.b_cst], lambda: nc.vector.memset(self.one_t[:], 1.0))
        self.I("dve", [], [self.b_cst], lambda: nc.vector.memset(self.hpi_t[:], math.pi / 2))
        self.foxbf = self.sb(es, "foxbf", [128, L * 6], F32)
        self.kvnorm = self.sb(es, "kvnorm", [128, L], F32)
        self.s5d = self.sb(es, "s5d", [128, L * 2], F32)
        self.biasT = self.sb(es, "biasT", [128, 2, 6, 128], BF16)
        self.dma("sp", self.foxbf[:], self.foxbf_in[:, :], self.b_cst, writes=[self.b_cst])
        self.dma("sp", self.kvnorm[:], self.kvnorm_in[:, :], self.b_cst, writes=[self.b_cst])
        self.dma("sp", self.s5d[:], self.s5d_in[:, :], self.b_cst, writes=[self.b_cst])
        self.dma("pool", self.biasT[:].rearrange("p a b c -> p (a b c)"), self.rbT_in[:, :], self.b_cst, writes=[self.b_cst])
        self.ps = []
        self.b_ps = []
        for i in range(8):
            self.ps.append(es.enter_context(nc.psum_tensor(f"ps{i}", [128, 512], F32)))
            self.b_ps.append(self.buf(f"ps{i}", psum=True))

        self.cast_weights()

        self.zero_dbr()
        for l in range(nl + 1):
            if self.debug.get('only_cast'):
                break
            self.phase_a(l)
            if self.debug.get("stop_after_a") == l:
                break
            if l < nl:
                self.phase_b(l)
        self.finish()
        return nc

    def cast_weights(self):
        def cast(dst, src, rows_per):
            n = dst.shape[0]
            for r0 in range(0, n, rows_per):
                r1 = min(n, r0 + rows_per)
                self.dma("pool", dst[r0:r1, :], src[r0:r1, :], self.b_wcast, writes=[self.b_wcast])
        nl = self.nl
        cast(self.wgu_b[0:nl * 2 * NFC * 128, :], self.wgu_in[0:nl * 2 * NFC * 128, :], 128)
        cast(self.wd_b[0:nl * 2 * 8 * 128, :], self.wd_in[0:nl * 2 * 8 * 128, :], 128)
        cast(self.winfm_b[0:nl * NFM * 128, :], self.winfm_in[0:nl * NFM * 128, :], 256)
        cast(self.wintm_b[0:nl * 128, :], self.wintm_in[0:nl * 128, :], 128)
        cast(self.wbr_b, self.wbr_in, 256)
        cast(self.wout_b, self.wout_in, 256)

    def zero_dbr(self):
        nc = self.nc
        self.phase_begin()
        with contextlib.ExitStack() as zes:
            z = self.sb(zes, "zt", [128, 4096], BF16)
            bz = self.buf("zt")
            self.I("dve", [], [bz], lambda: nc.vector.memset(z[:], 0.0))
            for c in range(8):
                for hh in range(2):
                    self.dma("sp", self.d_br[c, :, hh * 4096:(hh + 1) * 4096], z[:], bz, reads=[bz], writes=[self.b_dbr])
            self.phase_end()

    def phase_b(self, l):
        if self.debug.get("no_b"):
            return
        which = self.debug.get("branches", "sdf")
        if "s" in which:
            self.s5_phase(l)
        if "d" in which:
            self.dsa_phase(l)
        if "f" in which:
            self.fox_phase(l)

    def nseq(self):
        return self.debug.get("nseq", NSEQ)

    def s5_phase(self, l):
        nc = self.nc
        self.phase_begin()
        ps, b_ps = self.ps, self.b_ps
        B = self.buf
        LC = 512
        NCH = self.debug.get("nch", S // LC)
        with contextlib.ExitStack() as pes:
            prm = self.sb(pes, "prm", [128, 8, 3], F32)
            Bt = self.sb(pes, "Bt", [128, 8, 2, 16], F32)
            Ct = self.sb(pes, "Ct", [128, 8, 2, 16], F32)
            wglu = self.sb(pes, "wglu", [128, 2, 512], BF16)
            sm = self.sb(pes, "sm", [128, 20, 8], F32)
            tmp16 = self.sb(pes, "tmp16", [128, 16], F32)
            Mre = self.sb(pes, "Mre", [128, 8, 128], F32)
            Mim = self.sb(pes, "Mim", [128, 8, 128], F32)
            BbdT = self.sb(pes, "BbdT", [128, 8, 2, 128], BF16)
            Cbd = self.sb(pes, "Cbd", [128, 8, 2, 128], BF16)
            tabc = self.sb(pes, "tabc", [128, 8, LC], F32)
            tabs = self.sb(pes, "tabs", [128, 8, LC], F32)
            rB = self.sb(pes, "rB", [128, 8, LC], F32)
            tmpa = self.sb(pes, "tmpa", [128, LC], F32)
            tmpb = self.sb(pes, "tmpb", [128, LC], F32)
            b_set = B("s5set")
            b_wg = B("wglu")
            V = lambda k: sm[:, k, :]
            (DT, R, TH, C, Sn, CC, SS, SC, NR, NI, DEN, SRE, SIM, T1, T2, NSK, NSL) = range(17)
            lr, li, ldt = prm[:, :, 0], prm[:, :, 1], prm[:, :, 2]
            self.dma("sp", prm[:].rearrange("p a b -> p (a b)"), self.s5p_in[:, l * 24:(l + 1) * 24], b_set, writes=[b_set])
            self.dma("sp", Bt[:].rearrange("p a b c -> p (a b c)"), self.s5b_in[:, l * 256:(l + 1) * 256], b_set, writes=[b_set])
            self.dma("sp", Ct[:].rearrange("p a b c -> p (a b c)"), self.s5c_in[:, l * 256:(l + 1) * 256], b_set, writes=[b_set])
            self.dma("pool", wglu[:].rearrange("p k m -> p (k m)"), self.wglu_in[l * 128:(l + 1) * 128, :], b_wg, writes=[b_wg])

            def dv(fn):
                self.I("dve", [b_set, self.b_cst], [b_set], fn)

            def ac(fn):
                self.I("act", [b_set, self.b_cst], [b_set], fn)

            def tt(o, a, b, op):
                dv(lambda: nc.vector.tensor_tensor(out=o, in0=a, in1=b, op=op))

            ac(lambda: nc.scalar.activation(out=V(DT), in_=ldt, func=AF.Exp))
            tt(V(T1), lr, V(DT), ALU.mult)
            ac(lambda: nc.scalar.activation(out=V(R), in_=V(T1), func=AF.Exp))
            tt(V(TH), li, V(DT), ALU.mult)
            ac(lambda: nc.scalar.activation(out=V(Sn), in_=V(TH), func=AF.Sin, scale=1.0 / 16))
            ac(lambda: nc.scalar.activation(out=V(C), in_=V(TH), func=AF.Sin, scale=1.0 / 16, bias=self.hpi_t[:]))
            for _ in range(4):
                tt(V(CC), V(C), V(C), ALU.mult)
                tt(V(SS), V(Sn), V(Sn), ALU.mult)
                tt(V(SC), V(Sn), V(C), ALU.mult)
                tt(V(C), V(CC), V(SS), ALU.subtract)
                dv(lambda: nc.vector.tensor_scalar(out=V(Sn), in0=V(SC), scalar1=2.0, scalar2=None, op0=ALU.mult))
            tt(V(T1), V(R), V(C), ALU.mult)
            dv(lambda: nc.vector.tensor_scalar(out=V(NR), in0=V(T1), scalar1=-1.0, scalar2=None, op0=ALU.add))
            tt(V(NI), V(R), V(Sn), ALU.mult)
            tt(V(DEN), lr, lr, ALU.mult)
            tt(V(T1), li, li, ALU.mult)
            tt(V(DEN), V(DEN), V(T1), ALU.add)
            dv(lambda: nc.vector.reciprocal(out=V(DEN), in_=V(DEN)))
            tt(V(T1), V(NR), lr, ALU.mult)
            tt(V(T2), V(NI), li, ALU.mult)
            tt(V(T1), V(T1), V(T2), ALU.add)
            tt(V(SRE), V(T1), V(DEN), ALU.mult)
            tt(V(T1), V(NI), lr, ALU.mult)
            tt(V(T2), V(NR), li, ALU.mult)
            tt(V(T1), V(T1), V(T2), ALU.subtract)
            tt(V(SIM), V(T1), V(DEN), ALU.mult)
            dv(lambda: nc.vector.memset(Mre[:].rearrange("p a b -> p (a b)"), 0.0))
            dv(lambda: nc.vector.memset(Mim[:].rearrange("p a b -> p (a b)"), 0.0))
            dv(lambda: nc.vector.memset(Cbd[:].rearrange("p a b c -> p (a b c)"), 0.0))
            for sc in range(8):
                for hp in range(2):
                    P = slice(hp * 64, hp * 64 + 64)
                    col = ((2 * sc + hp) % 8) * 16
                    cs = slice(col, col + 16)
                    sre_c, sim_c = sm[P, SRE, sc:sc + 1], sm[P, SIM, sc:sc + 1]
                    dv(lambda: nc.vector.tensor_scalar(out=tmp16[P, :], in0=Bt[P, sc, 1, :], scalar1=sim_c, scalar2=None,
                                                       op0=ALU.mult))
                    dv(lambda: nc.vector.scalar_tensor_tensor(out=Mre[P, sc, cs], in0=Bt[P, sc, 0, :], scalar=sre_c,
                                                              in1=tmp16[P, :], op0=ALU.mult, op1=ALU.subtract))
                    dv(lambda: nc.vector.tensor_scalar(out=tmp16[P, :], in0=Bt[P, sc, 1, :], scalar1=sre_c, scalar2=None,
                                                       op0=ALU.mult))
                    dv(lambda: nc.vector.scalar_tensor_tensor(out=Mim[P, sc, cs], in0=Bt[P, sc, 0, :], scalar=sim_c,
                                                              in1=tmp16[P, :], op0=ALU.mult, op1=ALU.add))
                    dv(lambda: nc.vector.tensor_copy(out=Cbd[P, sc, 0, cs], in_=Ct[P, sc, 0, :]))
                    dv(lambda: nc.vector.tensor_scalar(out=Cbd[P, sc, 1, cs], in0=Ct[P, sc, 1, :], scalar1=-1.0,
                                                       scalar2=None, op0=ALU.mult))
            for sc in range(8):
                for ri, M in enumerate((Mre, Mim)):
                    pp, bp = ps[(sc * 2 + ri) % 4], b_ps[(sc * 2 + ri) % 4]
                    self.I("pe", [b_set, self.b_cst], [bp],
                           lambda: nc.tensor.transpose(out=pp[:, 0:128], in_=M[:, sc, :], identity=self.ident_f))
                    self.I("act", [bp, b_set], [b_set],
                           lambda: nc.scalar.copy(out=BbdT[:, sc, ri, :], in_=pp[:, 0:128]))
            dv(lambda: nc.vector.tensor_copy(out=tabc[:, :, 0], in_=V(C)))
            dv(lambda: nc.vector.tensor_copy(out=tabs[:, :, 0], in_=V(Sn)))
            k = 1
            while k < LC:
                dv(lambda: nc.vector.tensor_scalar(out=V(NSK), in0=tabs[:, :, k - 1], scalar1=-1.0, scalar2=None,
                                                   op0=ALU.mult))
                for sc in range(8):
                    ck, sk, nsk = tabc[:, sc, k - 1:k], tabs[:, sc, k - 1:k], sm[:, NSK, sc:sc + 1]
                    dv(lambda: nc.vector.tensor_scalar(out=tmpa[:, 0:k], in0=tabc[:, sc, 0:k], scalar1=ck, scalar2=None,
                                                       op0=ALU.mult))
                    dv(lambda: nc.vector.tensor_scalar(out=tmpb[:, 0:k], in0=tabs[:, sc, 0:k], scalar1=ck, scalar2=None,
                                                       op0=ALU.mult))
                    dv(lambda: nc.vector.scalar_tensor_tensor(out=tabc[:, sc, k:2 * k], in0=tabs[:, sc, 0:k], scalar=nsk,
                                                              in1=tmpa[:, 0:k], op0=ALU.mult, op1=ALU.add))
                    dv(lambda: nc.vector.scalar_tensor_tensor(out=tabs[:, sc, k:2 * k], in0=tabc[:, sc, 0:k], scalar=sk,
                                                              in1=tmpb[:, 0:k], op0=ALU.mult, op1=ALU.add))
                k *= 2
            dv(lambda: nc.vector.tensor_scalar(out=V(NSL), in0=tabs[:, :, LC - 1], scalar1=-1.0, scalar2=None, op0=ALU.mult))
            for sc in range(8):
                dv(lambda: nc.vector.tensor_scalar(out=rB[:, sc, :], in0=tabc[:, sc, :], scalar1=0.0,
                                                   scalar2=sm[:, R, sc:sc + 1], op0=ALU.mult, op1=ALU.add))

            uT = self.sb(pes, "uT", [128, 2, S], BF16)
            ast = self.sb(pes, "ast", [128, 2, S], BF16)
            carry = self.sb(pes, "carry", [128, 8, 2], F32)
            ctmp = self.sb(pes, "ctmp", [128, 2], F32)
            glT = self.sb(pes, "glT", [128, 2, LC], BF16)
            y2 = self.sb(pes, "y2", [128, LC], F32)
            x2 = self.sb(pes, "x2", [128, LC], F32)
            sgg = self.sb(pes, "sgg", [128, LC], F32)
            names = ("t1", "t2", "mre", "mim", "gre", "gim", "u1", "u2")
            W = {n: [self.sb(pes, f"{n}{i}", [128, LC], F32) for i in range(2)] for n in names}
            Hh = {n: [self.sb(pes, f"{n}{i}", [128, LC], BF16) for i in range(2)] for n in ("hre", "him")}
            bW = {n: [B(n) for _ in range(2)] for n in names + ("hre", "him")}
            b_uT, b_ast, b_carry, b_ctmp, b_glT, b_y2, b_x2, b_sgg = [B(n) for n in
                                                                     ("uT", "ast", "carry", "ctmp", "glT", "y2", "x2", "sgg")]
            for s in range(self.nseq()):
                t0 = s * S
                self.dma("sp", uT[:], self.d_fm[0:2, :, t0:t0 + S].rearrange("k p t -> p k t"), b_uT,
                         reads=[self.b_dfm], writes=[b_uT])
                self.I("dve", [], [b_carry], lambda: nc.vector.memset(carry[:].rearrange("p a b -> p (a b)"), 0.0))
                if NCH < S // LC:
                    self.I("dve", [], [b_ast], lambda: nc.vector.memset(ast[:].rearrange("p a b -> p (a b)"), 0.0))
                for c in range(NCH):
                    cs = slice(c * LC, (c + 1) * LC)
                    for cc in range(2):
                        py, bpy = ps[4 + cc], b_ps[4 + cc]
                        for kk, sc in enumerate(range(4 * cc, 4 * cc + 4)):
                            a = sc % 2
                            pbr, bpbr = ps[a * 2], b_ps[a * 2]
                            pbi, bpbi = ps[a * 2 + 1], b_ps[a * 2 + 1]
                            self.I("pe", [b_set, b_uT], [bpbr],
                                   lambda: nc.tensor.matmul(pbr[:], lhsT=BbdT[:, sc, 0, :], rhs=uT[:, cc, cs],
                                                            start=True, stop=True))
                            self.I("pe", [b_set, b_uT], [bpbi],
                                   lambda: nc.tensor.matmul(pbi[:], lhsT=BbdT[:, sc, 1, :], rhs=uT[:, cc, cs],
                                                            start=True, stop=True))
                            tc_, ts_ = tabc[:, sc, :], tabs[:, sc, :]
                            t1, t2, mre, mim = W["t1"][a], W["t2"][a], W["mre"][a], W["mim"][a]
                            gre, gim, u1, u2 = W["gre"][a], W["gim"][a], W["u1"][a], W["u2"][a]
                            hre, him = Hh["hre"][a], Hh["him"][a]

                            def TT(eng, reads, wr, o, x, y, op):
                                e = nc.vector if eng == "dve" else nc.gpsimd
                                self.I(eng, reads, wr, lambda: e.tensor_tensor(out=o, in0=x, in1=y, op=op))

                            TT("dve", [b_set, bpbr], [bW["t1"][a]], t1[:], tc_, pbr[:], ALU.mult)
                            TT("dve", [b_set, bpbi], [bW["t2"][a]], t2[:], ts_, pbi[:], ALU.mult)
                            TT("dve", [bW["t1"][a], bW["t2"][a]], [bW["mre"][a]], mre[:], t1[:], t2[:], ALU.add)
                            TT("dve", [b_set, bpbi], [bW["t1"][a]], t1[:], tc_, pbi[:], ALU.mult)
                            TT("dve", [b_set, bpbr], [bW["t2"][a]], t2[:], ts_, pbr[:], ALU.mult)
                            TT("dve", [bW["t1"][a], bW["t2"][a]], [bW["mim"][a]], mim[:], t1[:], t2[:], ALU.subtract)
                            self.I("dve", [b_set, bW["mre"][a], b_carry], [bW["gre"][a]],
                                   lambda: nc.vector.tensor_tensor_scan(out=gre[:], data0=rB[:, sc, :], data1=mre[:],
                                                                        initial=carry[:, sc, 0:1], op0=ALU.mult,
                                                                        op1=ALU.add))
                            self.I("dve", [b_set, bW["mim"][a], b_carry], [bW["gim"][a]],
                                   lambda: nc.vector.tensor_tensor_scan(out=gim[:], data0=rB[:, sc, :], data1=mim[:],
                                                                        initial=carry[:, sc, 1:2], op0=ALU.mult,
                                                                        op1=ALU.add))
                            cl, sl_, nsl = tabc[:, sc, LC - 1:LC], tabs[:, sc, LC - 1:LC], sm[:, NSL, sc:sc + 1]
                            self.I("dve", [bW["gre"][a], b_set], [b_ctmp],
                                   lambda: nc.vector.tensor_scalar(out=ctmp[:, 0:1], in0=gre[:, LC - 1:LC], scalar1=cl,
                                                                   scalar2=None, op0=ALU.mult))
                            self.I("dve", [bW["gre"][a], b_set], [b_ctmp],
                                   lambda: nc.vector.tensor_scalar(out=ctmp[:, 1:2], in0=gre[:, LC - 1:LC], scalar1=sl_,
                                                                   scalar2=None, op0=ALU.mult))
                            self.I("dve", [bW["gim"][a], b_set, b_ctmp], [b_carry],
                                   lambda: nc.vector.scalar_tensor_tensor(out=carry[:, sc, 0:1], in0=gim[:, LC - 1:LC],
                                                                          scalar=nsl, in1=ctmp[:, 0:1], op0=ALU.mult,
                                                                          op1=ALU.add))
                            self.I("dve", [bW["gim"][a], b_set, b_ctmp], [b_carry],
                                   lambda: nc.vector.scalar_tensor_tensor(out=carry[:, sc, 1:2], in0=gim[:, LC - 1:LC],
                                                                          scalar=cl, in1=ctmp[:, 1:2], op0=ALU.mult,
                                                                          op1=ALU.add))
                            TT("pool", [b_set, bW["gre"][a]], [bW["u1"][a]], u1[:], tc_, gre[:], ALU.mult)
                            TT("pool", [b_set, bW["gim"][a]], [bW["u2"][a]], u2[:], ts_, gim[:], ALU.mult)
                            TT("pool", [bW["u1"][a], bW["u2"][a]], [bW["hre"][a]], hre[:], u1[:], u2[:], ALU.subtract)
                            TT("pool", [b_set, bW["gre"][a]], [bW["u1"][a]], u1[:], ts_, gre[:], ALU.mult)
                            TT("pool", [b_set, bW["gim"][a]], [bW["u2"][a]], u2[:], tc_, gim[:], ALU.mult)
                            TT("pool", [bW["u1"][a], bW["u2"][a]], [bW["him"][a]], him[:], u1[:], u2[:], ALU.add)
                            self.I("pe", [b_set, bW["hre"][a]], [bpy],
                                   lambda: nc.tensor.matmul(py[:], lhsT=Cbd[:, sc, 0, :], rhs=hre[:], start=(kk == 0),
                                                            stop=False))
                            self.I("pe", [b_set, bW["him"][a]], [bpy],
                                   lambda: nc.tensor.matmul(py[:], lhsT=Cbd[:, sc, 1, :], rhs=him[:], start=False,
                                                            stop=(kk == 3)))
                        self.I("dve", [bpy, b_uT, self.b_cst], [b_y2],
                               lambda: nc.vector.scalar_tensor_tensor(out=y2[:], in0=uT[:, cc, cs],
                                                                      scalar=self.s5d[:, l * 2 + cc:l * 2 + cc + 1],
                                                                      in1=py[:], op0=ALU.mult, op1=ALU.add))
                        self.I("dve", [b_y2], [b_x2], lambda: nc.vector.tensor_tensor(out=x2[:], in0=y2[:], in1=y2[:], op=ALU.mult))
                        self.I("dve", [b_x2], [b_x2],
                               lambda: nc.vector.tensor_scalar(out=x2[:], in0=x2[:], scalar1=0.044715, scalar2=1.0,
                                                               op0=ALU.mult, op1=ALU.add))
                        self.I("dve", [b_x2, b_y2], [b_x2], lambda: nc.vector.tensor_tensor(out=x2[:], in0=x2[:], in1=y2[:], op=ALU.mult))
                        self.I("act", [b_x2], [b_sgg],
                               lambda: nc.scalar.activation(out=sgg[:], in_=x2[:], func=AF.Sigmoid, scale=2.0 * 0.7978845608028654))
                        self.I("dve", [b_sgg, b_y2], [b_glT],
                               lambda: nc.vector.tensor_tensor(out=glT[:, cc, :], in0=y2[:], in1=sgg[:], op=ALU.mult))
                    for o in range(2):
                        pg_, bpg = ps[6], b_ps[6]
                        pv_, bpv = ps[7], b_ps[7]
                        for kc in range(2):
                            self.I("pe", [b_wg, b_glT], [bpg],
                                   lambda: nc.tensor.matmul(pg_[:], lhsT=wglu[:, kc, (2 + o) * 128:(3 + o) * 128],
                                                            rhs=glT[:, kc, :], start=(kc == 0), stop=(kc == 1)))
                        for kc in range(2):
                            self.I("pe", [b_wg, b_glT], [bpv],
                                   lambda: nc.tensor.matmul(pv_[:], lhsT=wglu[:, kc, o * 128:(o + 1) * 128],
                                                            rhs=glT[:, kc, :], start=(kc == 0), stop=(kc == 1)))
                        self.I("act", [bpg], [b_sgg], lambda: nc.scalar.activation(out=sgg[:], in_=pg_[:], func=AF.Sigmoid))
                        self.I("dve", [bpv, b_sgg], [b_ast],
                               lambda: nc.vector.tensor_tensor(out=ast[:, o, cs], in0=sgg[:], in1=pv_[:], op=ALU.mult))
                self.dma("pool", self.d_br[0:2, :, t0:t0 + S].rearrange("k p t -> p k t"), ast[:], b_ast,
                         reads=[b_ast], writes=[self.b_dbr])
            self.phase_end()

    def fox_phase(self, l):
        nc = self.nc
        self.phase_begin()
        ps, b_ps = self.ps, self.b_ps
        B = self.buf
        NB = S // 128
        NJ = self.debug.get("nj", NB)
        with contextlib.ExitStack() as pes:
            wf = self.sb(pes, "wf", [128, NB, 16], F32)
            V = self.sb(pes, "fV", [128, NB, 384], BF16)
            qT = [self.sb(pes, f"fq{i}", [64, S], BF16) for i in range(2)]
            kT = [self.sb(pes, f"fk{i}", [64, S], BF16) for i in range(2)]
            cst = [self.sb(pes, f"fc{i}", [64, S], BF16) for i in range(2)]
            fb = self.sb(pes, "fb", [128, NB, 6], F32)
            tb = self.sb(pes, "ftb", [128, NB, 6], F32)
            offs = self.sb(pes, "foffs", [128, NB, 6], F32)
            cumT = self.sb(pes, "fcum", [128, NB, 6], F32)
            refB = self.sb(pes, "fref", [128, NB, 6], F32)
            biasJ = [self.sb(pes, f"fbj{i}", [128, NB], F32) for i in range(2)]
            PT = [self.sb(pes, f"fpt{i}", [128, 128], BF16) for i in range(3)]
            rec = [self.sb(pes, f"frec{i}", [64, 128], F32) for i in range(2)]
            b_wf, b_V, b_fb, b_tb, b_offs, b_cum, b_ref = B("wf"), B("V"), B("fb"), B("tb"), B("offs"), B("cum"), B("ref")
            b_qT = [B("q") for _ in range(2)]
            b_kT = [B("k") for _ in range(2)]
            b_cst = [B("c") for _ in range(2)]
            b_bj = [B("bj") for _ in range(2)]
            b_PT = [B("pt") for _ in range(3)]
            b_rec = [B("rec") for _ in range(2)]
            cnt = 0
            if NJ < NB:
                for i2 in range(2):
                    self.I("dve", [], [b_cst[i2]], lambda: nc.vector.memset(cst[i2][:], 0.0))
            for s in range(self.nseq()):
                t0 = s * S
                self.dma("sp", wf[:], self.d_wf[t0:t0 + S, :].rearrange("(n p) c -> p n c", p=128), b_wf,
                         reads=[self.b_dtm], writes=[b_wf])
                self.dma("sp", V[:], self.d_vc[t0:t0 + S, :].rearrange("(n p) c -> p n c", p=128), b_V,
                         reads=[self.b_dtm], writes=[b_V])
                for h in range(6):
                    self.I("dve", [b_wf, self.b_cst], [b_fb],
                           lambda: nc.vector.tensor_scalar(out=fb[:, :, h], in0=wf[:, :, 4 + h],
                                                           scalar1=self.foxbf[:, l * 6 + h:l * 6 + h + 1],
                                                           scalar2=None, op0=ALU.add))
                self.I("act", [b_fb], [b_fb], lambda: nc.scalar.activation(out=FL(fb), in_=FL(fb), func=AF.Exp, scale=-1.0))
                self.I("act", [b_fb, self.b_cst], [b_fb],
                       lambda: nc.scalar.activation(out=FL(fb), in_=FL(fb), func=AF.Ln, bias=self.one_t[:], scale=1.0))
                self.I("pe", [b_fb, self.b_cst], [b_ps[0]],
                       lambda: nc.tensor.matmul(ps[0][:, 0:NB * 6], lhsT=self.tri_f[:], rhs=FL(fb), start=True, stop=True))
                self.I("pe", [b_fb, self.b_cst], [b_ps[1]],
                       lambda: nc.tensor.matmul(ps[1][:, 0:NB * 6], lhsT=self.ones_f[:], rhs=FL(fb), start=True, stop=True))
                self.I("dve", [b_ps[1]], [b_tb], lambda: nc.vector.tensor_copy(out=FL(tb), in_=ps[1][:, 0:NB * 6]))
                self.I("dve", [], [b_offs], lambda: nc.vector.memset(offs[:, 0, :], 0.0))
                for b in range(1, NB):
                    self.I("dve", [b_offs, b_tb], [b_offs],
                           lambda: nc.vector.tensor_tensor(out=offs[:, b, :], in0=offs[:, b - 1, :], in1=tb[:, b - 1, :],
                                                           op=ALU.add))
                self.I("dve", [b_ps[0], b_offs], [b_cum],
                       lambda: nc.vector.tensor_tensor(out=FL(cumT), in0=FL(offs), in1=ps[0][:, 0:NB * 6], op=ALU.add))
                self.I("dve", [b_offs, b_tb], [b_ref],
                       lambda: nc.vector.tensor_tensor(out=FL(refB), in0=FL(offs), in1=FL(tb), op=ALU.add))
                for h in range(6):
                    sl = h % 2
                    r0 = (h % 2) * 64
                    self.dma("sp", qT[sl][:], self.d_fm[9 + h // 2, r0:r0 + 64, t0:t0 + S], b_qT[sl],
                             reads=[self.b_dfm], writes=[b_qT[sl]])
                    self.dma("sp", kT[sl][:], self.d_fm[12 + h // 2, r0:r0 + 64, t0:t0 + S], b_kT[sl],
                             reads=[self.b_dfm], writes=[b_kT[sl]])
                    for j in range(NJ):
                        bj, b_bjj = biasJ[j % 2], b_bj[j % 2]
                        self.I("dve", [b_cum, b_ref], [b_bjj],
                               lambda: nc.vector.tensor_scalar(out=bj[:, 0:j + 1], in0=cumT[:, 0:j + 1, h],
                                                               scalar1=refB[:, j, h:h + 1], scalar2=None,
                                                               op0=ALU.subtract))
                        po, bpo = ps[2 + (j % 2) * 2], b_ps[2 + (j % 2) * 2]
                        pso, bpso = ps[3 + (j % 2) * 2], b_ps[3 + (j % 2) * 2]
                        js = slice(j * 128, (j + 1) * 128)
                        for i in range(j + 1):
                            cnt += 1
                            pq, bpq = ps[6 + cnt % 2], b_ps[6 + cnt % 2]
                            pt, bpt = PT[cnt % 3], b_PT[cnt % 3]
                            isl = slice(i * 128, (i + 1) * 128)
                            self.I("pe", [b_kT[sl], b_qT[sl]], [bpq],
                                   lambda: nc.tensor.matmul(pq[:, 0:128], lhsT=kT[sl][:, isl], rhs=qT[sl][:, js],
                                                            start=True, stop=(i != j)))
                            if i == j:
                                self.I("pe", [self.b_cst], [bpq],
                                       lambda: nc.tensor.matmul(pq[:, 0:128], lhsT=self.ident_bf[:], rhs=self.causneg_bf[:],
                                                                start=False, stop=True))
                            self.I("act", [bpq, b_bjj], [bpt],
                                   lambda: nc.scalar.activation(out=pt[:], in_=pq[:, 0:128], func=AF.Exp,
                                                                bias=bj[:, i:i + 1], scale=0.125))
                            self.I("pe", [b_V, bpt], [bpo],
                                   lambda: nc.tensor.matmul(po[0:64, 0:128], lhsT=V[:, i, h * 64:(h + 1) * 64], rhs=pt[:],
                                                            start=(i == 0), stop=(i == j)))
                            self.I("pe", [self.b_ones, bpt], [bpso],
                                   lambda: nc.tensor.matmul(pso[0:64, 0:128], lhsT=self.ones_bf[:, 0:64], rhs=pt[:],
                                                            start=(i == 0), stop=(i == j)))
                        rc, brc = rec[j % 2], b_rec[j % 2]
                        self.I("dve", [bpso], [brc], lambda: nc.vector.reciprocal(out=rc[:], in_=pso[0:64, 0:128]))
                        self.I("dve", [bpo, brc], [b_cst[sl]],
                               lambda: nc.vector.tensor_tensor(out=cst[sl][:, js], in0=rc[:], in1=po[0:64, 0:128],
                                                               op=ALU.mult))
                    self.dma("pool", self.d_br[5 + h // 2, r0:r0 + 64, t0:t0 + S], cst[sl][:], b_cst[sl],
                             reads=[b_cst[sl]], writes=[self.b_dbr])
            self.phase_end()

    def dsa_phase(self, l):
        nc = self.nc
        self.phase_begin()
        ps, b_ps = self.ps, self.b_ps
        B = self.buf
        NB = S // 128
        NJ = self.debug.get("nj", NB)
        NR = self.debug.get("nrounds", 32)
        with contextlib.ExitStack() as pes:
            wf = self.sb(pes, "dwf", [128, NB, 16], F32)
            ckv = self.sb(pes, "ckv", [128, S], BF16)
            cn = self.sb(pes, "cn", [128, S], BF16)
            sq = [self.sb(pes, f"dsq{i}", [128, 512], BF16) for i in range(2)]
            rs = [self.sb(pes, f"drs{i}", [128, 512], F32) for i in range(2)]
            Kp = self.sb(pes, "Kp", [65, S], BF16)
            Vd = self.sb(pes, "Vd", [128, NB, 64], BF16)
            Qp = self.sb(pes, "Qp", [65, 6, S], BF16)
            qi = self.sb(pes, "qi", [64, 4, S], BF16)
            ki = self.sb(pes, "ki", [64, S], BF16)
            wuk = self.sb(pes, "wuk", [128, 64], BF16)
            wuv = self.sb(pes, "wuv", [128, 64], BF16)
            score = self.sb(pes, "score", [128, S], F32)
            work = self.sb(pes, "work", [128, S], F32)
            rr = [self.sb(pes, f"rr{i}", [128, 512], F32) for i in range(2)]
            m8 = [self.sb(pes, f"m8{i}", [128, 8], F32) for i in range(2)]
            thr0 = self.sb(pes, "thr0", [128, 1], F32)
            negm = [self.sb(pes, f"negm{i}", [128, S], BF16) for i in range(2)]
            PT = [self.sb(pes, f"dpt{i}", [128, 384], BF16) for i in range(3)]
            rec = [self.sb(pes, f"drec{i}", [64, 384], F32) for i in range(2)]
            bst = self.sb(pes, "bst", [64, NB, 6, 128], BF16)
            (b_wf, b_ckv, b_cn, b_Kp, b_Vd, b_Qp, b_qi, b_ki, b_w, b_score, b_work, b_thr0, b_bst) = [
                B(n) for n in ("wf", "ckv", "cn", "Kp", "Vd", "Qp", "qi", "ki", "w", "score", "work", "thr0", "bst")]
            b_sq = [B("sq") for _ in range(2)]
            b_rs = [B("rs") for _ in range(2)]
            b_rr = [B("rr") for _ in range(2)]
            b_m8 = [B("m8") for _ in range(2)]
            b_negm = [B("negm") for _ in range(2)]
            b_PT = [B("pt") for _ in range(3)]
            b_rec = [B("rec") for _ in range(2)]
            self.dma("pool", wuk[:], self.wuk_in[l * 128:(l + 1) * 128, :], b_w, writes=[b_w])
            self.dma("pool", wuv[:], self.wuv_in[l * 128:(l + 1) * 128, :], b_w, writes=[b_w])
            self.I("dve", [], [b_Kp], lambda: nc.vector.memset(Kp[64:65, :], 1.0))
            self.I("dve", [], [b_thr0], lambda: nc.vector.memset(thr0[:], -1e29))
            if NJ < NB:
                self.I("dve", [], [b_bst], lambda: nc.vector.memset(bst[:].rearrange("p a b c -> p (a b c)"), 0.0))
            self.dma("pool", Qp[64:65, :, :], self.rb31_in[0:1, :].rearrange("o (h t) -> o h t", h=6), b_Qp, writes=[b_Qp])
            cnt = 0
            for s in range(self.nseq()):
                t0 = s * S
                self.dma("sp", wf[:], self.d_wf[t0:t0 + S, :].rearrange("(n p) c -> p n c", p=128), b_wf,
                         reads=[self.b_dtm], writes=[b_wf])
                self.dma("sp", ckv[:], self.d_fm[5, :, t0:t0 + S], b_ckv, reads=[self.b_dfm], writes=[b_ckv])
                for h in range(6):
                    r0 = (h % 2) * 64
                    self.dma("sp", Qp[0:64, h, :], self.d_fm[2 + h // 2, r0:r0 + 64, t0:t0 + S], b_Qp,
                             reads=[self.b_dfm], writes=[b_Qp])
                for h in range(4):
                    r0 = (h % 2) * 64
                    self.dma("sp", qi[:, h, :], self.d_fm[6 + h // 2, r0:r0 + 64, t0:t0 + S], b_qi,
                             reads=[self.b_dfm], writes=[b_qi])
                self.dma("sp", ki[:], self.d_fm[8, 0:64, t0:t0 + S], b_ki, reads=[self.b_dfm], writes=[b_ki])
                for c4 in range(4):
                    cs = slice(c4 * 512, (c4 + 1) * 512)
                    i2 = c4 % 2
                    pp, bp = ps[i2], b_ps[i2]
                    self.I("act", [b_ckv], [b_sq[i2]],
                           lambda: nc.scalar.activation(out=sq[i2][:], in_=ckv[:, cs], func=AF.Square))
                    self.I("pe", [b_sq[i2], self.b_ones], [bp],
                           lambda: nc.tensor.matmul(pp[:], lhsT=self.ones_bf[:], rhs=sq[i2][:], start=True, stop=True))
                    self.I("act", [bp, self.b_ones], [b_rs[i2]],
                           lambda: nc.scalar.activation(out=rs[i2][:], in_=pp[:], func=AF.Sqrt, bias=self.eps_t[:],
                                                        scale=1.0 / 128))
                    self.I("dve", [b_rs[i2]], [b_rs[i2]], lambda: nc.vector.reciprocal(out=rs[i2][:], in_=rs[i2][:]))
                    self.I("dve", [b_ckv, b_rs[i2], self.b_cst], [b_cn],
                           lambda: nc.vector.scalar_tensor_tensor(out=cn[:, cs], in0=ckv[:, cs],
                                                                  scalar=self.kvnorm[:, l:l + 1], in1=rs[i2][:],
                                                                  op0=ALU.mult, op1=ALU.mult))
                for c4 in range(4):
                    cs = slice(c4 * 512, (c4 + 1) * 512)
                    pp, bp = ps[2 + c4 % 2], b_ps[2 + c4 % 2]
                    self.I("pe", [b_w, b_cn], [bp],
                           lambda: nc.tensor.matmul(pp[0:64, :], lhsT=wuk[:], rhs=cn[:, cs], start=True, stop=True))
                    self.I("act", [bp], [b_Kp], lambda: nc.scalar.copy(out=Kp[0:64, cs], in_=pp[0:64, :]))
                for blk in range(NB):
                    pp, bp = ps[4 + blk % 2], b_ps[4 + blk % 2]
                    self.I("pe", [b_w, b_cn], [bp],
                           lambda: nc.tensor.matmul(pp[:, 0:64], lhsT=cn[:, blk * 128:(blk + 1) * 128], rhs=wuv[:],
                                                    start=True, stop=True))
                    self.I("dve", [bp], [b_Vd], lambda: nc.vector.tensor_copy(out=Vd[:, blk, :], in_=pp[:, 0:64]))
                cntl = [0]

                def front(j):
                    nk = (j + 1) * 128
                    js = slice(j * 128, (j + 1) * 128)
                    for h in range(4):
                        for c0 in range(0, nk, 512):
                            w = min(512, nk - c0)
                            cntl[0] += 1
                            cnt = cntl[0]
                            pi, bpi = ps[cnt % 2], b_ps[cnt % 2]
                            rt, brt = rr[cnt % 2], b_rr[cnt % 2]
                            self.I("pe", [b_qi, b_ki], [bpi],
                                   lambda: nc.tensor.matmul(pi[:, 0:w], lhsT=qi[:, h, js], rhs=ki[:, c0:c0 + w],
                                                            start=True, stop=True))
                            self.I("act", [bpi], [brt],
                                   lambda: nc.scalar.activation(out=rt[:, 0:w], in_=pi[:, 0:w], func=AF.Relu))
                            if h == 0:
                                self.I("dve", [brt, b_wf], [b_score],
                                       lambda: nc.vector.tensor_scalar(out=score[:, c0:c0 + w], in0=rt[:, 0:w],
                                                                       scalar1=wf[:, j, 0:1], scalar2=None, op0=ALU.mult))
                            else:
                                self.I("dve", [brt, b_wf, b_score], [b_score],
                                       lambda: nc.vector.scalar_tensor_tensor(
                                           out=score[:, c0:c0 + w], in0=rt[:, 0:w], scalar=wf[:, j, h:h + 1],
                                           in1=score[:, c0:c0 + w], op0=ALU.mult, op1=ALU.add))
                    self.I("dve", [b_score, self.b_cst], [b_score],
                           lambda: nc.vector.tensor_tensor(out=score[:, js], in0=score[:, js], in1=self.cneg_f[:],
                                                           op=ALU.add))
                    if j >= 2:
                        src = score
                        for r in range(NR):
                            m, bm = m8[r % 2], b_m8[r % 2]
                            self.I("dve", [b_score, b_work], [bm], lambda: nc.vector.max(out=m[:], in_=src[:, 0:nk]))
                            if r < NR - 1:
                                self.I("dve", [b_score, b_work, bm], [b_work],
                                       lambda: nc.vector.match_replace(out=work[:, 0:nk], in_to_replace=m[:],
                                                                       in_values=src[:, 0:nk], imm_value=-3.0e38))
                                src = work
                        thr, bthr = m[:, 7:8], bm
                    else:
                        thr, bthr = thr0[:], b_thr0
                    ng, bng = negm[j % 2], b_negm[j % 2]
                    self.I("dve", [b_score, bthr], [bng],
                           lambda: nc.vector.tensor_scalar(out=ng[:, 0:nk], in0=score[:, 0:nk], scalar1=thr,
                                                           scalar2=-30000.0, op0=ALU.is_lt, op1=ALU.mult))
                    return ng, bng

                def back(j, ng, bng):
                    js = slice(j * 128, (j + 1) * 128)
                    for hf in range(2):
                        po, bpo = ps[4 + hf], b_ps[4 + hf]
                        pso, bpso = ps[6 + hf], b_ps[6 + hf]
                        hs = slice(3 * hf, 3 * hf + 3)
                        for i in range(j + 1):
                            cntl[0] += 1
                            cnt = cntl[0]
                            pq, bpq = ps[2 + cnt % 2], b_ps[2 + cnt % 2]
                            pt, bpt = PT[cnt % 3], b_PT[cnt % 3]
                            isl = slice(i * 128, (i + 1) * 128)
                            far = (j - i) >= 2
                            kr = 65 if far else 64
                            self.I("pe", [b_Kp, b_Qp], [bpq],
                                   lambda: nc.tensor.matmul(pq[:, 0:384], lhsT=Kp[0:kr, isl], rhs=Qp[0:kr, hs, js],
                                                            start=True, stop=False))
                            if not far:
                                self.I("pe", [self.b_cst], [bpq],
                                       lambda: nc.tensor.matmul(pq[:, 0:384], lhsT=self.ident_bf[:],
                                                                rhs=self.biasT[:, j - i, hs, :], start=False, stop=False))
                            self.I("pe", [bng, self.b_cst], [bpq],
                                   lambda: nc.tensor.matmul(pq[:, 0:384], lhsT=ng[:, isl], rhs=self.I3[:],
                                                            start=False, stop=True))
                            self.I("act", [bpq], [bpt],
                                   lambda: nc.scalar.activation(out=pt[:], in_=pq[:, 0:384], func=AF.Exp))
                            self.I("pe", [b_Vd, bpt], [bpo],
                                   lambda: nc.tensor.matmul(po[0:64, 0:384], lhsT=Vd[:, i, :], rhs=pt[:],
                                                            start=(i == 0), stop=(i == j)))
                            self.I("pe", [self.b_ones, bpt], [bpso],
                                   lambda: nc.tensor.matmul(pso[0:64, 0:384], lhsT=self.ones_bf[:, 0:64], rhs=pt[:],
                                                            start=(i == 0), stop=(i == j)))
                        rc, brc = rec[hf], b_rec[hf]
                        self.I("dve", [bpso], [brc], lambda: nc.vector.reciprocal(out=rc[:], in_=pso[0:64, 0:384]))
                        self.I("dve", [bpo, brc], [b_bst],
                               lambda: nc.vector.tensor_tensor(out=bst[:, j, hs, :].rearrange("p a b -> p (a b)"), in0=rc[:], in1=po[0:64, 0:384],
                                                               op=ALU.mult))

                nxt = front(0)
                for j in range(NJ):
                    cur = nxt
                    if j + 1 < NJ:
                        nxt = front(j + 1)
                    back(j, *cur)
                for h in range(6):
                    r0 = (h % 2) * 64
                    self.dma("pool", self.d_br[2 + h // 2, r0:r0 + 64, t0:t0 + S].rearrange("p (n t) -> p n t", t=128), bst[:, :, h, :], b_bst,
                             reads=[b_bst], writes=[self.b_dbr])
            self.phase_end()

    def phase_a(self, l):
        nc = self.nc
        pes = contextlib.ExitStack()
        self.phase_begin()
        with pes:
            xb = self.sb(pes, "xb", [128, 8, TB], F32)
            hT = self.sb(pes, "hT", [128, 8, TB], BF16)
            actT = self.sb(pes, "actT", [128, NFC, TB], BF16)
            wgu = [self.sb(pes, f"wgu{i}", [128, 2, 8, 128], BF16) for i in range(2)]
            wd = [self.sb(pes, f"wd{i}", [128, NFC, 128], BF16) for i in range(2)]
            wfm = [self.sb(pes, f"wfm{i}", [128, 8, 128], BF16) for i in range(3)]
            wtm = self.sb(pes, "wtm", [128, 8, NTM], BF16)
            sq = [self.sb(pes, f"sq{i}", [128, 512], BF16) for i in range(2)]
            rs = [self.sb(pes, f"rs{i}", [128, 512], F32) for i in range(2)]
            sg = [self.sb(pes, f"sg{i}", [128, 512], F32) for i in range(2)]
            stg = [self.sb(pes, f"stg{i}", [128, TB], BF16) for i in range(4)]
            stv = self.sb(pes, "stv", [128, 8, 384], BF16)
            stw = self.sb(pes, "stw", [128, 8, 16], F32)
            B = self.buf
            b_xb = [B("xb") for _ in range(8)]
            b_hT = [B("hT") for _ in range(8)]
            b_actT = [B("actT") for _ in range(NFC)]
            b_wgu = [B("wgu") for _ in range(2)]
            b_wd = [B("wd") for _ in range(2)]
            b_wfm = [B("wfm") for _ in range(3)]
            b_wtm = B("wtm")
            b_sq = [B("sq") for _ in range(2)]
            b_rs = [B("rs") for _ in range(2)]
            b_sg = [B("sg") for _ in range(2)]
            b_stg = [B("stg") for _ in range(4)]
            b_stv = B("stv")
            b_stw = B("stw")
            ps, b_ps = self.ps, self.b_ps
            self.I("dve", [], [b_stw], lambda: nc.vector.memset(stw[:], 0.0))
            NFCL = self.debug.get('nfc_lim', NFC)

            def nw(g, ll, kc):
                c = (g * L + ll) * 8 + kc
                return self.normw[:, c:c + 1]

            def norm(g, ll, final=False):
                for tg in range(2):
                    cs = slice(tg * 512, (tg + 1) * 512)
                    ssp, b_ss = ps[6 + tg], b_ps[6 + tg]
                    for kc in range(8):
                        i = kc % 2
                        self.I("act", [b_xb[kc]], [b_sq[i]],
                               lambda: nc.scalar.activation(out=sq[i][:], in_=xb[:, kc, cs], func=AF.Square))
                        self.I("pe", [b_sq[i], self.b_ones], [b_ss],
                               lambda: nc.tensor.matmul(ssp[:], lhsT=self.ones_bf[:], rhs=sq[i][:],
                                                        start=(kc == 0), stop=(kc == 7)))
                    self.I("act", [b_ss, self.b_ones], [b_rs[tg]],
                           lambda: nc.scalar.activation(out=rs[tg][:], in_=ssp[:], func=AF.Sqrt,
                                                        bias=self.eps_t[:], scale=1.0 / D))
                    self.I("dve", [b_rs[tg]], [b_rs[tg]],
                           lambda: nc.vector.reciprocal(out=rs[tg][:], in_=rs[tg][:]))
                    for kc in range(8):
                        if final:
                            self.I("dve", [b_xb[kc], b_rs[tg], self.b_normw], [b_xb[kc]],
                                   lambda: nc.vector.scalar_tensor_tensor(
                                       out=xb[:, kc, cs], in0=xb[:, kc, cs],
                                       scalar=self.normw[:, 3 * L * 8 + kc:3 * L * 8 + kc + 1],
                                       in1=rs[tg][:], op0=ALU.mult, op1=ALU.mult))
                        else:
                            self.I("dve", [b_xb[kc], b_rs[tg], self.b_normw], [b_hT[kc]],
                                   lambda: nc.vector.scalar_tensor_tensor(
                                       out=hT[:, kc, cs], in0=xb[:, kc, cs], scalar=nw(g, ll, kc),
                                       in1=rs[tg][:], op0=ALU.mult, op1=ALU.mult))

            def load_wgu(ll, j, fc):
                r0 = ((ll * 2 + j) * NFC + fc) * 128
                i = fc % 2
                self.dma("sp", wgu[i][:], self.wgu_b[r0:r0 + 128, :].rearrange("p (a k f) -> p a k f", a=2, k=8),
                         b_wgu[i], reads=[self.b_wcast], writes=[b_wgu[i]])

            def load_wd(ll, j, dc):
                r0 = ((ll * 2 + j) * 8 + dc) * 128
                i = dc % 2
                self.dma("sp", wd[i][:], self.wd_b[r0:r0 + 128, :].rearrange("p (c d) -> p c d", c=NFC),
                         b_wd[i], reads=[self.b_wcast], writes=[b_wd[i]])

            def ffn(ll, j, g):
                load_wgu(ll, j, 0)
                norm(g, ll)
                for fc in range(NFCL):
                    if fc + 1 < NFCL:
                        load_wgu(ll, j, fc + 1)
                    else:
                        load_wd(ll, j, 0)
                    i = fc % 2
                    for tg in range(2):
                        cs = slice(tg * 512, (tg + 1) * 512)
                        pg, bg = ps[tg * 2], b_ps[tg * 2]
                        pu, bu = ps[tg * 2 + 1], b_ps[tg * 2 + 1]
                        for kc in range(8):
                            self.I("pe", [b_wgu[i], b_hT[kc]], [bg],
                                   lambda: nc.tensor.matmul(pg[:], lhsT=wgu[i][:, 0, kc, :], rhs=hT[:, kc, cs],
                                                            start=(kc == 0), stop=(kc == 7)))
                        for kc in range(8):
                            self.I("pe", [b_wgu[i], b_hT[kc]], [bu],
                                   lambda: nc.tensor.matmul(pu[:], lhsT=wgu[i][:, 1, kc, :], rhs=hT[:, kc, cs],
                                                            start=(kc == 0), stop=(kc == 7)))
                        self.I("act", [bg], [b_sg[tg]],
                               lambda: nc.scalar.activation(out=sg[tg][:], in_=pg[:], func=AF.Silu))
                        self.I("dve", [b_sg[tg], bu], [b_actT[fc]],
                               lambda: nc.vector.tensor_tensor(out=actT[:, fc, cs], in0=sg[tg][:], in1=pu[:],
                                                               op=ALU.mult))
                for dc in range(8):
                    if dc + 1 < 8:
                        load_wd(ll, j, dc + 1)
                    i = dc % 2
                    for tg in range(2):
                        cs = slice(tg * 512, (tg + 1) * 512)
                        pd, bd = ps[4 + tg], b_ps[4 + tg]
                        for fc in range(NFCL):
                            self.I("pe", [b_wd[i], b_actT[fc]], [bd],
                                   lambda: nc.tensor.matmul(pd[:], lhsT=wd[i][:, fc, :], rhs=actT[:, fc, cs],
                                                            start=(fc == 0), stop=(fc == NFCL - 1)))
                        self.I("dve", [bd, b_xb[dc]], [b_xb[dc]],
                               lambda: nc.vector.scalar_tensor_tensor(
                                   out=xb[:, dc, cs], in0=pd[:], scalar=0.5, in1=xb[:, dc, cs],
                                   op0=ALU.mult, op1=ALU.add))

            def load_wfm(ll, c, slot):
                r0 = (ll * NFM + c) * 128
                self.dma("sp", wfm[slot][:], self.winfm_b[r0:r0 + 128, :].rearrange("p (k m) -> p k m", k=8),
                         b_wfm[slot], reads=[self.b_wcast], writes=[b_wfm[slot]])

            def proj(ll, tb):
                t0 = tb * TB
                self.dma("sp", wtm[:], self.wintm_b[ll * 128:(ll + 1) * 128, :].rearrange("p (k m) -> p k m", k=8),
                         b_wtm, reads=[self.b_wcast], writes=[b_wtm])
                load_wfm(ll, 0, 0)
                load_wfm(ll, 1, 1)
                norm(1, ll)
                for c in range(NFM):
                    if c + 2 < NFM:
                        load_wfm(ll, c + 2, (c + 2) % 3)
                    slot = c % 3
                    si = c % 4
                    is_gate = c >= 15
                    for tg in range(2):
                        cs = slice(tg * 512, (tg + 1) * 512)
                        pp, bp = ps[(c * 2 + tg) % 4], b_ps[(c * 2 + tg) % 4]
                        for kc in range(8):
                            self.I("pe", [b_wfm[slot], b_hT[kc]], [bp],
                                   lambda: nc.tensor.matmul(pp[:], lhsT=wfm[slot][:, kc, :], rhs=hT[:, kc, cs],
                                                            start=(kc == 0), stop=(kc == 7)))
                        if is_gate:
                            self.I("act", [bp], [b_stg[si]],
                                   lambda: nc.scalar.activation(out=stg[si][:, cs], in_=pp[:], func=AF.Sigmoid))
                        elif c in (2, 3, 4):
                            self.I("act", [bp], [b_stg[si]],
                                   lambda: nc.scalar.mul(out=stg[si][:, cs], in_=pp[:], mul=0.125))
                        elif (c + tg) % 2 == 0:
                            self.I("act", [bp], [b_stg[si]],
                                   lambda: nc.scalar.copy(out=stg[si][:, cs], in_=pp[:]))
                        else:
                            self.I("dve", [bp], [b_stg[si]],
                                   lambda: nc.vector.tensor_copy(out=stg[si][:, cs], in_=pp[:]))
                    self.dma("pool", self.d_fm[c, :, t0:t0 + TB], stg[si][:], b_stg[si],
                             reads=[b_stg[si]], writes=[self.b_dfm])
                for tt in range(0 if self.debug.get('no_tm') else 8):
                    pp, bp = ps[4 + tt % 2], b_ps[4 + tt % 2]
                    for kc in range(8):
                        self.I("pe", [b_wtm, b_hT[kc]], [bp],
                               lambda: nc.tensor.matmul(pp[:, 0:NTM], lhsT=hT[:, kc, tt * 128:(tt + 1) * 128],
                                                        rhs=wtm[:, kc, :], start=(kc == 0), stop=(kc == 7)))
                    self.I("act", [bp], [b_stv],
                           lambda: nc.scalar.copy(out=stv[:, tt, :], in_=pp[:, 0:384]))
                    self.I("dve", [bp], [b_stw],
                           lambda: nc.vector.tensor_copy(out=stw[:, tt, 0:10], in_=pp[:, 384:394]))
                if self.debug.get('no_tm'):
                    return
                self.dma("pool", self.d_vc[t0:t0 + TB, :].rearrange("(n p) c -> p n c", p=128), stv[:], b_stv,
                         reads=[b_stv], writes=[self.b_dtm])
                if self.debug.get('no_wf'):
                    return
                self.dma("pool", self.d_wf[t0:t0 + TB, :].rearrange("(n p) c -> p n c", p=128), stw[:], b_stw,
                         reads=[b_stw], writes=[self.b_dtm])

            def load_w8(src, ll, oc, slot):
                r0 = (ll * 8 + oc) * 128
                self.dma("sp", wfm[slot][:], src[r0:r0 + 128, :].rearrange("p (k m) -> p k m", k=8),
                         b_wfm[slot], reads=[self.b_wcast], writes=[b_wfm[slot]])

            KCS = [(0, 2), (2, 5), (5, 8)]

            def merge(ll, tb):
                t0 = tb * TB
                self.dma("sp", hT[:], self.d_br[:, :, t0:t0 + TB].rearrange("k p t -> p k t"), b_hT[0],
                         reads=[self.b_dbr], writes=b_hT)
                load_w8(self.wbr_b, ll, 0, 0)
                for oc in range(8):
                    if oc + 1 < 8:
                        load_w8(self.wbr_b, ll, oc + 1, (oc + 1) % 3)
                    else:
                        load_w8(self.wout_b, ll, 0, (oc + 1) % 3)
                    slot = oc % 3
                    for br in range(3):
                        self.dma("sp", stg[br][:], self.d_fm[15 + br * 8 + oc, :, t0:t0 + TB], b_stg[br],
                                 reads=[self.b_dfm], writes=[b_stg[br]])
                    for tg in range(2):
                        cs = slice(tg * 512, (tg + 1) * 512)
                        for br in range(3):
                            pp, bp = ps[tg * 3 + br], b_ps[tg * 3 + br]
                            k0, k1 = KCS[br]
                            for kc in range(k0, k1):
                                self.I("pe", [b_wfm[slot], b_hT[kc]], [bp],
                                       lambda: nc.tensor.matmul(pp[:], lhsT=wfm[slot][:, kc, :], rhs=hT[:, kc, cs],
                                                                start=(kc == k0), stop=(kc == k1 - 1)))
                        p0, p1, p2 = ps[tg * 3], ps[tg * 3 + 1], ps[tg * 3 + 2]
                        q0, q1, q2 = b_ps[tg * 3], b_ps[tg * 3 + 1], b_ps[tg * 3 + 2]
                        self.I("dve", [q0, b_stg[0]], [b_sg[0]],
                               lambda: nc.vector.tensor_tensor(out=sg[0][:], in0=stg[0][:, cs], in1=p0[:], op=ALU.mult))
                        self.I("dve", [q1, b_stg[1]], [b_sg[1]],
                               lambda: nc.vector.tensor_tensor(out=sg[1][:], in0=stg[1][:, cs], in1=p1[:], op=ALU.mult))
                        self.I("dve", [b_sg[0], b_sg[1]], [b_sg[0]],
                               lambda: nc.vector.tensor_tensor(out=sg[0][:], in0=sg[0][:], in1=sg[1][:], op=ALU.add))
                        self.I("dve", [q2, b_stg[2]], [b_sg[1]],
                               lambda: nc.vector.tensor_tensor(out=sg[1][:], in0=stg[2][:, cs], in1=p2[:], op=ALU.mult))
                        self.I("dve", [b_sg[0], b_sg[1]], [b_actT[oc]],
                               lambda: nc.vector.tensor_tensor(out=actT[:, oc, cs], in0=sg[0][:], in1=sg[1][:],
                                                               op=ALU.add))
                for oc in range(8):
                    if oc + 1 < 8:
                        load_w8(self.wout_b, ll, oc + 1, (oc + 9) % 3)
                    slot = (oc + 8) % 3
                    for tg in range(2):
                        cs = slice(tg * 512, (tg + 1) * 512)
                        pp, bp = ps[6 + tg], b_ps[6 + tg]
                        for kc in range(8):
                            self.I("pe", [b_wfm[slot], b_actT[kc]], [bp],
                                   lambda: nc.tensor.matmul(pp[:], lhsT=wfm[slot][:, kc, :], rhs=actT[:, kc, cs],
                                                            start=(kc == 0), stop=(kc == 7)))
                        self.I("dve", [bp, b_xb[oc]], [b_xb[oc]],
                               lambda: nc.vector.tensor_tensor(out=xb[:, oc, cs], in0=xb[:, oc, cs], in1=pp[:],
                                                               op=ALU.add))

            for tb in range(self.debug.get('ntb', NTB)):
                t0 = tb * TB
                src = self.xT_in if l == 0 else self.xw
                self.dma("sp", xb[:], src.rearrange("(k p) t -> p k t", p=128)[:, :, t0:t0 + TB], b_xb[0],
                         reads=([] if l == 0 else [self.b_xw]), writes=b_xb)
                if l > 0:
                    merge(l - 1, tb)
                    ffn(l - 1, 1, 2)
                if l < self.nl:
                    ffn(l, 0, 0)
                    if not self.debug.get("no_proj"):
                        proj(l, tb)
                else:
                    norm(0, 0, final=True)
                dst = self.xw if l < self.nl else self.out
                if self.debug.get("x_to_out"):
                    dst = self.out
                self.dma("pool", dst.rearrange("(k p) t -> p k t", p=128)[:, :, t0:t0 + TB], xb[:], b_xb[0],
                         reads=b_xb, writes=[self.b_xw])
            self.phase_end()

    def finish(self):
        self.barrier()


def prep_weights(inp):
    f = np.float32
    out = {}
    nw = np.zeros((128, 3 * L * 8 + 8), f)
    for g, k in enumerate(["ffn1_norm", "mix_norm", "ffn2_norm"]):
        a = np.asarray(inp[k], f)
        nw[:, g * L * 8:(g + 1) * L * 8] = a.reshape(L, 8, 128).transpose(2, 0, 1).reshape(128, L * 8)
    nw[:, 3 * L * 8:] = np.asarray(inp["final_norm"], f).reshape(8, 128).T
    out["normw"] = nw
    wgu = np.empty((L, 2, NFC, 128, 2, 8, 128), f)
    wd = np.empty((L, 2, 8, 128, NFC, 128), f)
    for j, pre in enumerate(["ffn1", "ffn2"]):
        g = np.asarray(inp[pre + "_w_gate"], f).reshape(L, 8, 128, NFC, 128)
        u = np.asarray(inp[pre + "_w_up"], f).reshape(L, 8, 128, NFC, 128)
        wgu[:, j, :, :, 0] = g.transpose(0, 3, 2, 1, 4)
        wgu[:, j, :, :, 1] = u.transpose(0, 3, 2, 1, 4)
        dn = np.asarray(inp[pre + "_w_down"], f).reshape(L, NFC, 128, 8, 128)
        wd[:, j] = dn.transpose(0, 3, 2, 1, 4)
    out["wgu"] = wgu.reshape(L * 2 * NFC * 128, 2 * 8 * 128)
    out["wd"] = wd.reshape(L * 2 * 8 * 128, NFC * 128)
    w_in = np.asarray(inp["w_in"], f)
    w_pad = np.concatenate([w_in, np.zeros((L, D, 1), f)], axis=2)
    chunks = np.array(fm_chunk_cols())
    fm = w_pad[:, :, chunks]
    fm = fm.reshape(L, 8, 128, NFM, 128).transpose(0, 3, 2, 1, 4)
    out["winfm"] = np.ascontiguousarray(fm).reshape(L * NFM * 128, 8 * 128)
    tmc = list(range(O_VC, O_VC + 384)) + list(range(O_WI, O_WI + 4)) + list(range(O_FC, O_FC + 6))
    tm = w_in[:, :, tmc].reshape(L, 8, 128, NTM).transpose(0, 2, 1, 3)
    out["wintm"] = np.ascontiguousarray(tm).reshape(L * 128, 8 * NTM)
    ii = np.arange(128)
    cst = np.zeros((128, 6 * 128), f)
    cst[:, 0:128] = np.eye(128, dtype=f)
    cst[:, 128:256] = np.where(ii[:, None] > ii[None, :], -30000.0, 0.0)
    cst[:, 256:384] = np.where(ii[None, :] > ii[:, None], -1e30, 0.0)
    cst[:, 384:512] = (ii[:, None] <= ii[None, :]).astype(f)
    cst[:, 512:640] = 1.0
    out["consts"] = cst
    out["foxbf"] = np.ascontiguousarray(np.broadcast_to(np.asarray(inp["fox_b_f"], f).reshape(1, L * 6), (128, L * 6)))
    out["kvnorm"] = np.ascontiguousarray(np.asarray(inp["dsa_kv_norm"], f).T)
    out["wuk"] = np.ascontiguousarray(np.asarray(inp["dsa_w_uk"], f).reshape(L * 128, 64))
    out["wuv"] = np.ascontiguousarray(np.asarray(inp["dsa_w_uv"], f).reshape(L * 128, 64))
    rb = np.asarray(inp["rel_bias"], f)
    out["rb31"] = np.ascontiguousarray(np.broadcast_to(rb[31][:, None], (6, S))).reshape(1, 6 * S)
    dist = ii[None, None, :] - ii[:, None, None] + 128 * np.arange(2)[None, :, None]
    dd = np.maximum(dist, 1).astype(np.float32)
    logb = 16 + (np.log(dd / np.float32(16)) / np.float32(math.log(128 / 16)) * np.float32(16)).astype(np.int32)
    bucket = np.where(np.maximum(dist, 0) < 16, np.maximum(dist, 0), np.minimum(logb, 31))
    rbT = rb[bucket]
    rbT = np.where((dist >= 0)[..., None], rbT, 0.0).astype(f)
    out["rbT"] = np.ascontiguousarray(rbT.transpose(0, 1, 3, 2)).reshape(128, 2 * 6 * 128)
    def st(a):
        a = np.asarray(a, f)
        a = a.reshape((L, 8, 128) + a.shape[3:])
        return np.ascontiguousarray(np.moveaxis(a, 2, 0))
    lr = st(inp["ssm_lambda_re"]); li = st(inp["ssm_lambda_im"])
    ldt = st(np.broadcast_to(np.asarray(inp["ssm_log_dt"], f)[:, :, None], (L, 16, 64)))
    out["s5p"] = np.ascontiguousarray(np.stack([lr, li, ldt], axis=-1)).reshape(128, L * 8 * 3)
    bre = st(inp["ssm_b_re"]); bim = st(inp["ssm_b_im"])
    out["s5b"] = np.ascontiguousarray(np.stack([bre, bim], axis=3)).reshape(128, L * 8 * 2 * 16)
    cre = st(np.asarray(inp["ssm_c_re"], f).transpose(0, 1, 3, 2)); cim = st(np.asarray(inp["ssm_c_im"], f).transpose(0, 1, 3, 2))
    out["s5c"] = np.ascontiguousarray(np.stack([cre, cim], axis=3)).reshape(128, L * 8 * 2 * 16)
    out["s5d"] = np.ascontiguousarray(np.asarray(inp["ssm_d"], f).reshape(L, 2, 128).transpose(2, 0, 1)).reshape(128, L * 2)
    wg = np.asarray(inp["ssm_w_glu"], f).reshape(L, 2, 128, 512).transpose(0, 2, 1, 3)
    out["wglu"] = np.ascontiguousarray(wg).reshape(L * 128, 2 * 512)
    wbr = np.concatenate([np.asarray(inp["w_branch_ssm"], f), np.asarray(inp["w_branch_dsa"], f),
                          np.asarray(inp["w_branch_fox"], f)], axis=1)
    out["wbr"] = np.ascontiguousarray(wbr.reshape(L, 8, 128, 8, 128).transpose(0, 3, 2, 1, 4)).reshape(L * 8 * 128, 8 * 128)
    wo = np.asarray(inp["w_out"], f)
    out["wout"] = np.ascontiguousarray(wo.reshape(L, 8, 128, 8, 128).transpose(0, 3, 2, 1, 4)).reshape(L * 8 * 128, 8 * 128)
    return out


def run(inputs, n_layers=L, debug=None, cores=NCORES):
    b = Builder(n_layers=n_layers, debug=debug)
    nc = b.build()
    w = prep_weights(inputs)
    x = np.asarray(inputs["x"], np.float32)
    in_maps = []
    for c in range(cores):
        xs = x[c * NSEQ:(c + 1) * NSEQ].reshape(TOK, D)
        m = {"xT": np.ascontiguousarray(xs.T)}
        m.update(w)
        if n_layers < L:
            m["wgu"] = m["wgu"][:n_layers * 2 * NFC * 128]
            m["wd"] = m["wd"][:n_layers * 2 * 8 * 128]
            m["winfm"] = m["winfm"][:n_layers * NFM * 128]
            m["wintm"] = m["wintm"][:n_layers * 128]
            m["wbr"] = m["wbr"][:n_layers * 8 * 128]
            m["wout"] = m["wout"][:n_layers * 8 * 128]
        in_maps.append(m)
    res = run_bass_kernel_spmd(nc, in_maps, core_ids=list(range(cores)))
    return res


def kernel(**inputs):
    res = run(inputs)
    outs = []
    for c in range(NCORES):
        o = res.results[c]["out"]
        outs.append(np.ascontiguousarray(o.T).reshape(NSEQ, S, D))
    return np.concatenate(outs, axis=0).astype(np.float32)
```

```python
import contextlib
import math
import numpy as np
import concourse.bass as bass
import concourse.mybir as mybir
from concourse.bass_utils import run_bass_kernel_spmd

F32 = mybir.dt.float32
BF16 = mybir.dt.bfloat16
ALU = mybir.AluOpType
AF = mybir.ActivationFunctionType
AX = mybir.AxisListType

D = 1024
FH = 2816
NFC = FH // 128
S = 2048
NSEQ = 4
TOK = NSEQ * S
L = 4
TB = 1024
NTB = TOK // TB
EPS = 1e-6
NCORES = 8

IN_SPLITS = (256, 384, 128, 256, 64, 4, 384, 384, 384, 6, 1024, 1024, 1024)
OFF = np.concatenate([[0], np.cumsum(IN_SPLITS)]).tolist()
(O_U, O_QB, O_CKV, O_QI, O_KI, O_WI, O_QC, O_KC, O_VC, O_FC, O_GA, O_GB, O_GC, O_END) = OFF
NFM = 39
NTM = 394


def fm_chunk_cols():
    chunks = []
    for c in range(8):
        chunks.append(list(range(c * 128, (c + 1) * 128)))
    chunks.append(list(range(O_KI, O_KI + 64)) + [-1] * 64)
    for c in range(3):
        chunks.append(list(range(O_QC + c * 128, O_QC + (c + 1) * 128)))
    for c in range(3):
        chunks.append(list(range(O_KC + c * 128, O_KC + (c + 1) * 128)))
    for c in range(24):
        chunks.append(list(range(O_GA + c * 128, O_GA + (c + 1) * 128)))
    assert len(chunks) == NFM
    return chunks


def FL(t):
    return t[:].rearrange("p a b -> p (a b)")


class Buf:
    __slots__ = ("name", "w", "r", "sems", "psum")

    def __init__(self, name, psum=False):
        self.name = name
        self.psum = psum
        self.w = None
        self.r = {}
        self.sems = {}


class Builder:
    def __init__(self, n_layers=L, debug=None):
        self.nl = n_layers
        self.debug = debug or {}
        self.nc = bass.Bass("TRN2", target_bir_lowering=False)
        nc = self.nc
        self.es = contextlib.ExitStack()
        self.engs = {"pe": nc.tensor, "act": nc.scalar, "dve": nc.vector, "pool": nc.gpsimd, "sp": nc.sync}
        self.esem = {}
        self.ecnt = {}
        self.seen = {}
        for e in self.engs:
            self.esem[e] = self.es.enter_context(nc.semaphore("E" + e))
            self.ecnt[e] = 0
            self.seen[e] = {}
        self.dma_sems = []
        self.free_sems = {"hw": [], "sw": []}
        self.phase_sems = []
        self.in_phase = False
        self.nbuf = 0
        self.out_toks = []

    def buf(self, name, psum=False):
        self.nbuf += 1
        return Buf(f"{name}_{self.nbuf}", psum)

    def _wait(self, eng, reads, writes):
        need = {}

        def add(tok):
            key, sem, val = tok
            if key == "pe" and eng == "pe":
                return
            k = id(sem)
            if k not in need or need[k][1] < val:
                need[k] = (sem, val)

        for b in reads:
            if b.w is not None:
                add(b.w)
            if b.psum:
                for kk, t in b.r.items():
                    if kk != eng:
                        add(t)
        for b in writes:
            if b.w is not None:
                add(b.w)
            for t in b.r.values():
                add(t)
        seen = self.seen[eng]
        h = self.engs[eng]
        for k, (sem, val) in need.items():
            if seen.get(k, 0) >= val:
                continue
            h.wait_ge(sem, val)
            seen[k] = val

    def I(self, eng, reads, writes, fn):
        self._wait(eng, reads, writes)
        ins = fn()
        self.ecnt[eng] += 1
        ins.then_inc(self.esem[eng], 1)
        tok = (eng, self.esem[eng], self.ecnt[eng])
        for b in reads:
            b.r[eng] = tok
        for b in writes:
            b.w = tok
            b.r = {}
        return tok

    def dma(self, q, out, in_, owner, reads=(), writes=(), **kw):
        self._wait(q, reads, writes)
        qk = "sw" if q == "pool" else "hw"
        if qk not in owner.sems:
            if self.free_sems[qk]:
                owner.sems[qk] = self.free_sems[qk].pop()
            else:
                owner.sems[qk] = [self.es.enter_context(self.nc.semaphore("D" + qk + owner.name)), 0]
                self.dma_sems.append(owner.sems[qk])
            if self.in_phase:
                self.phase_sems.append((qk, owner.sems[qk]))
        sc = owner.sems[qk]
        ins = self.engs[q].dma_start(out=out, in_=in_, **kw)
        sc[1] += 16
        ins.then_inc(sc[0], 16)
        tok = ("dma:" + qk + owner.name, sc[0], sc[1])
        for b in reads:
            b.r[tok[0]] = tok
        for b in writes:
            b.w = tok
            b.r = {}
        return tok

    def phase_begin(self):
        self.in_phase = True
        self.phase_sems = []

    def phase_end(self):
        self.barrier()
        for qk, sc in self.phase_sems:
            self.free_sems[qk].append(sc)
        self.phase_sems = []
        self.in_phase = False

    def barrier(self):
        for e, h in self.engs.items():
            seen = self.seen[e]
            for e2 in self.engs:
                if e2 == e or self.ecnt[e2] == 0:
                    continue
                k = id(self.esem[e2])
                if seen.get(k, 0) < self.ecnt[e2]:
                    h.wait_ge(self.esem[e2], self.ecnt[e2])
                    seen[k] = self.ecnt[e2]
            for sc in self.dma_sems:
                k = id(sc[0])
                if seen.get(k, 0) < sc[1]:
                    h.wait_ge(sc[0], sc[1])
                    seen[k] = sc[1]

    def sb(self, es, name, shape, dt):
        self.nbuf += 1
        t = es.enter_context(self.nc.sbuf_tensor(f"s_{name}_{self.nbuf}", shape, dt))
        return t

    def dram(self, name, shape, dt, kind="Internal"):
        return self.nc.dram_tensor(name, shape, dt, kind=kind).ap()

    def build(self):
        nc = self.nc
        nl = self.nl
        es = self.es
        self.xT_in = self.dram("xT", [D, TOK], F32, "ExternalInput")
        self.normw_in = self.dram("normw", [128, 3 * L * 8 + 8], F32, "ExternalInput")
        self.wgu_in = self.dram("wgu", [nl * 2 * NFC * 128, 2 * 8 * 128], F32, "ExternalInput")
        self.wd_in = self.dram("wd", [nl * 2 * 8 * 128, NFC * 128], F32, "ExternalInput")
        self.winfm_in = self.dram("winfm", [nl * NFM * 128, 8 * 128], F32, "ExternalInput")
        self.wintm_in = self.dram("wintm", [nl * 128, 8 * NTM], F32, "ExternalInput")
        self.wbr_in = self.dram("wbr", [nl * 8 * 128, 8 * 128], F32, "ExternalInput")
        self.wout_in = self.dram("wout", [nl * 8 * 128, 8 * 128], F32, "ExternalInput")
        self.consts_in = self.dram("consts", [128, 6 * 128], F32, "ExternalInput")
        self.foxbf_in = self.dram("foxbf", [128, L * 6], F32, "ExternalInput")
        self.kvnorm_in = self.dram("kvnorm", [128, L], F32, "ExternalInput")
        self.wuk_in = self.dram("wuk", [L * 128, 64], F32, "ExternalInput")
        self.wuv_in = self.dram("wuv", [L * 128, 64], F32, "ExternalInput")
        self.rb31_in = self.dram("rb31", [1, 6 * S], F32, "ExternalInput")
        self.rbT_in = self.dram("rbT", [128, 2 * 6 * 128], F32, "ExternalInput")
        self.s5p_in = self.dram("s5p", [128, L * 8 * 3], F32, "ExternalInput")
        self.s5b_in = self.dram("s5b", [128, L * 8 * 2 * 16], F32, "ExternalInput")
        self.s5c_in = self.dram("s5c", [128, L * 8 * 2 * 16], F32, "ExternalInput")
        self.s5d_in = self.dram("s5d", [128, L * 2], F32, "ExternalInput")
        self.wglu_in = self.dram("wglu", [L * 128, 2 * 512], F32, "ExternalInput")
        self.out = self.dram("out", [D, TOK], F32, "ExternalOutput")
        self.wbr_b = self.dram("wbr_b", [nl * 8 * 128, 8 * 128], BF16)
        self.wout_b = self.dram("wout_b", [nl * 8 * 128, 8 * 128], BF16)
        self.d_br = self.dram("d_br", [8, 128, TOK], BF16, "ExternalOutput" if self.debug.get("dump") else "Internal")
        self.b_dbr = self.buf("dbr")
        self.xw = self.dram("xw", [D, TOK], F32)
        self.wgu_b = self.dram("wgu_b", [nl * 2 * NFC * 128, 2 * 8 * 128], BF16)
        self.wd_b = self.dram("wd_b", [nl * 2 * 8 * 128, NFC * 128], BF16)
        self.winfm_b = self.dram("winfm_b", [nl * NFM * 128, 8 * 128], BF16)
        self.wintm_b = self.dram("wintm_b", [nl * 128, 8 * NTM], BF16)
        dk = "ExternalOutput" if self.debug.get("dump") else "Internal"
        self.d_fm = self.dram("d_fm", [NFM, 128, TOK], BF16, dk)
        self.d_vc = self.dram("d_vc", [TOK, 384], BF16, dk)
        self.d_wf = self.dram("d_wf", [TOK, 16], F32, dk)
        self.b_xw = self.buf("xw")
        self.b_wcast = self.buf("wcast")
        self.b_dfm = self.buf("dfm")
        self.b_dtm = self.buf("dtm")

        self.normw = self.sb(es, "normw", [128, 3 * L * 8 + 8], F32)
        self.b_normw = self.buf("normw")
        self.ones_bf = self.sb(es, "ones_bf", [128, 128], BF16)
        self.b_ones = self.buf("ones")
        self.eps_t = self.sb(es, "eps_t", [128, 1], F32)
        self.I("dve", [], [self.b_ones], lambda: nc.vector.memset(self.ones_bf[:], 1.0))
        self.I("dve", [], [self.b_ones], lambda: nc.vector.memset(self.eps_t[:], EPS))
        self.dma("sp", self.normw[:], self.normw_in[:, :], self.b_normw, writes=[self.b_normw])
        self.b_cst = self.buf("cst")
        cf = self.sb(es, "constf", [128, 6 * 128], F32)
        self.dma("sp", cf[:], self.consts_in[:, :], self.b_cst, writes=[self.b_cst])
        self.ident_f = cf[:, 0:128]
        self.cneg_f = cf[:, 256:384]
        self.tri_f = cf[:, 384:512]
        self.ones_f = cf[:, 512:640]
        self.ident_bf = self.sb(es, "ident_bf", [128, 128], BF16)
        self.causneg_bf = self.sb(es, "causneg_bf", [128, 128], BF16)
        self.I3 = self.sb(es, "I3", [128, 384], BF16)
        self.one_t = self.sb(es, "one_t", [128, 1], F32)
        self.hpi_t = self.sb(es, "hpi_t", [128, 1], F32)
        self.I("dve", [self.b_cst], [self.b_cst], lambda: nc.vector.tensor_copy(out=self.ident_bf[:], in_=cf[:, 0:128]))
        self.I("dve", [self.b_cst], [self.b_cst], lambda: nc.vector.tensor_copy(out=self.causneg_bf[:], in_=cf[:, 128:256]))
        for k3 in range(3):
            self.I("dve", [self.b_cst], [self.b_cst],
                   lambda: nc.vector.tensor_copy(out=self.I3[:, k3 * 128:(k3 + 1) * 128], in_=cf[:, 0:128]))
        self.I("dve", [], [self.b_cst], lambda: nc.vector.memset(self.one_t[:], 1.0))
        self.I("dve", [], [self.b_cst], lambda: nc.vector.memset(self.hpi_t[:], math.pi / 2))
        self.foxbf = self.sb(es, "foxbf", [128, L * 6], F32)
        self.kvnorm = self.sb(es, "kvnorm", [128, L], F32)
        self.s5d = self.sb(es, "s5d", [128, L * 2], F32)
        self.biasT = self.sb(es, "biasT", [128, 2, 6, 128], BF16)
        self.dma("sp", self.foxbf[:], self.foxbf_in[:, :], self.b_cst, writes=[self.b_cst])
        self.dma("sp", self.kvnorm[:], self.kvnorm_in[:, :], self.b_cst, writes=[self.b_cst])
        self.dma("sp", self.s5d[:], self.s5d_in[:, :], self.b_cst, writes=[self.b_cst])
        self.dma("pool", self.biasT[:].rearrange("p a b c -> p (a b c)"), self.rbT_in[:, :], self.b_cst, writes=[self.b_cst])
        self.ps = []
        self.b_ps = []
        for i in range(8):
            self.ps.append(es.enter_context(nc.psum_tensor(f"ps{i}", [128, 512], F32)))
            self.b_ps.append(self.buf(f"ps{i}", psum=True))

        self.cast_weights()

        self.zero_dbr()
        for l in range(nl + 1):
            if self.debug.get('only_cast'):
                break
            self.phase_a(l)
            if self.debug.get("stop_after_a") == l:
                break
            if l < nl:
                self.phase_b(l)
        self.finish()
        return nc

    def cast_weights(self):
        def cast(dst, src, rows_per):
            n = dst.shape[0]
            for r0 in range(0, n, rows_per):
                r1 = min(n, r0 + rows_per)
                self.dma("pool", dst[r0:r1, :], src[r0:r1, :], self.b_wcast, writes=[self.b_wcast])
        nl = self.nl
        cast(self.wgu_b[0:nl * 2 * NFC * 128, :], self.wgu_in[0:nl * 2 * NFC * 128, :], 128)
        cast(self.wd_b[0:nl * 2 * 8 * 128, :], self.wd_in[0:nl * 2 * 8 * 128, :], 128)
        cast(self.winfm_b[0:nl * NFM * 128, :], self.winfm_in[0:nl * NFM * 128, :], 256)
        cast(self.wintm_b[0:nl * 128, :], self.wintm_in[0:nl * 128, :], 128)
        cast(self.wbr_b, self.wbr_in, 256)
        cast(self.wout_b, self.wout_in, 256)

    def zero_dbr(self):
        nc = self.nc
        self.phase_begin()
        with contextlib.ExitStack() as zes:
            z = self.sb(zes, "zt", [128, 4096], BF16)
            bz = self.buf("zt")
            self.I("dve", [], [bz], lambda: nc.vector.memset(z[:], 0.0))
            for c in range(8):
                for hh in range(2):
                    self.dma("sp", self.d_br[c, :, hh * 4096:(hh + 1) * 4096], z[:], bz, reads=[bz], writes=[self.b_dbr])
            self.phase_end()

    def phase_b(self, l):
        if self.debug.get("no_b"):
            return
        which = self.debug.get("branches", "sdf")
        if "s" in which:
            self.s5_phase(l)
        if "d" in which:
            self.dsa_phase(l)
        if "f" in which:
            self.fox_phase(l)

    def nseq(self):
        return self.debug.get("nseq", NSEQ)

    def s5_phase(self, l):
        nc = self.nc
        self.phase_begin()
        ps, b_ps = self.ps, self.b_ps
        B = self.buf
        LC = 512
        NCH = self.debug.get("nch", S // LC)
        with contextlib.ExitStack() as pes:
            prm = self.sb(pes, "prm", [128, 8, 3], F32)
            Bt = self.sb(pes, "Bt", [128, 8, 2, 16], F32)
            Ct = self.sb(pes, "Ct", [128, 8, 2, 16], F32)
            wglu = self.sb(pes, "wglu", [128, 2, 512], BF16)
            sm = self.sb(pes, "sm", [128, 20, 8], F32)
            tmp16 = self.sb(pes, "tmp16", [128, 16], F32)
            Mre = self.sb(pes, "Mre", [128, 8, 128], F32)
            Mim = self.sb(pes, "Mim", [128, 8, 128], F32)
            BbdT = self.sb(pes, "BbdT", [128, 8, 2, 128], BF16)
            Cbd = self.sb(pes, "Cbd", [128, 8, 2, 128], BF16)
            tabc = self.sb(pes, "tabc", [128, 8, LC], F32)
            tabs = self.sb(pes, "tabs", [128, 8, LC], F32)
            rB = self.sb(pes, "rB", [128, 8, LC], F32)
            tmpa = self.sb(pes, "tmpa", [128, LC], F32)
            tmpb = self.sb(pes, "tmpb", [128, LC], F32)
            b_set = B("s5set")
            b_wg = B("wglu")
            V = lambda k: sm[:, k, :]
            (DT, R, TH, C, Sn, CC, SS, SC, NR, NI, DEN, SRE, SIM, T1, T2, NSK, NSL) = range(17)
            lr, li, ldt = prm[:, :, 0], prm[:, :, 1], prm[:, :, 2]
            self.dma("sp", prm[:].rearrange("p a b -> p (a b)"), self.s5p_in[:, l * 24:(l + 1) * 24], b_set, writes=[b_set])
            self.dma("sp", Bt[:].rearrange("p a b c -> p (a b c)"), self.s5b_in[:, l * 256:(l + 1) * 256], b_set, writes=[b_set])
            self.dma("sp", Ct[:].rearrange("p a b c -> p (a b c)"), self.s5c_in[:, l * 256:(l + 1) * 256], b_set, writes=[b_set])
            self.dma("pool", wglu[:].rearrange("p k m -> p (k m)"), self.wglu_in[l * 128:(l + 1) * 128, :], b_wg, writes=[b_wg])

            def dv(fn):
                self.I("dve", [b_set, self.b_cst], [b_set], fn)

            def ac(fn):
                self.I("act", [b_set, self.b_cst], [b_set], fn)

            def tt(o, a, b, op):
                dv(lambda: nc.vector.tensor_tensor(out=o, in0=a, in1=b, op=op))

            ac(lambda: nc.scalar.activation(out=V(DT), in_=ldt, func=AF.Exp))
            tt(V(T1), lr, V(DT), ALU.mult)
            ac(lambda: nc.scalar.activation(out=V(R), in_=V(T1), func=AF.Exp))
            tt(V(TH), li, V(DT), ALU.mult)
            ac(lambda: nc.scalar.activation(out=V(Sn), in_=V(TH), func=AF.Sin, scale=1.0 / 16))
            ac(lambda: nc.scalar.activation(out=V(C), in_=V(TH), func=AF.Sin, scale=1.0 / 16, bias=self.hpi_t[:]))
            for _ in range(4):
                tt(V(CC), V(C), V(C), ALU.mult)
                tt(V(SS), V(Sn), V(Sn), ALU.mult)
                tt(V(SC), V(Sn), V(C), ALU.mult)
                tt(V(C), V(CC), V(SS), ALU.subtract)
                dv(lambda: nc.vector.tensor_scalar(out=V(Sn), in0=V(SC), scalar1=2.0, scalar2=None, op0=ALU.mult))
            tt(V(T1), V(R), V(C), ALU.mult)
            dv(lambda: nc.vector.tensor_scalar(out=V(NR), in0=V(T1), scalar1=-1.0, scalar2=None, op0=ALU.add))
            tt(V(NI), V(R), V(Sn), ALU.mult)
            tt(V(DEN), lr, lr, ALU.mult)
            tt(V(T1), li, li, ALU.mult)
            tt(V(DEN), V(DEN), V(T1), ALU.add)
            dv(lambda: nc.vector.reciprocal(out=V(DEN), in_=V(DEN)))
            tt(V(T1), V(NR), lr, ALU.mult)
            tt(V(T2), V(NI), li, ALU.mult)
            tt(V(T1), V(T1), V(T2), ALU.add)
            tt(V(SRE), V(T1), V(DEN), ALU.mult)
            tt(V(T1), V(NI), lr, ALU.mult)
            tt(V(T2), V(NR), li, ALU.mult)
            tt(V(T1), V(T1), V(T2), ALU.subtract)
            tt(V(SIM), V(T1), V(DEN), ALU.mult)
            dv(lambda: nc.vector.memset(Mre[:].rearrange("p a b -> p (a b)"), 0.0))
            dv(lambda: nc.vector.memset(Mim[:].rearrange("p a b -> p (a b)"), 0.0))
            dv(lambda: nc.vector.memset(Cbd[:].rearrange("p a b c -> p (a b c)"), 0.0))
            for sc in range(8):
                for hp in range(2):
                    P = slice(hp * 64, hp * 64 + 64)
                    col = ((2 * sc + hp) % 8) * 16
                    cs = slice(col, col + 16)
                    sre_c, sim_c = sm[P, SRE, sc:sc + 1], sm[P, SIM, sc:sc + 1]
                    dv(lambda: nc.vector.tensor_scalar(out=tmp16[P, :], in0=Bt[P, sc, 1, :], scalar1=sim_c, scalar2=None,
                                                       op0=ALU.mult))
                    dv(lambda: nc.vector.scalar_tensor_tensor(out=Mre[P, sc, cs], in0=Bt[P, sc, 0, :], scalar=sre_c,
                                                              in1=tmp16[P, :], op0=ALU.mult, op1=ALU.subtract))
                    dv(lambda: nc.vector.tensor_scalar(out=tmp16[P, :], in0=Bt[P, sc, 1, :], scalar1=sre_c, scalar2=None,
                                                       op0=ALU.mult))
                    dv(lambda: nc.vector.scalar_tensor_tensor(out=Mim[P, sc, cs], in0=Bt[P, sc, 0, :], scalar=sim_c,
                                                              in1=tmp16[P, :], op0=ALU.mult, op1=ALU.add))
                    dv(lambda: nc.vector.tensor_copy(out=Cbd[P, sc, 0, cs], in_=Ct[P, sc, 0, :]))
                    dv(lambda: nc.vector.tensor_scalar(out=Cbd[P, sc, 1, cs], in0=Ct[P, sc, 1, :], scalar1=-1.0,
                                                       scalar2=None, op0=ALU.mult))
            for sc in range(8):
                for ri, M in enumerate((Mre, Mim)):
                    pp, bp = ps[(sc * 2 + ri) % 4], b_ps[(sc * 2 + ri) % 4]
                    self.I("pe", [b_set, self.b_cst], [bp],
                           lambda: nc.tensor.transpose(out=pp[:, 0:128], in_=M[:, sc, :], identity=self.ident_f))
                    self.I("act", [bp, b_set], [b_set],
                           lambda: nc.scalar.copy(out=BbdT[:, sc, ri, :], in_=pp[:, 0:128]))
            dv(lambda: nc.vector.tensor_copy(out=tabc[:, :, 0], in_=V(C)))
            dv(lambda: nc.vector.tensor_copy(out=tabs[:, :, 0], in_=V(Sn)))
            k = 1
            while k < LC:
                dv(lambda: nc.vector.tensor_scalar(out=V(NSK), in0=tabs[:, :, k - 1], scalar1=-1.0, scalar2=None,
                                                   op0=ALU.mult))
                for sc in range(8):
                    ck, sk, nsk = tabc[:, sc, k - 1:k], tabs[:, sc, k - 1:k], sm[:, NSK, sc:sc + 1]
                    dv(lambda: nc.vector.tensor_scalar(out=tmpa[:, 0:k], in0=tabc[:, sc, 0:k], scalar1=ck, scalar2=None,
                                                       op0=ALU.mult))
                    dv(lambda: nc.vector.tensor_scalar(out=tmpb[:, 0:k], in0=tabs[:, sc, 0:k], scalar1=ck, scalar2=None,
                                                       op0=ALU.mult))
                    dv(lambda: nc.vector.scalar_tensor_tensor(out=tabc[:, sc, k:2 * k], in0=tabs[:, sc, 0:k], scalar=nsk,
                                                              in1=tmpa[:, 0:k], op0=ALU.mult, op1=ALU.add))
                    dv(lambda: nc.vector.scalar_tensor_tensor(out=tabs[:, sc, k:2 * k], in0=tabc[:, sc, 0:k], scalar=sk,
                                                              in1=tmpb[:, 0:k], op0=ALU.mult, op1=ALU.add))
                k *= 2
            dv(lambda: nc.vector.tensor_scalar(out=V(NSL), in0=tabs[:, :, LC - 1], scalar1=-1.0, scalar2=None, op0=ALU.mult))
            for sc in range(8):
                dv(lambda: nc.vector.tensor_scalar(out=rB[:, sc, :], in0=tabc[:, sc, :], scalar1=0.0,
                                                   scalar2=sm[:, R, sc:sc + 1], op0=ALU.mult, op1=ALU.add))

            uT = self.sb(pes, "uT", [128, 2, S], BF16)
            ast = self.sb(pes, "ast", [128, 2, S], BF16)
            carry = self.sb(pes, "carry", [128, 8, 2], F32)
            ctmp = self.sb(pes, "ctmp", [128, 2], F32)
            glT = self.sb(pes, "glT", [128, 2, LC], BF16)
            y2 = self.sb(pes, "y2", [128, LC], F32)
            x2 = self.sb(pes, "x2", [128, LC], F32)
            sgg = self.sb(pes, "sgg", [128, LC], F32)
            names = ("t1", "t2", "mre", "mim", "gre", "gim", "u1", "u2")
            W = {n: [self.sb(pes, f"{n}{i}", [128, LC], F32) for i in range(2)] for n in names}
            Hh = {n: [self.sb(pes, f"{n}{i}", [128, LC], BF16) for i in range(2)] for n in ("hre", "him")}
            bW = {n: [B(n) for _ in range(2)] for n in names + ("hre", "him")}
            b_uT, b_ast, b_carry, b_ctmp, b_glT, b_y2, b_x2, b_sgg = [B(n) for n in
                                                                     ("uT", "ast", "carry", "ctmp", "glT", "y2", "x2", "sgg")]
            for s in range(self.nseq()):
                t0 = s * S
                self.dma("sp", uT[:], self.d_fm[0:2, :, t0:t0 + S].rearrange("k p t -> p k t"), b_uT,
                         reads=[self.b_dfm], writes=[b_uT])
                self.I("dve", [], [b_carry], lambda: nc.vector.memset(carry[:].rearrange("p a b -> p (a b)"), 0.0))
                if NCH < S // LC:
                    self.I("dve", [], [b_ast], lambda: nc.vector.memset(ast[:].rearrange("p a b -> p (a b)"), 0.0))
                for c in range(NCH):
                    cs = slice(c * LC, (c + 1) * LC)
                    for cc in range(2):
                        py, bpy = ps[4 + cc], b_ps[4 + cc]
                        for kk, sc in enumerate(range(4 * cc, 4 * cc + 4)):
                            a = sc % 2
                            pbr, bpbr = ps[a * 2], b_ps[a * 2]
                            pbi, bpbi = ps[a * 2 + 1], b_ps[a * 2 + 1]
                            self.I("pe", [b_set, b_uT], [bpbr],
                                   lambda: nc.tensor.matmul(pbr[:], lhsT=BbdT[:, sc, 0, :], rhs=uT[:, cc, cs],
                                                            start=True, stop=True))
                            self.I("pe", [b_set, b_uT], [bpbi],
                                   lambda: nc.tensor.matmul(pbi[:], lhsT=BbdT[:, sc, 1, :], rhs=uT[:, cc, cs],
                                                            start=True, stop=True))
                            tc_, ts_ = tabc[:, sc, :], tabs[:, sc, :]
                            t1, t2, mre, mim = W["t1"][a], W["t2"][a], W["mre"][a], W["mim"][a]
                            gre, gim, u1, u2 = W["gre"][a], W["gim"][a], W["u1"][a], W["u2"][a]
                            hre, him = Hh["hre"][a], Hh["him"][a]

                            def TT(eng, reads, wr, o, x, y, op):
                                e = nc.vector if eng == "dve" else nc.gpsimd
                                self.I(eng, reads, wr, lambda: e.tensor_tensor(out=o, in0=x, in1=y, op=op))

                            TT("dve", [b_set, bpbr], [bW["t1"][a]], t1[:], tc_, pbr[:], ALU.mult)
                            TT("dve", [b_set, bpbi], [bW["t2"][a]], t2[:], ts_, pbi[:], ALU.mult)
                            TT("dve", [bW["t1"][a], bW["t2"][a]], [bW["mre"][a]], mre[:], t1[:], t2[:], ALU.add)
                            TT("dve", [b_set, bpbi], [bW["t1"][a]], t1[:], tc_, pbi[:], ALU.mult)
                            TT("dve", [b_set, bpbr], [bW["t2"][a]], t2[:], ts_, pbr[:], ALU.mult)
                            TT("dve", [bW["t1"][a], bW["t2"][a]], [bW["mim"][a]], mim[:], t1[:], t2[:], ALU.subtract)
                            self.I("dve", [b_set, bW["mre"][a], b_carry], [bW["gre"][a]],
                                   lambda: nc.vector.tensor_tensor_scan(out=gre[:], data0=rB[:, sc, :], data1=mre[:],
                                                                        initial=carry[:, sc, 0:1], op0=ALU.mult,
                                                                        op1=ALU.add))
                            self.I("dve", [b_set, bW["mim"][a], b_carry], [bW["gim"][a]],
                                   lambda: nc.vector.tensor_tensor_scan(out=gim[:], data0=rB[:, sc, :], data1=mim[:],
                                                                        initial=carry[:, sc, 1:2], op0=ALU.mult,
                                                                        op1=ALU.add))
                            cl, sl_, nsl = tabc[:, sc, LC - 1:LC], tabs[:, sc, LC - 1:LC], sm[:, NSL, sc:sc + 1]
                            self.I("dve", [bW["gre"][a], b_set], [b_ctmp],
                                   lambda: nc.vector.tensor_scalar(out=ctmp[:, 0:1], in0=gre[:, LC - 1:LC], scalar1=cl,
                                                                   scalar2=None, op0=ALU.mult))
                            self.I("dve", [bW["gre"][a], b_set], [b_ctmp],
                                   lambda: nc.vector.tensor_scalar(out=ctmp[:, 1:2], in0=gre[:, LC - 1:LC], scalar1=sl_,
                                                                   scalar2=None, op0=ALU.mult))
                            self.I("dve", [bW["gim"][a], b_set, b_ctmp], [b_carry],
                                   lambda: nc.vector.scalar_tensor_tensor(out=carry[:, sc, 0:1], in0=gim[:, LC - 1:LC],
                                                                          scalar=nsl, in1=ctmp[:, 0:1], op0=ALU.mult,
                                                                          op1=ALU.add))
                            self.I("dve", [bW["gim"][a], b_set, b_ctmp], [b_carry],
                                   lambda: nc.vector.scalar_tensor_tensor(out=carry[:, sc, 1:2], in0=gim[:, LC - 1:LC],
                                                                          scalar=cl, in1=ctmp[:, 1:2], op0=ALU.mult,
                                                                          op1=ALU.add))
                            TT("pool", [b_set, bW["gre"][a]], [bW["u1"][a]], u1[:], tc_, gre[:], ALU.mult)
                            TT("pool", [b_set, bW["gim"][a]], [bW["u2"][a]], u2[:], ts_, gim[:], ALU.mult)
                            TT("pool", [bW["u1"][a], bW["u2"][a]], [bW["hre"][a]], hre[:], u1[:], u2[:], ALU.subtract)
                            TT("pool", [b_set, bW["gre"][a]], [bW["u1"][a]], u1[:], ts_, gre[:], ALU.mult)
                            TT("pool", [b_set, bW["gim"][a]], [bW["u2"][a]], u2[:], tc_, gim[:], ALU.mult)
                            TT("pool", [bW["u1"][a], bW["u2"][a]], [bW["him"][a]], him[:], u1[:], u2[:], ALU.add)
                            self.I("pe", [b_set, bW["hre"][a]], [bpy],
                                   lambda: nc.tensor.matmul(py[:], lhsT=Cbd[:, sc, 0, :], rhs=hre[:], start=(kk == 0),
                                                            stop=False))
                            self.I("pe", [b_set, bW["him"][a]], [bpy],
                                   lambda: nc.tensor.matmul(py[:], lhsT=Cbd[:, sc, 1, :], rhs=him[:], start=False,
                                                            stop=(kk == 3)))
                        self.I("dve", [bpy, b_uT, self.b_cst], [b_y2],
                               lambda: nc.vector.scalar_tensor_tensor(out=y2[:], in0=uT[:, cc, cs],
                                                                      scalar=self.s5d[:, l * 2 + cc:l * 2 + cc + 1],
                                                                      in1=py[:], op0=ALU.mult, op1=ALU.add))
                        self.I("dve", [b_y2], [b_x2], lambda: nc.vector.tensor_tensor(out=x2[:], in0=y2[:], in1=y2[:], op=ALU.mult))
                        self.I("dve", [b_x2], [b_x2],
                               lambda: nc.vector.tensor_scalar(out=x2[:], in0=x2[:], scalar1=0.044715, scalar2=1.0,
                                                               op0=ALU.mult, op1=ALU.add))
                        self.I("dve", [b_x2, b_y2], [b_x2], lambda: nc.vector.tensor_tensor(out=x2[:], in0=x2[:], in1=y2[:], op=ALU.mult))
                        self.I("act", [b_x2], [b_sgg],
                               lambda: nc.scalar.activation(out=sgg[:], in_=x2[:], func=AF.Sigmoid, scale=2.0 * 0.7978845608028654))
                        self.I("dve", [b_sgg, b_y2], [b_glT],
                               lambda: nc.vector.tensor_tensor(out=glT[:, cc, :], in0=y2[:], in1=sgg[:], op=ALU.mult))
                    for o in range(2):
                        pg_, bpg = ps[6], b_ps[6]
                        pv_, bpv = ps[7], b_ps[7]
                        for kc in range(2):
                            self.I("pe", [b_wg, b_glT], [bpg],
                                   lambda: nc.tensor.matmul(pg_[:], lhsT=wglu[:, kc, (2 + o) * 128:(3 + o) * 128],
                                                            rhs=glT[:, kc, :], start=(kc == 0), stop=(kc == 1)))
                        for kc in range(2):
                            self.I("pe", [b_wg, b_glT], [bpv],
                                   lambda: nc.tensor.matmul(pv_[:], lhsT=wglu[:, kc, o * 128:(o + 1) * 128],
                                                            rhs=glT[:, kc, :], start=(kc == 0), stop=(kc == 1)))
                        self.I("act", [bpg], [b_sgg], lambda: nc.scalar.activation(out=sgg[:], in_=pg_[:], func=AF.Sigmoid))
                        self.I("dve", [bpv, b_sgg], [b_ast],
                               lambda: nc.vector.tensor_tensor(out=ast[:, o, cs], in0=sgg[:], in1=pv_[:], op=ALU.mult))
                self.dma("pool", self.d_br[0:2, :, t0:t0 + S].rearrange("k p t -> p k t"), ast[:], b_ast,
                         reads=[b_ast], writes=[self.b_dbr])
            self.phase_end()

    def fox_phase(self, l):
        nc = self.nc
        self.phase_begin()
        ps, b_ps = self.ps, self.b_ps
        B = self.buf
        NB = S // 128
        NJ = self.debug.get("nj", NB)
        with contextlib.ExitStack() as pes:
            wf = self.sb(pes, "wf", [128, NB, 16], F32)
            V = self.sb(pes, "fV", [128, NB, 384], BF16)
            qT = [self.sb(pes, f"fq{i}", [64, S], BF16) for i in range(2)]
            kT = [self.sb(pes, f"fk{i}", [64, S], BF16) for i in range(2)]
            cst = [self.sb(pes, f"fc{i}", [64, S], BF16) for i in range(2)]
            fb = self.sb(pes, "fb", [128, NB, 6], F32)
            tb = self.sb(pes, "ftb", [128, NB, 6], F32)
            offs = self.sb(pes, "foffs", [128, NB, 6], F32)
            cumT = self.sb(pes, "fcum", [128, NB, 6], F32)
            refB = self.sb(pes, "fref", [128, NB, 6], F32)
            biasJ = [self.sb(pes, f"fbj{i}", [128, NB], F32) for i in range(2)]
            PT = [self.sb(pes, f"fpt{i}", [128, 128], BF16) for i in range(3)]
            rec = [self.sb(pes, f"frec{i}", [64, 128], F32) for i in range(2)]
            b_wf, b_V, b_fb, b_tb, b_offs, b_cum, b_ref = B("wf"), B("V"), B("fb"), B("tb"), B("offs"), B("cum"), B("ref")
            b_qT = [B("q") for _ in range(2)]
            b_kT = [B("k") for _ in range(2)]
            b_cst = [B("c") for _ in range(2)]
            b_bj = [B("bj") for _ in range(2)]
            b_PT = [B("pt") for _ in range(3)]
            b_rec = [B("rec") for _ in range(2)]
            cnt = 0
            if NJ < NB:
                for i2 in range(2):
                    self.I("dve", [], [b_cst[i2]], lambda: nc.vector.memset(cst[i2][:], 0.0))
            for s in range(self.nseq()):
                t0 = s * S
                self.dma("sp", wf[:], self.d_wf[t0:t0 + S, :].rearrange("(n p) c -> p n c", p=128), b_wf,
                         reads=[self.b_dtm], writes=[b_wf])
                self.dma("sp", V[:], self.d_vc[t0:t0 + S, :].rearrange("(n p) c -> p n c", p=128), b_V,
                         reads=[self.b_dtm], writes=[b_V])
                for h in range(6):
                    self.I("dve", [b_wf, self.b_cst], [b_fb],
                           lambda: nc.vector.tensor_scalar(out=fb[:, :, h], in0=wf[:, :, 4 + h],
                                                           scalar1=self.foxbf[:, l * 6 + h:l * 6 + h + 1],
                                                           scalar2=None, op0=ALU.add))
                self.I("act", [b_fb], [b_fb], lambda: nc.scalar.activation(out=FL(fb), in_=FL(fb), func=AF.Exp, scale=-1.0))
                self.I("act", [b_fb, self.b_cst], [b_fb],
                       lambda: nc.scalar.activation(out=FL(fb), in_=FL(fb), func=AF.Ln, bias=self.one_t[:], scale=1.0))
                self.I("pe", [b_fb, self.b_cst], [b_ps[0]],
                       lambda: nc.tensor.matmul(ps[0][:, 0:NB * 6], lhsT=self.tri_f[:], rhs=FL(fb), start=True, stop=True))
                self.I("pe", [b_fb, self.b_cst], [b_ps[1]],
                       lambda: nc.tensor.matmul(ps[1][:, 0:NB * 6], lhsT=self.ones_f[:], rhs=FL(fb), start=True, stop=True))
                self.I("dve", [b_ps[1]], [b_tb], lambda: nc.vector.tensor_copy(out=FL(tb), in_=ps[1][:, 0:NB * 6]))
                self.I("dve", [], [b_offs], lambda: nc.vector.memset(offs[:, 0, :], 0.0))
                for b in range(1, NB):
                    self.I("dve", [b_offs, b_tb], [b_offs],
                           lambda: nc.vector.tensor_tensor(out=offs[:, b, :], in0=offs[:, b - 1, :], in1=tb[:, b - 1, :],
                                                           op=ALU.add))
                self.I("dve", [b_ps[0], b_offs], [b_cum],
                       lambda: nc.vector.tensor_tensor(out=FL(cumT), in0=FL(offs), in1=ps[0][:, 0:NB * 6], op=ALU.add))
                self.I("dve", [b_offs, b_tb], [b_ref],
                       lambda: nc.vector.tensor_tensor(out=FL(refB), in0=FL(offs), in1=FL(tb), op=ALU.add))
                for h in range(6):
                    sl = h % 2
                    r0 = (h % 2) * 64
                    self.dma("sp", qT[sl][:], self.d_fm[9 + h // 2, r0:r0 + 64, t0:t0 + S], b_qT[sl],
                             reads=[self.b_dfm], writes=[b_qT[sl]])
                    self.dma("sp", kT[sl][:], self.d_fm[12 + h // 2, r0:r0 + 64, t0:t0 + S], b_kT[sl],
                             reads=[self.b_dfm], writes=[b_kT[sl]])
                    for j in range(NJ):
                        bj, b_bjj = biasJ[j % 2], b_bj[j % 2]
                        self.I("dve", [b_cum, b_ref], [b_bjj],
                               lambda: nc.vector.tensor_scalar(out=bj[:, 0:j + 1], in0=cumT[:, 0:j + 1, h],
                                                               scalar1=refB[:, j, h:h + 1], scalar2=None,
                                                               op0=ALU.subtract))
                        po, bpo = ps[2 + (j % 2) * 2], b_ps[2 + (j % 2) * 2]
                        pso, bpso = ps[3 + (j % 2) * 2], b_ps[3 + (j % 2) * 2]
                        js = slice(j * 128, (j + 1) * 128)
                        def fqk(i):
                            nonlocal cnt
                            cnt += 1
                            pq, bpq = ps[6 + cnt % 2], b_ps[6 + cnt % 2]
                            pt, bpt = PT[cnt % 3], b_PT[cnt % 3]
                            isl = slice(i * 128, (i + 1) * 128)
                            self.I("pe", [b_kT[sl], b_qT[sl]], [bpq],
                                   lambda: nc.tensor.matmul(pq[:, 0:128], lhsT=kT[sl][:, isl], rhs=qT[sl][:, js],
                                                            start=True, stop=(i != j)))
                            if i == j:
                                self.I("pe", [self.b_cst], [bpq],
                                       lambda: nc.tensor.matmul(pq[:, 0:128], lhsT=self.ident_bf[:], rhs=self.causneg_bf[:],
                                                                start=False, stop=True))
                            return pq, bpq, pt, bpt

                        nxq = fqk(0)
                        for i in range(j + 1):
                            pq, bpq, pt, bpt = nxq
                            if i + 1 <= j:
                                nxq = fqk(i + 1)
                            self.I("act", [bpq, b_bjj], [bpt],
                                   lambda: nc.scalar.activation(out=pt[:], in_=pq[:, 0:128], func=AF.Exp,
                                                                bias=bj[:, i:i + 1], scale=0.125))
                            self.I("pe", [b_V, bpt], [bpo],
                                   lambda: nc.tensor.matmul(po[0:64, 0:128], lhsT=V[:, i, h * 64:(h + 1) * 64], rhs=pt[:],
                                                            start=(i == 0), stop=(i == j)))
                            self.I("pe", [self.b_ones, bpt], [bpso],
                                   lambda: nc.tensor.matmul(pso[0:64, 0:128], lhsT=self.ones_bf[:, 0:64], rhs=pt[:],
                                                            start=(i == 0), stop=(i == j)))
                        rc, brc = rec[j % 2], b_rec[j % 2]
                        self.I("dve", [bpso], [brc], lambda: nc.vector.reciprocal(out=rc[:], in_=pso[0:64, 0:128]))
                        self.I("dve", [bpo, brc], [b_cst[sl]],
                               lambda: nc.vector.tensor_tensor(out=cst[sl][:, js], in0=rc[:], in1=po[0:64, 0:128],
                                                               op=ALU.mult))
                    self.dma("pool", self.d_br[5 + h // 2, r0:r0 + 64, t0:t0 + S], cst[sl][:], b_cst[sl],
                             reads=[b_cst[sl]], writes=[self.b_dbr])
            self.phase_end()

    def dsa_phase(self, l):
        nc = self.nc
        self.phase_begin()
        ps, b_ps = self.ps, self.b_ps
        B = self.buf
        NB = S // 128
        NJ = self.debug.get("nj", NB)
        NR = self.debug.get("nrounds", 32)
        with contextlib.ExitStack() as pes:
            wf = self.sb(pes, "dwf", [128, NB, 16], F32)
            ckv = self.sb(pes, "ckv", [128, S], BF16)
            cn = self.sb(pes, "cn", [128, S], BF16)
            sq = [self.sb(pes, f"dsq{i}", [128, 512], BF16) for i in range(2)]
            rs = [self.sb(pes, f"drs{i}", [128, 512], F32) for i in range(2)]
            Kp = self.sb(pes, "Kp", [65, S], BF16)
            Vd = self.sb(pes, "Vd", [128, NB, 64], BF16)
            Qp = self.sb(pes, "Qp", [65, 6, S], BF16)
            qi = self.sb(pes, "qi", [64, 4, S], BF16)
            ki = self.sb(pes, "ki", [64, S], BF16)
            wuk = self.sb(pes, "wuk", [128, 64], BF16)
            wuv = self.sb(pes, "wuv", [128, 64], BF16)
            score = self.sb(pes, "score", [128, S], F32)
            work = self.sb(pes, "work", [128, S], F32)
            rr = [self.sb(pes, f"rr{i}", [128, 512], F32) for i in range(2)]
            m8 = [self.sb(pes, f"m8{i}", [128, 8], F32) for i in range(2)]
            thr0 = self.sb(pes, "thr0", [128, 1], F32)
            negm = [self.sb(pes, f"negm{i}", [128, S], BF16) for i in range(2)]
            PT = [self.sb(pes, f"dpt{i}", [128, 384], BF16) for i in range(3)]
            rec = [self.sb(pes, f"drec{i}", [64, 384], F32) for i in range(2)]
            bst = self.sb(pes, "bst", [64, NB, 6, 128], BF16)
            (b_wf, b_ckv, b_cn, b_Kp, b_Vd, b_Qp, b_qi, b_ki, b_w, b_score, b_work, b_thr0, b_bst) = [
                B(n) for n in ("wf", "ckv", "cn", "Kp", "Vd", "Qp", "qi", "ki", "w", "score", "work", "thr0", "bst")]
            b_sq = [B("sq") for _ in range(2)]
            b_rs = [B("rs") for _ in range(2)]
            b_rr = [B("rr") for _ in range(2)]
            b_m8 = [B("m8") for _ in range(2)]
            b_negm = [B("negm") for _ in range(2)]
            b_PT = [B("pt") for _ in range(3)]
            b_rec = [B("rec") for _ in range(2)]
            self.dma("pool", wuk[:], self.wuk_in[l * 128:(l + 1) * 128, :], b_w, writes=[b_w])
            self.dma("pool", wuv[:], self.wuv_in[l * 128:(l + 1) * 128, :], b_w, writes=[b_w])
            self.I("dve", [], [b_Kp], lambda: nc.vector.memset(Kp[64:65, :], 1.0))
            self.I("dve", [], [b_thr0], lambda: nc.vector.memset(thr0[:], -1e29))
            if NJ < NB:
                self.I("dve", [], [b_bst], lambda: nc.vector.memset(bst[:].rearrange("p a b c -> p (a b c)"), 0.0))
            self.dma("pool", Qp[64:65, :, :], self.rb31_in[0:1, :].rearrange("o (h t) -> o h t", h=6), b_Qp, writes=[b_Qp])
            cnt = 0
            for s in range(self.nseq()):
                t0 = s * S
                self.dma("sp", wf[:], self.d_wf[t0:t0 + S, :].rearrange("(n p) c -> p n c", p=128), b_wf,
                         reads=[self.b_dtm], writes=[b_wf])
                self.dma("sp", ckv[:], self.d_fm[5, :, t0:t0 + S], b_ckv, reads=[self.b_dfm], writes=[b_ckv])
                for h in range(6):
                    r0 = (h % 2) * 64
                    self.dma("sp", Qp[0:64, h, :], self.d_fm[2 + h // 2, r0:r0 + 64, t0:t0 + S], b_Qp,
                             reads=[self.b_dfm], writes=[b_Qp])
                for h in range(4):
                    r0 = (h % 2) * 64
                    self.dma("sp", qi[:, h, :], self.d_fm[6 + h // 2, r0:r0 + 64, t0:t0 + S], b_qi,
                             reads=[self.b_dfm], writes=[b_qi])
                self.dma("sp", ki[:], self.d_fm[8, 0:64, t0:t0 + S], b_ki, reads=[self.b_dfm], writes=[b_ki])
                for c4 in range(4):
                    cs = slice(c4 * 512, (c4 + 1) * 512)
                    i2 = c4 % 2
                    pp, bp = ps[i2], b_ps[i2]
                    self.I("act", [b_ckv], [b_sq[i2]],
                           lambda: nc.scalar.activation(out=sq[i2][:], in_=ckv[:, cs], func=AF.Square))
                    self.I("pe", [b_sq[i2], self.b_ones], [bp],
                           lambda: nc.tensor.matmul(pp[:], lhsT=self.ones_bf[:], rhs=sq[i2][:], start=True, stop=True))
                    self.I("act", [bp, self.b_ones], [b_rs[i2]],
                           lambda: nc.scalar.activation(out=rs[i2][:], in_=pp[:], func=AF.Sqrt, bias=self.eps_t[:],
                                                        scale=1.0 / 128))
                    self.I("dve", [b_rs[i2]], [b_rs[i2]], lambda: nc.vector.reciprocal(out=rs[i2][:], in_=rs[i2][:]))
                    self.I("dve", [b_ckv, b_rs[i2], self.b_cst], [b_cn],
                           lambda: nc.vector.scalar_tensor_tensor(out=cn[:, cs], in0=ckv[:, cs],
                                                                  scalar=self.kvnorm[:, l:l + 1], in1=rs[i2][:],
                                                                  op0=ALU.mult, op1=ALU.mult))
                for c4 in range(4):
                    cs = slice(c4 * 512, (c4 + 1) * 512)
                    pp, bp = ps[2 + c4 % 2], b_ps[2 + c4 % 2]
                    self.I("pe", [b_w, b_cn], [bp],
                           lambda: nc.tensor.matmul(pp[0:64, :], lhsT=wuk[:], rhs=cn[:, cs], start=True, stop=True))
                    self.I("act", [bp], [b_Kp], lambda: nc.scalar.copy(out=Kp[0:64, cs], in_=pp[0:64, :]))
                for blk in range(NB):
                    pp, bp = ps[4 + blk % 2], b_ps[4 + blk % 2]
                    self.I("pe", [b_w, b_cn], [bp],
                           lambda: nc.tensor.matmul(pp[:, 0:64], lhsT=cn[:, blk * 128:(blk + 1) * 128], rhs=wuv[:],
                                                    start=True, stop=True))
                    self.I("dve", [bp], [b_Vd], lambda: nc.vector.tensor_copy(out=Vd[:, blk, :], in_=pp[:, 0:64]))
                cntl = [0]

                def front(j):
                    nk = (j + 1) * 128
                    js = slice(j * 128, (j + 1) * 128)
                    for h in range(4):
                        for c0 in range(0, nk, 512):
                            w = min(512, nk - c0)
                            cntl[0] += 1
                            cnt = cntl[0]
                            pi, bpi = ps[cnt % 2], b_ps[cnt % 2]
                            rt, brt = rr[cnt % 2], b_rr[cnt % 2]
                            self.I("pe", [b_qi, b_ki], [bpi],
                                   lambda: nc.tensor.matmul(pi[:, 0:w], lhsT=qi[:, h, js], rhs=ki[:, c0:c0 + w],
                                                            start=True, stop=True))
                            self.I("act", [bpi], [brt],
                                   lambda: nc.scalar.activation(out=rt[:, 0:w], in_=pi[:, 0:w], func=AF.Relu))
                            if h == 0:
                                self.I("dve", [brt, b_wf], [b_score],
                                       lambda: nc.vector.tensor_scalar(out=score[:, c0:c0 + w], in0=rt[:, 0:w],
                                                                       scalar1=wf[:, j, 0:1], scalar2=None, op0=ALU.mult))
                            else:
                                self.I("dve", [brt, b_wf, b_score], [b_score],
                                       lambda: nc.vector.scalar_tensor_tensor(
                                           out=score[:, c0:c0 + w], in0=rt[:, 0:w], scalar=wf[:, j, h:h + 1],
                                           in1=score[:, c0:c0 + w], op0=ALU.mult, op1=ALU.add))
                    self.I("dve", [b_score, self.b_cst], [b_score],
                           lambda: nc.vector.tensor_tensor(out=score[:, js], in0=score[:, js], in1=self.cneg_f[:],
                                                           op=ALU.add))
                    if j >= 2:
                        src = score
                        for r in range(NR):
                            m, bm = m8[r % 2], b_m8[r % 2]
                            self.I("dve", [b_score, b_work], [bm], lambda: nc.vector.max(out=m[:], in_=src[:, 0:nk]))
                            if r < NR - 1:
                                self.I("dve", [b_score, b_work, bm], [b_work],
                                       lambda: nc.vector.match_replace(out=work[:, 0:nk], in_to_replace=m[:],
                                                                       in_values=src[:, 0:nk], imm_value=-3.0e38))
                                src = work
                        thr, bthr = m[:, 7:8], bm
                    else:
                        thr, bthr = thr0[:], b_thr0
                    ng, bng = negm[j % 2], b_negm[j % 2]
                    self.I("dve", [b_score, bthr], [bng],
                           lambda: nc.vector.tensor_scalar(out=ng[:, 0:nk], in0=score[:, 0:nk], scalar1=thr,
                                                           scalar2=-30000.0, op0=ALU.is_lt, op1=ALU.mult))
                    return ng, bng

                def back(j, ng, bng):
                    js = slice(j * 128, (j + 1) * 128)
                    for hf in range(2):
                        po, bpo = ps[4 + hf], b_ps[4 + hf]
                        pso, bpso = ps[6 + hf], b_ps[6 + hf]
                        hs = slice(3 * hf, 3 * hf + 3)
                        def dqk(i):
                            cntl[0] += 1
                            cnt = cntl[0]
                            pq, bpq = ps[2 + cnt % 2], b_ps[2 + cnt % 2]
                            pt, bpt = PT[cnt % 3], b_PT[cnt % 3]
                            isl = slice(i * 128, (i + 1) * 128)
                            far = (j - i) >= 2
                            kr = 65 if far else 64
                            self.I("pe", [b_Kp, b_Qp], [bpq],
                                   lambda: nc.tensor.matmul(pq[:, 0:384], lhsT=Kp[0:kr, isl], rhs=Qp[0:kr, hs, js],
                                                            start=True, stop=False))
                            if not far:
                                self.I("pe", [self.b_cst], [bpq],
                                       lambda: nc.tensor.matmul(pq[:, 0:384], lhsT=self.ident_bf[:],
                                                                rhs=self.biasT[:, j - i, hs, :], start=False, stop=False))
                            self.I("pe", [bng, self.b_cst], [bpq],
                                   lambda: nc.tensor.matmul(pq[:, 0:384], lhsT=ng[:, isl], rhs=self.I3[:],
                                                            start=False, stop=True))
                            return pq, bpq, pt, bpt

                        nxq = dqk(0)
                        for i in range(j + 1):
                            pq, bpq, pt, bpt = nxq
                            if i + 1 <= j:
                                nxq = dqk(i + 1)
                            self.I("act", [bpq], [bpt],
                                   lambda: nc.scalar.activation(out=pt[:], in_=pq[:, 0:384], func=AF.Exp))
                            self.I("pe", [b_Vd, bpt], [bpo],
                                   lambda: nc.tensor.matmul(po[0:64, 0:384], lhsT=Vd[:, i, :], rhs=pt[:],
                                                            start=(i == 0), stop=(i == j)))
                            self.I("pe", [self.b_ones, bpt], [bpso],
                                   lambda: nc.tensor.matmul(pso[0:64, 0:384], lhsT=self.ones_bf[:, 0:64], rhs=pt[:],
                                                            start=(i == 0), stop=(i == j)))
                        rc, brc = rec[hf], b_rec[hf]
                        self.I("dve", [bpso], [brc], lambda: nc.vector.reciprocal(out=rc[:], in_=pso[0:64, 0:384]))
                        self.I("dve", [bpo, brc], [b_bst],
                               lambda: nc.vector.tensor_tensor(out=bst[:, j, hs, :].rearrange("p a b -> p (a b)"), in0=rc[:], in1=po[0:64, 0:384],
                                                               op=ALU.mult))

                nxt = front(0)
                for j in range(NJ):
                    cur = nxt
                    if j + 1 < NJ:
                        nxt = front(j + 1)
                    back(j, *cur)
                for h in range(6):
                    r0 = (h % 2) * 64
                    self.dma("pool", self.d_br[2 + h // 2, r0:r0 + 64, t0:t0 + S].rearrange("p (n t) -> p n t", t=128), bst[:, :, h, :], b_bst,
                             reads=[b_bst], writes=[self.b_dbr])
            self.phase_end()

    def phase_a(self, l):
        nc = self.nc
        pes = contextlib.ExitStack()
        self.phase_begin()
        with pes:
            xb = self.sb(pes, "xb", [128, 8, TB], F32)
            hT = self.sb(pes, "hT", [128, 8, TB], BF16)
            actT = self.sb(pes, "actT", [128, NFC, TB], BF16)
            wgu = [self.sb(pes, f"wgu{i}", [128, 2, 8, 128], BF16) for i in range(2)]
            wd = [self.sb(pes, f"wd{i}", [128, NFC, 128], BF16) for i in range(2)]
            wfm = [self.sb(pes, f"wfm{i}", [128, 8, 128], BF16) for i in range(3)]
            wtm = self.sb(pes, "wtm", [128, 8, NTM], BF16)
            sq = [self.sb(pes, f"sq{i}", [128, 512], BF16) for i in range(2)]
            rs = [self.sb(pes, f"rs{i}", [128, 512], F32) for i in range(2)]
            sg = [self.sb(pes, f"sg{i}", [128, 512], F32) for i in range(2)]
            stg = [self.sb(pes, f"stg{i}", [128, TB], BF16) for i in range(4)]
            stv = self.sb(pes, "stv", [128, 8, 384], BF16)
            stw = self.sb(pes, "stw", [128, 8, 16], F32)
            B = self.buf
            b_xb = [B("xb") for _ in range(8)]
            b_hT = [B("hT") for _ in range(8)]
            b_actT = [B("actT") for _ in range(NFC)]
            b_wgu = [B("wgu") for _ in range(2)]
            b_wd = [B("wd") for _ in range(2)]
            b_wfm = [B("wfm") for _ in range(3)]
            b_wtm = B("wtm")
            b_sq = [B("sq") for _ in range(2)]
            b_rs = [B("rs") for _ in range(2)]
            b_sg = [B("sg") for _ in range(2)]
            b_stg = [B("stg") for _ in range(4)]
            b_stv = B("stv")
            b_stw = B("stw")
            ps, b_ps = self.ps, self.b_ps
            self.I("dve", [], [b_stw], lambda: nc.vector.memset(stw[:], 0.0))
            NFCL = self.debug.get('nfc_lim', NFC)

            def nw(g, ll, kc):
                c = (g * L + ll) * 8 + kc
                return self.normw[:, c:c + 1]

            def norm(g, ll, final=False):
                for tg in range(2):
                    cs = slice(tg * 512, (tg + 1) * 512)
                    ssp, b_ss = ps[6 + tg], b_ps[6 + tg]
                    for kc in range(8):
                        i = kc % 2
                        self.I("act", [b_xb[kc]], [b_sq[i]],
                               lambda: nc.scalar.activation(out=sq[i][:], in_=xb[:, kc, cs], func=AF.Square))
                        self.I("pe", [b_sq[i], self.b_ones], [b_ss],
                               lambda: nc.tensor.matmul(ssp[:], lhsT=self.ones_bf[:], rhs=sq[i][:],
                                                        start=(kc == 0), stop=(kc == 7)))
                    self.I("act", [b_ss, self.b_ones], [b_rs[tg]],
                           lambda: nc.scalar.activation(out=rs[tg][:], in_=ssp[:], func=AF.Sqrt,
                                                        bias=self.eps_t[:], scale=1.0 / D))
                    self.I("dve", [b_rs[tg]], [b_rs[tg]],
                           lambda: nc.vector.reciprocal(out=rs[tg][:], in_=rs[tg][:]))
                    for kc in range(8):
                        if final:
                            self.I("dve", [b_xb[kc], b_rs[tg], self.b_normw], [b_xb[kc]],
                                   lambda: nc.vector.scalar_tensor_tensor(
                                       out=xb[:, kc, cs], in0=xb[:, kc, cs],
                                       scalar=self.normw[:, 3 * L * 8 + kc:3 * L * 8 + kc + 1],
                                       in1=rs[tg][:], op0=ALU.mult, op1=ALU.mult))
                        else:
                            self.I("dve", [b_xb[kc], b_rs[tg], self.b_normw], [b_hT[kc]],
                                   lambda: nc.vector.scalar_tensor_tensor(
                                       out=hT[:, kc, cs], in0=xb[:, kc, cs], scalar=nw(g, ll, kc),
                                       in1=rs[tg][:], op0=ALU.mult, op1=ALU.mult))

            def load_wgu(ll, j, fc):
                r0 = ((ll * 2 + j) * NFC + fc) * 128
                i = fc % 2
                self.dma("sp", wgu[i][:], self.wgu_b[r0:r0 + 128, :].rearrange("p (a k f) -> p a k f", a=2, k=8),
                         b_wgu[i], reads=[self.b_wcast], writes=[b_wgu[i]])

            def load_wd(ll, j, dc):
                r0 = ((ll * 2 + j) * 8 + dc) * 128
                i = dc % 2
                self.dma("sp", wd[i][:], self.wd_b[r0:r0 + 128, :].rearrange("p (c d) -> p c d", c=NFC),
                         b_wd[i], reads=[self.b_wcast], writes=[b_wd[i]])

            def ffn(ll, j, g):
                load_wgu(ll, j, 0)
                norm(g, ll)
                for fc in range(NFCL):
                    if fc + 1 < NFCL:
                        load_wgu(ll, j, fc + 1)
                    else:
                        load_wd(ll, j, 0)
                    i = fc % 2
                    for tg in range(2):
                        cs = slice(tg * 512, (tg + 1) * 512)
                        pg, bg = ps[tg * 2], b_ps[tg * 2]
                        pu, bu = ps[tg * 2 + 1], b_ps[tg * 2 + 1]
                        for kc in range(8):
                            self.I("pe", [b_wgu[i], b_hT[kc]], [bg],
                                   lambda: nc.tensor.matmul(pg[:], lhsT=wgu[i][:, 0, kc, :], rhs=hT[:, kc, cs],
                                                            start=(kc == 0), stop=(kc == 7)))
                        for kc in range(8):
                            self.I("pe", [b_wgu[i], b_hT[kc]], [bu],
                                   lambda: nc.tensor.matmul(pu[:], lhsT=wgu[i][:, 1, kc, :], rhs=hT[:, kc, cs],
                                                            start=(kc == 0), stop=(kc == 7)))
                        self.I("act", [bg], [b_sg[tg]],
                               lambda: nc.scalar.activation(out=sg[tg][:], in_=pg[:], func=AF.Silu))
                        self.I("dve", [b_sg[tg], bu], [b_actT[fc]],
                               lambda: nc.vector.tensor_tensor(out=actT[:, fc, cs], in0=sg[tg][:], in1=pu[:],
                                                               op=ALU.mult))
                for dc in range(8):
                    if dc + 1 < 8:
                        load_wd(ll, j, dc + 1)
                    i = dc % 2
                    for tg in range(2):
                        cs = slice(tg * 512, (tg + 1) * 512)
                        pd, bd = ps[4 + tg], b_ps[4 + tg]
                        for fc in range(NFCL):
                            self.I("pe", [b_wd[i], b_actT[fc]], [bd],
                                   lambda: nc.tensor.matmul(pd[:], lhsT=wd[i][:, fc, :], rhs=actT[:, fc, cs],
                                                            start=(fc == 0), stop=(fc == NFCL - 1)))
                        self.I("dve", [bd, b_xb[dc]], [b_xb[dc]],
                               lambda: nc.vector.scalar_tensor_tensor(
                                   out=xb[:, dc, cs], in0=pd[:], scalar=0.5, in1=xb[:, dc, cs],
                                   op0=ALU.mult, op1=ALU.add))

            def load_wfm(ll, c, slot):
                r0 = (ll * NFM + c) * 128
                self.dma("sp", wfm[slot][:], self.winfm_b[r0:r0 + 128, :].rearrange("p (k m) -> p k m", k=8),
                         b_wfm[slot], reads=[self.b_wcast], writes=[b_wfm[slot]])

            def proj(ll, tb):
                t0 = tb * TB
                self.dma("sp", wtm[:], self.wintm_b[ll * 128:(ll + 1) * 128, :].rearrange("p (k m) -> p k m", k=8),
                         b_wtm, reads=[self.b_wcast], writes=[b_wtm])
                load_wfm(ll, 0, 0)
                load_wfm(ll, 1, 1)
                norm(1, ll)
                for c in range(NFM):
                    if c + 2 < NFM:
                        load_wfm(ll, c + 2, (c + 2) % 3)
                    slot = c % 3
                    si = c % 4
                    is_gate = c >= 15
                    for tg in range(2):
                        cs = slice(tg * 512, (tg + 1) * 512)
                        pp, bp = ps[(c * 2 + tg) % 4], b_ps[(c * 2 + tg) % 4]
                        for kc in range(8):
                            self.I("pe", [b_wfm[slot], b_hT[kc]], [bp],
                                   lambda: nc.tensor.matmul(pp[:], lhsT=wfm[slot][:, kc, :], rhs=hT[:, kc, cs],
                                                            start=(kc == 0), stop=(kc == 7)))
                        if is_gate:
                            self.I("act", [bp], [b_stg[si]],
                                   lambda: nc.scalar.activation(out=stg[si][:, cs], in_=pp[:], func=AF.Sigmoid))
                        elif c in (2, 3, 4):
                            self.I("act", [bp], [b_stg[si]],
                                   lambda: nc.scalar.mul(out=stg[si][:, cs], in_=pp[:], mul=0.125))
                        elif (c + tg) % 2 == 0:
                            self.I("act", [bp], [b_stg[si]],
                                   lambda: nc.scalar.copy(out=stg[si][:, cs], in_=pp[:]))
                        else:
                            self.I("dve", [bp], [b_stg[si]],
                                   lambda: nc.vector.tensor_copy(out=stg[si][:, cs], in_=pp[:]))
                    self.dma("pool", self.d_fm[c, :, t0:t0 + TB], stg[si][:], b_stg[si],
                             reads=[b_stg[si]], writes=[self.b_dfm])
                for tt in range(0 if self.debug.get('no_tm') else 8):
                    pp, bp = ps[4 + tt % 2], b_ps[4 + tt % 2]
                    for kc in range(8):
                        self.I("pe", [b_wtm, b_hT[kc]], [bp],
                               lambda: nc.tensor.matmul(pp[:, 0:NTM], lhsT=hT[:, kc, tt * 128:(tt + 1) * 128],
                                                        rhs=wtm[:, kc, :], start=(kc == 0), stop=(kc == 7)))
                    self.I("act", [bp], [b_stv],
                           lambda: nc.scalar.copy(out=stv[:, tt, :], in_=pp[:, 0:384]))
                    self.I("dve", [bp], [b_stw],
                           lambda: nc.vector.tensor_copy(out=stw[:, tt, 0:10], in_=pp[:, 384:394]))
                if self.debug.get('no_tm'):
                    return
                self.dma("pool", self.d_vc[t0:t0 + TB, :].rearrange("(n p) c -> p n c", p=128), stv[:], b_stv,
                         reads=[b_stv], writes=[self.b_dtm])
                if self.debug.get('no_wf'):
                    return
                self.dma("pool", self.d_wf[t0:t0 + TB, :].rearrange("(n p) c -> p n c", p=128), stw[:], b_stw,
                         reads=[b_stw], writes=[self.b_dtm])

            def load_w8(src, ll, oc, slot):
                r0 = (ll * 8 + oc) * 128
                self.dma("sp", wfm[slot][:], src[r0:r0 + 128, :].rearrange("p (k m) -> p k m", k=8),
                         b_wfm[slot], reads=[self.b_wcast], writes=[b_wfm[slot]])

            KCS = [(0, 2), (2, 5), (5, 8)]

            def merge(ll, tb):
                t0 = tb * TB
                self.dma("sp", hT[:], self.d_br[:, :, t0:t0 + TB].rearrange("k p t -> p k t"), b_hT[0],
                         reads=[self.b_dbr], writes=b_hT)
                load_w8(self.wbr_b, ll, 0, 0)
                for oc in range(8):
                    if oc + 1 < 8:
                        load_w8(self.wbr_b, ll, oc + 1, (oc + 1) % 3)
                    else:
                        load_w8(self.wout_b, ll, 0, (oc + 1) % 3)
                    slot = oc % 3
                    for br in range(3):
                        self.dma("sp", stg[br][:], self.d_fm[15 + br * 8 + oc, :, t0:t0 + TB], b_stg[br],
                                 reads=[self.b_dfm], writes=[b_stg[br]])
                    for tg in range(2):
                        cs = slice(tg * 512, (tg + 1) * 512)
                        for br in range(3):
                            pp, bp = ps[tg * 3 + br], b_ps[tg * 3 + br]
                            k0, k1 = KCS[br]
                            for kc in range(k0, k1):
                                self.I("pe", [b_wfm[slot], b_hT[kc]], [bp],
                                       lambda: nc.tensor.matmul(pp[:], lhsT=wfm[slot][:, kc, :], rhs=hT[:, kc, cs],
                                                                start=(kc == k0), stop=(kc == k1 - 1)))
                        p0, p1, p2 = ps[tg * 3], ps[tg * 3 + 1], ps[tg * 3 + 2]
                        q0, q1, q2 = b_ps[tg * 3], b_ps[tg * 3 + 1], b_ps[tg * 3 + 2]
                        self.I("dve", [q0, b_stg[0]], [b_sg[0]],
                               lambda: nc.vector.tensor_tensor(out=sg[0][:], in0=stg[0][:, cs], in1=p0[:], op=ALU.mult))
                        self.I("dve", [q1, b_stg[1]], [b_sg[1]],
                               lambda: nc.vector.tensor_tensor(out=sg[1][:], in0=stg[1][:, cs], in1=p1[:], op=ALU.mult))
                        self.I("dve", [b_sg[0], b_sg[1]], [b_sg[0]],
                               lambda: nc.vector.tensor_tensor(out=sg[0][:], in0=sg[0][:], in1=sg[1][:], op=ALU.add))
                        self.I("dve", [q2, b_stg[2]], [b_sg[1]],
                               lambda: nc.vector.tensor_tensor(out=sg[1][:], in0=stg[2][:, cs], in1=p2[:], op=ALU.mult))
                        self.I("dve", [b_sg[0], b_sg[1]], [b_actT[oc]],
                               lambda: nc.vector.tensor_tensor(out=actT[:, oc, cs], in0=sg[0][:], in1=sg[1][:],
                                                               op=ALU.add))
                for oc in range(8):
                    if oc + 1 < 8:
                        load_w8(self.wout_b, ll, oc + 1, (oc + 9) % 3)
                    slot = (oc + 8) % 3
                    for tg in range(2):
                        cs = slice(tg * 512, (tg + 1) * 512)
                        pp, bp = ps[6 + tg], b_ps[6 + tg]
                        for kc in range(8):
                            self.I("pe", [b_wfm[slot], b_actT[kc]], [bp],
                                   lambda: nc.tensor.matmul(pp[:], lhsT=wfm[slot][:, kc, :], rhs=actT[:, kc, cs],
                                                            start=(kc == 0), stop=(kc == 7)))
                        self.I("dve", [bp, b_xb[oc]], [b_xb[oc]],
                               lambda: nc.vector.tensor_tensor(out=xb[:, oc, cs], in0=xb[:, oc, cs], in1=pp[:],
                                                               op=ALU.add))

            for tb in range(self.debug.get('ntb', NTB)):
                t0 = tb * TB
                src = self.xT_in if l == 0 else self.xw
                self.dma("sp", xb[:], src.rearrange("(k p) t -> p k t", p=128)[:, :, t0:t0 + TB], b_xb[0],
                         reads=([] if l == 0 else [self.b_xw]), writes=b_xb)
                if l > 0:
                    merge(l - 1, tb)
                    ffn(l - 1, 1, 2)
                if l < self.nl:
                    ffn(l, 0, 0)
                    if not self.debug.get("no_proj"):
                        proj(l, tb)
                else:
                    norm(0, 0, final=True)
                dst = self.xw if l < self.nl else self.out
                if self.debug.get("x_to_out"):
                    dst = self.out
                self.dma("pool", dst.rearrange("(k p) t -> p k t", p=128)[:, :, t0:t0 + TB], xb[:], b_xb[0],
                         reads=b_xb, writes=[self.b_xw])
            self.phase_end()

    def finish(self):
        self.barrier()


def prep_weights(inp):
    f = np.float32
    out = {}
    nw = np.zeros((128, 3 * L * 8 + 8), f)
    for g, k in enumerate(["ffn1_norm", "mix_norm", "ffn2_norm"]):
        a = np.asarray(inp[k], f)
        nw[:, g * L * 8:(g + 1) * L * 8] = a.reshape(L, 8, 128).transpose(2, 0, 1).reshape(128, L * 8)
    nw[:, 3 * L * 8:] = np.asarray(inp["final_norm"], f).reshape(8, 128).T
    out["normw"] = nw
    wgu = np.empty((L, 2, NFC, 128, 2, 8, 128), f)
    wd = np.empty((L, 2, 8, 128, NFC, 128), f)
    for j, pre in enumerate(["ffn1", "ffn2"]):
        g = np.asarray(inp[pre + "_w_gate"], f).reshape(L, 8, 128, NFC, 128)
        u = np.asarray(inp[pre + "_w_up"], f).reshape(L, 8, 128, NFC, 128)
        wgu[:, j, :, :, 0] = g.transpose(0, 3, 2, 1, 4)
        wgu[:, j, :, :, 1] = u.transpose(0, 3, 2, 1, 4)
        dn = np.asarray(inp[pre + "_w_down"], f).reshape(L, NFC, 128, 8, 128)
        wd[:, j] = dn.transpose(0, 3, 2, 1, 4)
    out["wgu"] = wgu.reshape(L * 2 * NFC * 128, 2 * 8 * 128)
    out["wd"] = wd.reshape(L * 2 * 8 * 128, NFC * 128)
    w_in = np.asarray(inp["w_in"], f)
    w_pad = np.concatenate([w_in, np.zeros((L, D, 1), f)], axis=2)
    chunks = np.array(fm_chunk_cols())
    fm = w_pad[:, :, chunks]
    fm = fm.reshape(L, 8, 128, NFM, 128).transpose(0, 3, 2, 1, 4)
    out["winfm"] = np.ascontiguousarray(fm).reshape(L * NFM * 128, 8 * 128)
    tmc = list(range(O_VC, O_VC + 384)) + list(range(O_WI, O_WI + 4)) + list(range(O_FC, O_FC + 6))
    tm = w_in[:, :, tmc].reshape(L, 8, 128, NTM).transpose(0, 2, 1, 3)
    out["wintm"] = np.ascontiguousarray(tm).reshape(L * 128, 8 * NTM)
    ii = np.arange(128)
    cst = np.zeros((128, 6 * 128), f)
    cst[:, 0:128] = np.eye(128, dtype=f)
    cst[:, 128:256] = np.where(ii[:, None] > ii[None, :], -30000.0, 0.0)
    cst[:, 256:384] = np.where(ii[None, :] > ii[:, None], -1e30, 0.0)
    cst[:, 384:512] = (ii[:, None] <= ii[None, :]).astype(f)
    cst[:, 512:640] = 1.0
    out["consts"] = cst
    out["foxbf"] = np.ascontiguousarray(np.broadcast_to(np.asarray(inp["fox_b_f"], f).reshape(1, L * 6), (128, L * 6)))
    out["kvnorm"] = np.ascontiguousarray(np.asarray(inp["dsa_kv_norm"], f).T)
    out["wuk"] = np.ascontiguousarray(np.asarray(inp["dsa_w_uk"], f).reshape(L * 128, 64))
    out["wuv"] = np.ascontiguousarray(np.asarray(inp["dsa_w_uv"], f).reshape(L * 128, 64))
    rb = np.asarray(inp["rel_bias"], f)
    out["rb31"] = np.ascontiguousarray(np.broadcast_to(rb[31][:, None], (6, S))).reshape(1, 6 * S)
    dist = ii[None, None, :] - ii[:, None, None] + 128 * np.arange(2)[None, :, None]
    dd = np.maximum(dist, 1).astype(np.float32)
    logb = 16 + (np.log(dd / np.float32(16)) / np.float32(math.log(128 / 16)) * np.float32(16)).astype(np.int32)
    bucket = np.where(np.maximum(dist, 0) < 16, np.maximum(dist, 0), np.minimum(logb, 31))
    rbT = rb[bucket]
    rbT = np.where((dist >= 0)[..., None], rbT, 0.0).astype(f)
    out["rbT"] = np.ascontiguousarray(rbT.transpose(0, 1, 3, 2)).reshape(128, 2 * 6 * 128)
    def st(a):
        a = np.asarray(a, f)
        a = a.reshape((L, 8, 128) + a.shape[3:])
        return np.ascontiguousarray(np.moveaxis(a, 2, 0))
    lr = st(inp["ssm_lambda_re"]); li = st(inp["ssm_lambda_im"])
    ldt = st(np.broadcast_to(np.asarray(inp["ssm_log_dt"], f)[:, :, None], (L, 16, 64)))
    out["s5p"] = np.ascontiguousarray(np.stack([lr, li, ldt], axis=-1)).reshape(128, L * 8 * 3)
    bre = st(inp["ssm_b_re"]); bim = st(inp["ssm_b_im"])
    out["s5b"] = np.ascontiguousarray(np.stack([bre, bim], axis=3)).reshape(128, L * 8 * 2 * 16)
    cre = st(np.asarray(inp["ssm_c_re"], f).transpose(0, 1, 3, 2)); cim = st(np.asarray(inp["ssm_c_im"], f).transpose(0, 1, 3, 2))
    out["s5c"] = np.ascontiguousarray(np.stack([cre, cim], axis=3)).reshape(128, L * 8 * 2 * 16)
    out["s5d"] = np.ascontiguousarray(np.asarray(inp["ssm_d"], f).reshape(L, 2, 128).transpose(2, 0, 1)).reshape(128, L * 2)
    wg = np.asarray(inp["ssm_w_glu"], f).reshape(L, 2, 128, 512).transpose(0, 2, 1, 3)
    out["wglu"] = np.ascontiguousarray(wg).reshape(L * 128, 2 * 512)
    wbr = np.concatenate([np.asarray(inp["w_branch_ssm"], f), np.asarray(inp["w_branch_dsa"], f),
                          np.asarray(inp["w_branch_fox"], f)], axis=1)
    out["wbr"] = np.ascontiguousarray(wbr.reshape(L, 8, 128, 8, 128).transpose(0, 3, 2, 1, 4)).reshape(L * 8 * 128, 8 * 128)
    wo = np.asarray(inp["w_out"], f)
    out["wout"] = np.ascontiguousarray(wo.reshape(L, 8, 128, 8, 128).transpose(0, 3, 2, 1, 4)).reshape(L * 8 * 128, 8 * 128)
    return out


def run(inputs, n_layers=L, debug=None, cores=NCORES):
    b = Builder(n_layers=n_layers, debug=debug)
    nc = b.build()
    w = prep_weights(inputs)
    x = np.asarray(inputs["x"], np.float32)
    in_maps = []
    for c in range(cores):
        xs = x[c * NSEQ:(c + 1) * NSEQ].reshape(TOK, D)
        m = {"xT": np.ascontiguousarray(xs.T)}
        m.update(w)
        if n_layers < L:
            m["wgu"] = m["wgu"][:n_layers * 2 * NFC * 128]
            m["wd"] = m["wd"][:n_layers * 2 * 8 * 128]
            m["winfm"] = m["winfm"][:n_layers * NFM * 128]
            m["wintm"] = m["wintm"][:n_layers * 128]
            m["wbr"] = m["wbr"][:n_layers * 8 * 128]
            m["wout"] = m["wout"][:n_layers * 8 * 128]
        in_maps.append(m)
    res = run_bass_kernel_spmd(nc, in_maps, core_ids=list(range(cores)))
    return res


def kernel(**inputs):
    res = run(inputs)
    outs = []
    for c in range(NCORES):
        o = res.results[c]["out"]
        outs.append(np.ascontiguousarray(o.T).reshape(NSEQ, S, D))
    return np.concatenate(outs, axis=0).astype(np.float32)
```
